# Optimizing a Trainium2 kernel written in Bass

```python
import jax
import jax.numpy as jnp
from jax import lax
import numpy as np


D_MODEL = 1024
BATCH = 4
SEQ = 8192
DEPTH = 1

N_META = 16
CHUNK = 128
PAD_FRONT = CHUNK - N_META
D_MIX = D_MODEL
RET_HEADS = 4
RET_DK = D_MIX // 2 // RET_HEADS
RET_DV = D_MIX // 2 // RET_HEADS
MLSTM_HEADS = 4
MLSTM_DH = D_MIX // 2 // MLSTM_HEADS
CONV_K = 5
N_EXPERTS = 16
EC_CAPACITY = 2
D_FF = 128 * ((8 * D_MODEL // 3 + 127) // 128)
ROPE_BASE = 10000.0
EPS = 1e-6
NEG = -1e30
RET_QK = RET_HEADS * RET_DK
RET_V = RET_HEADS * RET_DV
ML_W = MLSTM_HEADS * MLSTM_DH
N_GATES = 4 * MLSTM_HEADS
PROJ_SIZES = (RET_QK, RET_QK, RET_V, RET_V, ML_W, ML_W, ML_W, ML_W, N_GATES)
PROJ_COLS = sum(PROJ_SIZES)
SPLIT_AT = tuple(int(s) for s in np.cumsum(PROJ_SIZES)[:-1])

kernel_name = 'hybrid_retention_mlstm_ec_block'


def _rmsnorm(x, g):
    xf = x.astype(jnp.float32)
    y = xf * lax.rsqrt(jnp.mean(xf * xf, axis=-1, keepdims=True) + EPS)
    return (y * g.astype(jnp.float32)).astype(x.dtype)


def _head_norm(y, g):
    mu = jnp.mean(y, axis=-1, keepdims=True)
    yc = y - mu
    var = jnp.mean(yc * yc, axis=-1, keepdims=True)
    out = (yc * lax.rsqrt(var + EPS)).reshape(y.shape[:2] + (-1,))
    return out * g.astype(jnp.float32)


def _rotary(x, pos):
    half = x.shape[-1] // 2
    inv = ROPE_BASE ** (-jnp.arange(half, dtype=jnp.float32) / half)
    ang = pos[:, None] * inv[None, :]
    cos = jnp.cos(ang)[None, :, None, :]
    sin = jnp.sin(ang)[None, :, None, :]
    x1, x2 = x[..., :half], x[..., half:]
    return jnp.concatenate([x1 * cos - x2 * sin, x2 * cos + x1 * sin], axis=-1)


def _pad_front(t, value=0.0):
    widths = [(0, 0)] * t.ndim
    widths[1] = (PAD_FRONT, 0)
    return jnp.pad(t, widths, constant_values=value)


def _flip(t):
    return jnp.flip(t, axis=1)


def _centred_conv(u, w, b):
    c = u.shape[-1]
    out = lax.conv_general_dilated(
        u, w.astype(u.dtype)[:, None, :], window_strides=(1,),
        padding=((CONV_K // 2, CONV_K // 2),),
        dimension_numbers=('NWC', 'WIO', 'NWC'), feature_group_count=c)
    return out + b.astype(u.dtype)


def _retention_dir(q, k, v, log_gamma):
    b_, t_, h_, dk = q.shape
    dv = v.shape[-1]
    nc = t_ // CHUNK
    q = q.reshape(b_, nc, CHUNK, h_, dk)
    k = k.reshape(b_, nc, CHUNK, h_, dk)
    v = v.reshape(b_, nc, CHUNK, h_, dv)
    idx = jnp.arange(CHUNK, dtype=jnp.float32)
    diff = idx[:, None] - idx[None, :]
    decay = jnp.where(diff >= 0, jnp.exp(log_gamma[:, None, None] * jnp.maximum(diff, 0.0)), 0.0)
    scores = jnp.einsum('bclhd,bcmhd->bchlm', q, k) * decay
    intra = jnp.einsum('bchlm,bcmhe->bclhe', scores, v)
    zeta = jnp.exp(log_gamma[:, None] * (CHUNK - 1.0 - idx)[None, :])
    upd = jnp.einsum('bclhd,bclhe,hl->bchde', k, v, zeta)
    g_chunk = jnp.exp(log_gamma * CHUNK)[:, None, None]

    def step(state, u_c):
        return g_chunk * state + u_c, state

    r0 = jnp.zeros((b_, h_, dk, dv), jnp.float32)
    _, r_prev = lax.scan(step, r0, jnp.moveaxis(upd, 1, 0))
    r_prev = jnp.moveaxis(r_prev, 0, 1)
    xi = jnp.exp(log_gamma[:, None] * (idx + 1.0)[None, :])
    cross = jnp.einsum('bclhd,bchde,hl->bclhe', q, r_prev, xi)
    return (intra + cross).reshape(b_, t_, h_, dv)


def _mlstm_dir(q, k, v, log_i, log_f):
    b_, t_, h_, d = q.shape
    nc = t_ // CHUNK
    q = q.reshape(b_, nc, CHUNK, h_, d)
    k = k.reshape(b_, nc, CHUNK, h_, d)
    v = v.reshape(b_, nc, CHUNK, h_, d)
    li = jnp.moveaxis(log_i.reshape(b_, nc, CHUNK, h_), 3, 2)
    lf = jnp.moveaxis(log_f.reshape(b_, nc, CHUNK, h_), 3, 2)
    bcum = jnp.cumsum(lf, axis=-1)
    b_last = bcum[..., -1]
    causal = jnp.tril(jnp.ones((CHUNK, CHUNK), dtype=bool))
    log_d = jnp.where(causal, bcum[..., :, None] - bcum[..., None, :] + li[..., None, :], NEG)
    log_u = b_last[..., None] - bcum + li
    a = jnp.max(log_u, axis=-1)
    w_u = jnp.exp(log_u - a[..., None])
    upd_c = jnp.einsum('bchl,bclhe,bclhd->bched', w_u, v, k)
    upd_n = jnp.einsum('bchl,bclhd->bchd', w_u, k)

    def step(carry, inp):
        c_st, n_st, m_st = carry
        uc, un, bl, ac = inp
        m_new = jnp.maximum(bl + m_st, ac)
        f = jnp.exp(bl + m_st - m_new)
        g = jnp.exp(ac - m_new)
        c_new = f[..., None, None] * c_st + g[..., None, None] * uc
        n_new = f[..., None] * n_st + g[..., None] * un
        return (c_new, n_new, m_new), (c_st, n_st, m_st)

    init = (jnp.zeros((b_, h_, d, d), jnp.float32),
            jnp.zeros((b_, h_, d), jnp.float32),
            jnp.zeros((b_, h_), jnp.float32))
    xs = (jnp.moveaxis(upd_c, 1, 0), jnp.moveaxis(upd_n, 1, 0),
          jnp.moveaxis(b_last, 1, 0), jnp.moveaxis(a, 1, 0))
    _, (c_prev, n_prev, m_prev) = lax.scan(step, init, xs)
    c_prev = jnp.moveaxis(c_prev, 0, 1)
    n_prev = jnp.moveaxis(n_prev, 0, 1)
    m_prev = jnp.moveaxis(m_prev, 0, 1)
    log_inter = bcum + m_prev[..., None]
    m_row = jnp.maximum(log_inter, jnp.max(log_d, axis=-1))
    s = jnp.einsum('bclhd,bcmhd->bchlm', q, k) * jnp.exp(log_d - m_row[..., None])
    w_inter = jnp.exp(log_inter - m_row)
    num = (jnp.einsum('bchlm,bcmhe->bclhe', s, v)
           + jnp.einsum('bclhd,bched,bchl->bclhe', q, c_prev, w_inter))
    den = jnp.sum(s, axis=-1) + jnp.einsum('bclhd,bchd,bchl->bchl', q, n_prev, w_inter)
    denom = jnp.maximum(jnp.abs(den), jnp.exp(-m_row))
    h = num / jnp.moveaxis(denom, 2, 3)[..., None]
    return h.reshape(b_, t_, h_, d)


def _token_mixer(h, norm_g, w_in, b_gates, conv_w, conv_b, ret_decay_logit,
                 ret_gn_g, mlstm_gn_g, w_out):
    b_, n_, _ = h.shape
    f32 = jnp.float32
    u = _rmsnorm(h, norm_g)
    proj = jnp.einsum('bnd,dk->bnk', u, w_in)
    rq, rk, rv, rg, mq, mk, mv, mo, gate_pre = jnp.split(proj, SPLIT_AT, axis=-1)

    pos = jnp.arange(n_, dtype=f32)
    rq = _rotary(rq.reshape(b_, n_, RET_HEADS, RET_DK).astype(f32), pos) * (RET_DK ** -0.5)
    rk = _rotary(rk.reshape(b_, n_, RET_HEADS, RET_DK).astype(f32), pos)
    rv = rv.reshape(b_, n_, RET_HEADS, RET_DV).astype(f32)
    rq, rk, rv = _pad_front(rq), _pad_front(rk), _pad_front(rv)
    log_gamma = jax.nn.log_sigmoid(ret_decay_logit.astype(f32))
    ret = (_retention_dir(rq, rk, rv, log_gamma[0])
           + _flip(_retention_dir(_flip(rq), _flip(rk), _flip(rv), log_gamma[1])))
    ret = ret[:, PAD_FRONT:]
    y_ret = _head_norm(ret, ret_gn_g) * jax.nn.silu(rg.astype(f32))

    qk = jax.nn.silu(_centred_conv(jnp.concatenate([mq, mk], axis=-1), conv_w, conv_b))
    mq, mk = jnp.split(qk, 2, axis=-1)
    mq = mq.reshape(b_, n_, MLSTM_HEADS, MLSTM_DH).astype(f32) * (MLSTM_DH ** -0.5)
    mk = mk.reshape(b_, n_, MLSTM_HEADS, MLSTM_DH).astype(f32)
    mv = mv.reshape(b_, n_, MLSTM_HEADS, MLSTM_DH).astype(f32)
    gts = gate_pre.astype(f32) + b_gates.astype(f32)
    log_i = gts[..., :2 * MLSTM_HEADS].reshape(b_, n_, 2, MLSTM_HEADS)
    log_f = jax.nn.log_sigmoid(gts[..., 2 * MLSTM_HEADS:]).reshape(b_, n_, 2, MLSTM_HEADS)
    mq, mk, mv = _pad_front(mq), _pad_front(mk), _pad_front(mv)
    log_i = _pad_front(log_i, NEG)
    log_f = _pad_front(log_f, 0.0)
    h_f = _mlstm_dir(mq, mk, mv, log_i[:, :, 0], log_f[:, :, 0])
    h_b = _flip(_mlstm_dir(_flip(mq), _flip(mk), _flip(mv),
                           _flip(log_i[:, :, 1]), _flip(log_f[:, :, 1])))
    hm = (h_f + h_b)[:, PAD_FRONT:]
    hm = jax.nn.sigmoid(mo.astype(f32)).reshape(b_, n_, MLSTM_HEADS, MLSTM_DH) * hm
    y_m = _head_norm(hm, mlstm_gn_g)

    y = jnp.concatenate([y_ret, y_m], axis=-1).astype(h.dtype)
    return jnp.einsum('bnk,kd->bnd', y, w_out)


def _expert_choice_ffn(h, norm_g, w_router, w_gate, w_up, w_down):
    b_, n_, _ = h.shape
    u = _rmsnorm(h, norm_g)
    aff = jax.nn.softmax(jnp.einsum('bnd,de->bne', u, w_router).astype(jnp.float32), axis=-1)
    cap = EC_CAPACITY * n_ // N_EXPERTS
    g, idx = lax.top_k(jnp.swapaxes(aff, 1, 2), cap)
    xs = jax.vmap(lambda ub, ib: ub[ib])(u, idx)
    hid = (jax.nn.silu(jnp.einsum('becd,edf->becf', xs, w_gate))
           * jnp.einsum('becd,edf->becf', xs, w_up))
    y = jnp.einsum('becf,efd->becd', hid, w_down) * g[..., None].astype(h.dtype)
    return jnp.zeros_like(h).at[jnp.arange(b_)[:, None, None], idx].add(y)


def setup_inputs(seed: int = 0) -> dict:
    key = jax.random.key(seed)
    ks = jax.random.split(key, 17)
    f32 = jnp.float32
    nrm = jax.random.normal
    x = nrm(ks[0], (BATCH, SEQ, D_MODEL), f32)
    meta_tokens = nrm(ks[1], (N_META, D_MODEL), f32)
    mix_norm_g = 1.0 + 0.05 * nrm(ks[2], (DEPTH, D_MODEL), f32)
    w_in = nrm(ks[3], (DEPTH, D_MODEL, PROJ_COLS), f32) * (D_MODEL ** -0.5)
    f_bias = jnp.tile(jnp.linspace(3.0, 6.0, MLSTM_HEADS, dtype=f32), 2)
    base_gates = jnp.concatenate([jnp.zeros((2 * MLSTM_HEADS,), f32), f_bias])
    b_gates = base_gates[None, :] + 0.1 * nrm(ks[4], (DEPTH, N_GATES), f32)
    conv_w = nrm(ks[5], (DEPTH, CONV_K, 2 * ML_W), f32) * (CONV_K ** -0.5)
    conv_b = 0.01 * nrm(ks[6], (DEPTH, 2 * ML_W), f32)
    scales = 5.0 + jnp.arange(RET_HEADS, dtype=f32)
    gamma_logit = jnp.log(2.0 ** scales - 1.0)
    ret_decay_logit = gamma_logit[None, None, :] + 0.05 * nrm(ks[7], (DEPTH, 2, RET_HEADS), f32)
    ret_gn_g = 1.0 + 0.05 * nrm(ks[8], (DEPTH, RET_V), f32)
    mlstm_gn_g = 1.0 + 0.05 * nrm(ks[9], (DEPTH, ML_W), f32)
    w_out = nrm(ks[10], (DEPTH, D_MIX, D_MODEL), f32) * (D_MIX ** -0.5)
    ffn_norm_g = 1.0 + 0.05 * nrm(ks[11], (DEPTH, D_MODEL), f32)
    w_router = nrm(ks[12], (DEPTH, D_MODEL, N_EXPERTS), f32) * (D_MODEL ** -0.5)
    w_gate = nrm(ks[13], (DEPTH, N_EXPERTS, D_MODEL, D_FF), f32) * (D_MODEL ** -0.5)
    w_up = nrm(ks[14], (DEPTH, N_EXPERTS, D_MODEL, D_FF), f32) * (D_MODEL ** -0.5)
    w_down = nrm(ks[15], (DEPTH, N_EXPERTS, D_FF, D_MODEL), f32) * (D_FF ** -0.5)
    final_norm_g = 1.0 + 0.05 * nrm(ks[16], (D_MODEL,), f32)
    return {'x': x, 'meta_tokens': meta_tokens, 'mix_norm_g': mix_norm_g, 'w_in': w_in,
            'b_gates': b_gates, 'conv_w': conv_w, 'conv_b': conv_b,
            'ret_decay_logit': ret_decay_logit, 'ret_gn_g': ret_gn_g, 'mlstm_gn_g': mlstm_gn_g,
            'w_out': w_out, 'ffn_norm_g': ffn_norm_g, 'w_router': w_router, 'w_gate': w_gate,
            'w_up': w_up, 'w_down': w_down, 'final_norm_g': final_norm_g}


def reference(x, meta_tokens, mix_norm_g, w_in, b_gates, conv_w, conv_b, ret_decay_logit,
              ret_gn_g, mlstm_gn_g, w_out, ffn_norm_g, w_router, w_gate, w_up, w_down,
              final_norm_g):
    b_ = x.shape[0]
    meta = jnp.broadcast_to(meta_tokens.astype(x.dtype)[None], (b_, N_META, x.shape[-1]))
    h = jnp.concatenate([meta, x], axis=1)
    for l in range(DEPTH):
        h = h + _token_mixer(h, mix_norm_g[l], w_in[l], b_gates[l], conv_w[l], conv_b[l],
                             ret_decay_logit[l], ret_gn_g[l], mlstm_gn_g[l], w_out[l])
        h = h + _expert_choice_ffn(h, ffn_norm_g[l], w_router[l], w_gate[l], w_up[l], w_down[l])
    h = _rmsnorm(h, final_norm_g)
    return h[:, N_META:]
```

```python
import math
import numpy as np
import ml_dtypes
from contextlib import ExitStack
import concourse.bass as bass
import concourse.mybir as mybir
from concourse.bass_utils import run_bass_kernel_spmd

F32 = mybir.dt.float32
BF16 = mybir.dt.bfloat16
I32 = mybir.dt.int32
ALU = mybir.AluOpType
AF = mybir.ActivationFunctionType
AX = mybir.AxisListType

ENGS = ['pe', 'act', 'dve', 'pool', 'sp']
EPOCH = 12000


class Res:
    __slots__ = ('name', 'w', 'rs')

    def __init__(self, name):
        self.name = name
        self.w = None
        self.rs = []


class Ins:
    __slots__ = ('eng', 'emit', 'deps', 'pos', 'needed', 'is_dma', 'semkey', 'dk', 'dv',
                 'waits', 'cbase', 'ord')


class Sched:
    def __init__(self, nc):
        self.nc = nc
        self.streams = {e: [] for e in ENGS}
        self.all = []
        self.finals = []
        self.pending = {e: [] for e in ENGS}

    def op(self, eng, emit, reads=(), writes=(), after=()):
        return self._add(eng, emit, reads, writes, False, None, after)

    def dma(self, queue, emit, reads=(), writes=(), semkey=None, after=()):
        return self._add(queue, emit, reads, writes, True, semkey, after)

    def final_wait(self, eng, ins_list):
        self.finals.append((eng, list(ins_list)))

    def barrier(self):
        lasts = []
        for e in ENGS:
            st = self.streams[e]
            for ins in reversed(st):
                if not ins.is_dma:
                    lasts.append(ins)
                    break
        dl = {}
        for i in self.all[getattr(self, '_bar_idx', 0):]:
            if i.is_dma:
                dl[i.semkey] = i
        dmas = list(dl.values())
        self._bar_idx = len(self.all)
        for e in ENGS:
            self.pending[e] = self.pending[e] + lasts + dmas

    def _add(self, eng, emit, reads, writes, is_dma, semkey, after=()):
        ins = Ins()
        ins.eng = eng
        ins.emit = emit
        ins.is_dma = is_dma
        ins.needed = is_dma
        ins.semkey = semkey
        deps = [(0, X) for X in after]
        if self.pending[eng]:
            deps += [(0, X) for X in self.pending[eng]]
            self.pending[eng] = []
        for r in reads:
            if r.w is not None:
                deps.append((0, r.w))
            r.rs.append(ins)
        for r in writes:
            if r.w is not None and r.w is not ins:
                deps.append((1, r.w))
            for q in r.rs:
                if q is not ins:
                    deps.append((2, q))
            r.w = ins
            r.rs = []
        ins.deps = deps
        ins.pos = len(self.streams[eng])
        self.streams[eng].append(ins)
        self.all.append(ins)
        return ins

    def finalize(self):
        eclock = {e: {} for e in ENGS}
        dcnt = {}
        for ins in self.all:
            ec = eclock[ins.eng]
            waits = []
            for kind, X in ins.deps:
                if X is ins:
                    continue
                if (not X.is_dma) and (not ins.is_dma) and X.eng == ins.eng:
                    if ins.eng == 'pe' or kind != 0:
                        continue
                if ec.get(X.dk, 0) >= X.dv:
                    continue
                waits.append(X)
                X.needed = True
                nec = dict(ec)
                for k, v in X.cbase.items():
                    if nec.get(k, 0) < v:
                        nec[k] = v
                if nec.get(X.dk, 0) < X.dv:
                    nec[X.dk] = X.dv
                ec = nec
                eclock[ins.eng] = ec
            ins.waits = waits
            if ins.is_dma:
                c = dcnt.get(ins.semkey, 0) + 1
                dcnt[ins.semkey] = c
                ins.dk = ('d', ins.semkey)
                ins.dv = c
            else:
                ins.dk = ins.eng
                ins.dv = ins.pos + 1
            ins.cbase = ec
        self.dma_keys = list(dcnt.keys())
        self.n_ord = {}
        for e in ENGS:
            o = 0
            for ins in self.streams[e]:
                if ins.needed and not ins.is_dma:
                    o += 1
                    ins.ord = o
            self.n_ord[e] = o

    def emit(self, stack):
        nc = self.nc
        self.finalize()
        csem = {}
        for e in ENGS:
            n = (self.n_ord[e] + EPOCH - 1) // EPOCH
            csem[e] = [stack.enter_context(nc.semaphore(f"c_{e}_{i}")) for i in range(n)]
        dsem = {}
        for i, k in enumerate(self.dma_keys):
            dsem[k] = stack.enter_context(nc.semaphore(f"d_{i}"))
        self.nsem = sum(len(v) for v in csem.values()) + len(dsem)

        def semval(X):
            if X.is_dma:
                return dsem[X.semkey], X.dv * 16
            o = X.ord - 1
            return csem[X.eng][o // EPOCH], (o % EPOCH) + 1

        def run(ename, eng):
            for ins in self.streams[ename]:
                for X in ins.waits:
                    s, v = semval(X)
                    eng.wait_ge(s, v)
                bi = ins.emit(eng)
                if ins.is_dma:
                    bi.then_inc(dsem[ins.semkey], 16)
                elif ins.needed:
                    s, v = semval(ins)
                    bi.then_inc(s, 1)
            for fe, lst in self.finals:
                if fe == ename:
                    for X in lst:
                        s, v = semval(X)
                        eng.wait_ge(s, v)

        block = stack.enter_context(nc.Block())

        @block.tensor
        def _(eng):
            run('pe', eng)

        @block.scalar
        def _(eng):
            run('act', eng)

        @block.vector
        def _(eng):
            run('dve', eng)

        @block.gpsimd
        def _(eng):
            run('pool', eng)

        @block.sync
        def _(eng):
            run('sp', eng)


D = 1024
PCOLS = 4112
DFF = 2816
NFC = DFF // 128
NEXP = 16
XW = 1088
EPS = 1e-6
PADF = 112
QSCALE = 128 ** -0.5


SB_BASE = 17408
SB_LIMIT = 229376


class Arena:
    def __init__(self, nc, base=0):
        self.nc = nc
        self.off = base
        self.n = 0

    def t(self, shape, dt, name=None):
        sz = {F32: 4, BF16: 2, I32: 4}[dt]
        per = 1
        for s in shape[1:]:
            per *= s
        nbytes = per * sz
        self.off = (self.off + 63) // 64 * 64
        Arena.cnt = getattr(Arena, 'cnt', 0) + 1
        h = self.nc.alloc_sbuf_tensor_at(name or f"t{Arena.cnt}", list(shape), dt, offset=self.off)
        self.off += nbytes
        assert self.off <= SB_LIMIT, f"SBUF overflow {self.off}"
        return h


def build(NT, NEXP_RUN=NEXP, dbg=False):
    T = NT * 128
    NREAL = T - PADF
    CAP = 2 * NREAL // 16
    NSL = (CAP + 127) // 128 + (1 if CAP % 128 == 0 else 0)
    NSLR = NSL * 128
    SEQ = T - 128
    nc = bass.Bass("TRN2", target_bir_lowering=False)

    def din(name, shape, dt=F32):
        return nc.dram_tensor(name, list(shape), dt, kind="ExternalInput").ap()

    def dscr(name, shape, dt):
        return nc.dram_tensor(name, list(shape), dt, kind=("ExternalOutput" if dbg else "Internal")).ap()

    x = din("x", [SEQ, D])
    meta = din("meta", [16, D])
    w_in = din("w_in", [D, PCOLS])
    mixg = din("mix_g", [1, D])
    b_gates = din("b_gates", [1, 16])
    conv_w = din("conv_w", [5, 1024])
    conv_b = din("conv_b", [1, 1024])
    rlogit = din("rlogit", [1, 8])
    gn_g = din("gn_g", [1, 1024])
    w_out = din("w_out", [D, D])
    ffn_g = din("ffn_g", [1, D])
    w_router = din("w_router", [D, 16])
    w_gate = din("w_gate", [NEXP, D, DFF])
    w_up = din("w_up", [NEXP, D, DFF])
    w_down = din("w_down", [NEXP, DFF, D])
    fin_g = din("fin_g", [1, D])
    c_ident = din("c_ident", [128, 128], BF16)
    c_tri = din("c_tri", [6, 128, 128])
    c_mask = din("c_mask", [2, 128, 128])
    c_slt = din("c_slt", [128, 128], BF16)
    c_cs = din("c_cs", [T, 512])
    c_tok = din("c_tok", [128, NT], I32)
    c_padm = din("c_padm", [128, 1])
    c_xsinit = din("c_xsinit", [NSLR, XW], BF16)

    out = nc.dram_tensor("out", [SEQ, D], F32, kind="ExternalOutput").ap()

    QT = dscr("QT", [NT, 128, 8, 128], BF16)
    KT = dscr("KT", [NT, 128, 8, 128], BF16)
    KM = dscr("KM", [NT, 128, 8, 128], BF16)
    VP = dscr("VP", [NT, 128, 8, 130], BF16)
    GO = dscr("GO", [NT, 128, 1024], F32)
    SBS = dscr("SBS", [NT, 128, 8, 130], BF16)
    U2X = dscr("U2X", [T, XW], BF16)
    XS = [dscr(f"XS{i}", [NSLR, XW], BF16) for i in range(NEXP)]
    OACC = dscr("OACC", [T + NSLR, D], F32)

    S = Sched(nc)
    st = ExitStack()
    _regs = {}

    def breg(e, v):
        if v not in _regs:
            _regs[v] = e.to_reg(v)
        return _regs[v]

    psb = [st.enter_context(nc.psum_tensor(f"ps{i}", [128, 512], F32)) for i in range(8)]
    r_ps = [Res(f"ps{i}") for i in range(8)]

    def psbf(i):
        return psb[i][:].bitcast(BF16)

    P = Arena(nc, SB_BASE)
    ident = P.t([128, 128], BF16); r_ident = Res("ident")
    tri = P.t([128, 6, 128], F32); r_tri = Res("tri")
    maskfb = P.t([128, 2, 128], F32); r_mask = Res("mask")
    padm = P.t([128, 1], F32); r_padm = Res("padm")
    tok = P.t([128, NT], I32); r_tok = Res("tok")
    PBASE0 = P.off
    SC = P.t([128, NT, 6, 8], F32); r_SC = [Res(f"SC{j}") for j in range(NT)]
    AFF = P.t([128, NT, 16], F32); r_AFF = [Res(f"AFF{j}") for j in range(NT)]
    PBASE = P.off

    def ld(dst, src, res, key, q='sp'):
        return S.dma(q, lambda e: e.dma_start(out=dst, in_=src), [], [res], key)

    ld(ident[:], c_ident[:, :], r_ident, 'c0')
    ld(tri[:], c_tri.rearrange("k m l -> m k l"), r_tri, 'c1')
    ld(maskfb[:], c_mask.rearrange("k m l -> m k l"), r_mask, 'c2')
    ld(padm[:], c_padm[:, :], r_padm, 'c3')
    ld(tok[:], c_tok[:, :], r_tok, 'c4')

    A1 = Arena(nc, PBASE)
    Wb = A1.t([128, 8, PCOLS], BF16); r_Wb = Res("Wb")
    wstg = [A1.t([128, 1028], F32) for _ in range(2)]; r_wstg = [Res("wstg0"), Res("wstg1")]
    mixgT = A1.t([128, 8], F32); r_mixgT = Res("mixgT")
    xt = [A1.t([128, D], F32) for _ in range(2)]; r_xt = [Res("xt0"), Res("xt1")]
    junk = A1.t([128, D], BF16); r_junk = Res("junk")
    ss = A1.t([128, 1], F32); r_ss = Res("ss")
    rstd = A1.t([128, 1], F32); r_rstd = Res("rstd")
    xn = A1.t([128, D], BF16); r_xn = Res("xn")
    UW = [A1.t([128, 8, 384], BF16) for _ in range(2)]; r_UW = [Res("UW0"), Res("UW1")]
    cs = [A1.t([128, 512], F32) for _ in range(2)]; r_cs = [Res("cs0"), Res("cs1")]
    qk32 = A1.t([128, 2, 512], F32); r_qk32 = Res("qk32")
    rt = A1.t([128, 4, 256], F32); r_rt = Res("rt")
    qkr = A1.t([128, 2, 512], BF16); r_qkr = Res("qkr")
    qkT = A1.t([128, 8, 128], BF16); r_qkT = Res("qkT")
    vp = [A1.t([128, 8, 130], BF16) for _ in range(2)]; r_vp = [Res("vp0"), Res("vp1")]
    go = [A1.t([128, 1024], F32) for _ in range(2)]; r_go = [Res("go0"), Res("go1")]
    G32 = A1.t([128, 32], F32); r_G32 = Res("G32")
    BASE = A1.t([128, 32], F32); r_BASE = Res("BASE")
    spx = A1.t([128, 16], F32); r_spx = Res("spx")
    A16 = A1.t([128, 16], F32); r_A16 = Res("A16")
    Xc = A1.t([128, 8, 132], F32); r_Xc = Res("Xc")
    cw = A1.t([128, 5, 8], F32); r_cw = Res("cw")
    cb = A1.t([128, 8], F32); r_cb = Res("cb")
    cacc = A1.t([128, 8, 128], F32); r_cacc = Res("cacc")
    mqkT = A1.t([128, 8, 128], BF16); r_mqkT = Res("mqkT")
    kmm = A1.t([128, 4, 128], BF16); r_kmm = Res("kmm")
    ctmp_t = A1.t([128, 8, 128], F32); r_ctmp = Res("ctmp")

    S.dma('sp', lambda e: e.dma_start(out=mixgT[:], in_=mixg.rearrange("o (c p) -> p (o c)", p=128),
                                      allow_slow_non_contiguous=True), [], [r_mixgT], 'w0')
    k = 0
    for c in range(8):
        for q4 in range(4):
            b = k % 2
            c0 = q4 * 1028
            S.dma('sp', lambda e, b=b, c=c, c0=c0: e.dma_start(out=wstg[b][:], in_=w_in[c * 128:(c + 1) * 128, c0:c0 + 1028]),
                  [], [r_wstg[b]], f'wst{b}')
            eng = 'dve' if k % 2 == 0 else 'pool'
            S.op(eng, lambda e, b=b, c=c, c0=c0: e.tensor_scalar(out=Wb[:, c, c0:c0 + 1028], in0=wstg[b][:], scalar1=mixgT[:, c:c + 1],
                                                                 scalar2=None, op0=ALU.mult),
                 [r_wstg[b], r_mixgT], [r_Wb])
            k += 1
    S.op('pool', lambda e: e.memset(BASE[:], 0.0), [], [r_BASE])
    S.op('pool', lambda e: e.memset(G32[:], 0.0), [], [r_G32])
    for (c0, src) in ((4, b_gates[:, 0:4]), (12, b_gates[:, 4:8]), (20, b_gates[:, 8:12]), (28, b_gates[:, 12:16]),
                      (16, rlogit[:, 0:4]), (24, rlogit[:, 4:8])):
        S.dma('sp', lambda e, c0=c0, src=src: e.dma_start(out=BASE[:, c0:c0 + 4], in_=src.to_broadcast([128, 4])), [], [r_BASE], 'w1')
    for t in range(5):
        S.dma('sp', lambda e, t=t: e.dma_start(out=cw[:, t, :], in_=conv_w[t:t + 1, :].rearrange("o (g p) -> p (o g)", p=128), allow_slow_non_contiguous=True), [], [r_cw], 'w2a')
    S.dma('sp', lambda e: e.dma_start(out=cb[:], in_=conv_b.rearrange("o (g p) -> p (o g)", p=128), allow_slow_non_contiguous=True), [], [r_cb], 'w2b')
    for b in range(2):
        S.op('pool', lambda e, b=b: e.memset(UW[b][:], 0.0), [], [r_UW[b]])
        S.op('pool', lambda e, b=b: e.memset(vp[b][:], 0.0), [], [r_vp[b]])
        S.op('pool', lambda e, b=b: e.memset(vp[b][:, :, 128:129], 1.0), [], [r_vp[b]])
    S.op('pool', lambda e: e.memset(xt[0][:], 0.0), [], [r_xt[0]])

    r_QT = [Res(f"QT{j}") for j in range(NT)]
    r_KT = [Res(f"KT{j}") for j in range(NT)]
    r_KM = [Res(f"KM{j}") for j in range(NT)]
    r_VP = [Res(f"VP{j}") for j in range(NT)]
    r_GO = [Res(f"GO{j}") for j in range(NT)]
    r_SBS = [Res(f"SBS{j}") for j in range(NT)]

    def x_rows(j):
        return x[(j - 1) * 128:j * 128, :]

    for j in range(NT + 1):
        b = j % 2
        if j < NT:
            if j == 0:
                S.dma('sp', lambda e: e.dma_start(out=xt[0][PADF:128, :], in_=meta[:, :]), [], [r_xt[0]], 'xt0')
            else:
                S.dma('sp', lambda e, j=j, b=b: e.dma_start(out=xt[b][:], in_=x_rows(j)), [], [r_xt[b]], f'xt{b}')
            S.dma('sp', lambda e, j=j, b=b: e.dma_start(out=cs[b][:], in_=c_cs[j * 128:(j + 1) * 128, :]), [], [r_cs[b]], f'cs{b}')
            S.op('act', lambda e, b=b: e.activation(out=junk[:], in_=xt[b][:], func=AF.Square, accum_out=ss[:]), [r_xt[b]], [r_junk, r_ss])
            S.op('act', lambda e: e.activation(out=rstd[:], in_=ss[:], func=AF.Sqrt, scale=1.0 / D, bias=EPS), [r_ss], [r_rstd])
            S.op('dve', lambda e: e.reciprocal(out=rstd[:], in_=rstd[:]), [r_rstd], [r_rstd])
            S.op('act', lambda e, b=b: e.activation(out=xn[:], in_=xt[b][:], func=AF.Copy, scale=rstd[:]), [r_xt[b], r_rstd], [r_xn])
            for c in range(8):
                S.op('pe', lambda e, c=c: e.transpose(out=psbf(0)[:, c * 128:(c + 1) * 128], in_=xn[:, c * 128:(c + 1) * 128], identity=ident[:]),
                     [r_xn, r_ident], [r_ps[0]])
        if j >= 1:
            S.op('dve', lambda e, b=b: e.tensor_copy(out=UW[b][:, :, 0:256], in_=UW[1 - b][:, :, 128:384]), [r_UW[1 - b]], [r_UW[b]])
        if j < NT:
            S.op('dve', lambda e, b=b: e.tensor_copy(out=UW[b][:, :, 256:384], in_=psbf(0).rearrange("p (c t) -> p c t", c=8)),
                 [r_ps[0]], [r_UW[b]])
        else:
            S.op('dve', lambda e, b=b: e.memset(UW[b][:, :, 256:384], 0.0), [], [r_UW[b]])

        if j < NT:
            def uT(c, b=b):
                return UW[b][:, c, 256:384]
            def tok_proj(bank, col0, ncol, b=b):
                for c in range(8):
                    lhs = UW[b][:, c, 256:384]
                    S.op('pe', lambda e, c=c, lhs=lhs: e.matmul(psb[bank][:, 0:ncol], lhsT=lhs, rhs=Wb[:, c, col0:col0 + ncol], start=(c == 0), stop=(c == 7)),
                         [r_UW[b], r_Wb], [r_ps[bank]])
            tok_proj(1, 0, 512)
            S.op('act', lambda e: e.copy(out=qk32[:, 0, :], in_=psb[1][:]), [r_ps[1]], [r_qk32])
            tok_proj(2, 512, 512)
            S.op('act', lambda e: e.copy(out=qk32[:, 1, :], in_=psb[2][:]), [r_ps[2]], [r_qk32])
            def rot(b=b):
                xv = qk32[:].rearrange("p a (h t d) -> p (a h) t d", h=4, t=2)
                ov = qkr[:].rearrange("p a (h t d) -> p (a h) t d", h=4, t=2)
                x1 = xv[:, :, 0, :]; x2 = xv[:, :, 1, :]
                cosv = cs[b][:, 0:256].rearrange("p (h d) -> p h d", h=4)
                sinv = cs[b][:, 256:512].rearrange("p (h d) -> p h d", h=4)
                tv = rt[:].rearrange("p k (a d) -> p k a d", a=4)
                for a in range(2):
                    X1 = x1[:, a * 4:(a + 1) * 4, :]; X2 = x2[:, a * 4:(a + 1) * 4, :]
                    O1 = ov[:, a * 4:(a + 1) * 4, 0, :]; O2 = ov[:, a * 4:(a + 1) * 4, 1, :]
                    S.op('dve', lambda e, X1=X1: e.tensor_tensor(out=tv[:, 0], in0=X1, in1=cosv, op=ALU.mult), [r_qk32, r_cs[b]], [r_rt])
                    S.op('pool', lambda e, X2=X2: e.tensor_tensor(out=tv[:, 1], in0=X2, in1=sinv, op=ALU.mult), [r_qk32, r_cs[b]], [r_rt])
                    S.op('dve', lambda e, O1=O1: e.tensor_tensor(out=O1, in0=tv[:, 0], in1=tv[:, 1], op=ALU.subtract), [r_rt], [r_qkr])
                    S.op('pool', lambda e, X2=X2: e.tensor_tensor(out=tv[:, 2], in0=X2, in1=cosv, op=ALU.mult), [r_qk32, r_cs[b]], [r_rt])
                    S.op('dve', lambda e, X1=X1: e.tensor_tensor(out=tv[:, 3], in0=X1, in1=sinv, op=ALU.mult), [r_qk32, r_cs[b]], [r_rt])
                    S.op('dve', lambda e, O2=O2: e.tensor_tensor(out=O2, in0=tv[:, 2], in1=tv[:, 3], op=ALU.add), [r_rt], [r_qkr])
            rot()
            for a in range(2):
                for h in range(4):
                    i = a * 4 + h
                    S.op('pe', lambda e, a=a, h=h, i=i: e.transpose(out=psbf(5)[:, i * 128:(i + 1) * 128], in_=qkr[:, a, h * 128:(h + 1) * 128], identity=ident[:]),
                         [r_qkr, r_ident], [r_ps[5]])
            S.op('act', lambda e: e.copy(out=qkT[:], in_=psbf(5).rearrange("p (i t) -> p i t", i=8)), [r_ps[5]], [r_qkT])
            S.dma('sp', lambda e, j=j: e.dma_start(out=QT[j, :, 0:4, :], in_=qkT[:, 0:4, :]), [r_qkT], [r_QT[j]], 'st_qkT')
            S.dma('sp', lambda e, j=j: e.dma_start(out=KT[j, :, 0:4, :], in_=qkT[:, 4:8, :]), [r_qkT], [r_KT[j]], 'st_qkT')
            S.dma('sp', lambda e, j=j: e.dma_start(out=KM[j, :, 0:4, :], in_=qkr[:, 1, :].rearrange("p (h d) -> p h d", h=4)), [r_qkr], [r_KM[j]], 'st_qkr')
            tok_proj(1, 1024, 512)
            S.op('act', lambda e, b=b: e.copy(out=vp[b][:, 0:4, 0:128], in_=psb[1][:].rearrange("p (h d) -> p h d", h=4)), [r_ps[1]], [r_vp[b]])
            tok_proj(2, 1536, 512)
            S.op('act', lambda e, b=b: e.activation(out=go[b][:, 0:512], in_=psb[2][:], func=AF.Silu), [r_ps[2]], [r_go[b]])
            tok_proj(1, 3072, 512)
            S.op('act', lambda e, b=b: e.copy(out=vp[b][:, 4:8, 0:128], in_=psb[1][:].rearrange("p (h d) -> p h d", h=4)), [r_ps[1]], [r_vp[b]])
            tok_proj(2, 3584, 512)
            S.op('act', lambda e, b=b: e.activation(out=go[b][:, 512:1024], in_=psb[2][:], func=AF.Sigmoid), [r_ps[2]], [r_go[b]])
            S.dma('sp', lambda e, j=j, b=b: e.dma_start(out=VP[j], in_=vp[b][:]), [r_vp[b]], [r_VP[j]], f'st_vp{b}')
            S.dma('sp', lambda e, j=j, b=b: e.dma_start(out=GO[j], in_=go[b][:]), [r_go[b]], [r_GO[j]], f'st_go{b}')
            tok_proj(6, 4096, 16)
            g32v = G32[:].rearrange("p (a h) -> p a h", a=4)
            basev = BASE[:].rearrange("p (a h) -> p a h", a=4)
            S.op('dve', lambda e: e.tensor_tensor(out=g32v[:, :, 4:8], in0=psb[6][:, 0:16].rearrange("p (a h) -> p a h", a=4), in1=basev[:, :, 4:8], op=ALU.add),
                 [r_ps[6], r_BASE], [r_G32])
            if j == 0:
                S.op('dve', lambda e: e.tensor_copy(out=g32v[:, :, 0:4], in_=basev[:, :, 0:4]), [r_BASE], [r_G32])
            S.op('act', lambda e: e.activation(out=spx[:], in_=G32[:, 16:32], func=AF.Exp, scale=-1.0), [r_G32], [r_spx])
            S.op('act', lambda e: e.activation(out=spx[:], in_=spx[:], func=AF.Ln, bias=1.0), [r_spx], [r_spx])
            k0 = 3 if j == 0 else 0
            for q in range(3):
                S.op('pe', lambda e, q=q, k0=k0: e.matmul(psb[6][:, 16 + q * 16:32 + q * 16], lhsT=tri[:, k0 + q, :], rhs=spx[:], start=True, stop=True),
                     [r_tri, r_spx], [r_ps[6]])
            cumv = psb[6][:, 16:64].rearrange("p (a b) -> p a b", b=24)[:, :, 0:8]
            totv = psb[6][:, 48:64].rearrange("p (a h) -> p a h", a=2)
            S.op('dve', lambda e: e.tensor_tensor(out=A16[:].rearrange("p (a h) -> p a h", a=2), in0=cumv, in1=G32[:, 0:16].rearrange("p (a h) -> p a h", a=2), op=ALU.add),
                 [r_ps[6], r_G32], [r_A16])
            S.op('act', lambda e, j=j: e.activation(out=SC[:, j, 0:2, :], in_=A16[:].rearrange("p (a h) -> p a h", a=2), func=AF.Exp), [r_A16], [r_SC[j]])
            S.op('act', lambda e, j=j: e.activation(out=SC[:, j, 2:4, :], in_=cumv, func=AF.Exp, scale=-1.0, bias=math.log(QSCALE)), [r_ps[6]], [r_SC[j]])
            S.op('act', lambda e, j=j: e.activation(out=SC[:, j, 4:6, :], in_=totv, func=AF.Exp, scale=-1.0), [r_ps[6]], [r_SC[j]])

        if j >= 1:
            jj = j - 1
            for g in range(8):
                bank = 3 if g < 3 else (4 if g < 6 else 7)
                off = (g % 3) * 132
                col0 = 2048 + g * 128
                for c in range(8):
                    S.op('pe', lambda e, c=c, bank=bank, off=off, col0=col0, b=b: e.matmul(psb[bank][:, off:off + 132], lhsT=Wb[:, c, col0:col0 + 128],
                                                                                      rhs=UW[b][:, c, 126:258], start=(c == 0), stop=(c == 7)),
                         [r_UW[b], r_Wb], [r_ps[bank]])
            for bank, g0, ng in ((3, 0, 3), (4, 3, 3), (7, 6, 2)):
                S.op('act', lambda e, bank=bank, g0=g0, ng=ng: e.copy(out=Xc[:, g0:g0 + ng, :], in_=psb[bank][:, 0:ng * 132].rearrange("p (g t) -> p g t", g=ng)),
                     [r_ps[bank]], [r_Xc])
            ctmp = ctmp_t[:]
            for t in range(5):
                wb_t = cw[:, t, :].unsqueeze(2).to_broadcast([128, 8, 128])
                if t == 0:
                    S.op('pool', lambda e, wb_t=wb_t: e.tensor_tensor(out=cacc[:], in0=Xc[:, :, 0:128], in1=wb_t, op=ALU.mult), [r_Xc, r_cw], [r_cacc])
                else:
                    S.op('pool', lambda e, wb_t=wb_t, t=t: e.tensor_tensor(out=ctmp, in0=Xc[:, :, t:t + 128], in1=wb_t, op=ALU.mult), [r_Xc, r_cw], [r_ctmp])
                    S.op('pool', lambda e: e.tensor_tensor(out=cacc[:], in0=cacc[:], in1=ctmp, op=ALU.add), [r_cacc, r_ctmp], [r_cacc])
            S.op('pool', lambda e: e.tensor_tensor(out=cacc[:], in0=cacc[:], in1=cb[:].unsqueeze(2).to_broadcast([128, 8, 128]), op=ALU.add), [r_cacc, r_cb], [r_cacc])
            S.op('act', lambda e: e.activation(out=mqkT[:], in_=cacc[:], func=AF.Silu), [r_cacc], [r_mqkT])
            S.dma('sp', lambda e, jj=jj: e.dma_start(out=QT[jj, :, 4:8, :], in_=mqkT[:, 0:4, :]), [r_mqkT], [r_QT[jj]], 'st_mqk')
            S.dma('sp', lambda e, jj=jj: e.dma_start(out=KT[jj, :, 4:8, :], in_=mqkT[:, 4:8, :]), [r_mqkT], [r_KT[jj]], 'st_mqk')
            for h in range(4):
                S.op('pe', lambda e, h=h: e.transpose(out=psbf(5)[:, h * 128:(h + 1) * 128], in_=mqkT[:, 4 + h, :], identity=ident[:]), [r_mqkT, r_ident], [r_ps[5]])
            S.op('dve', lambda e: e.tensor_copy(out=kmm[:], in_=psbf(5)[:, 0:512].rearrange("p (h d) -> p h d", h=4)), [r_ps[5]], [r_kmm])
            S.dma('sp', lambda e, jj=jj: e.dma_start(out=KM[jj, :, 4:8, :], in_=kmm[:]), [r_kmm], [r_KM[jj]], 'st_kmm')

    S.barrier()
    A2 = Arena(nc, PBASE)
    Sst = A2.t([128, 8, 130], F32); r_Sst = Res("Sst")
    Sbf = [A2.t([128, 8, 130], BF16) for _ in range(2)]; r_Sbf = [Res("Sbf0"), Res("Sbf1")]
    kmt = [A2.t([128, 8, 128], BF16) for _ in range(2)]; r_kmt = [Res("kmt0"), Res("kmt1")]
    vpt = [A2.t([128, 8, 130], BF16) for _ in range(2)]; r_vpt = [Res("vpt0"), Res("vpt1")]
    kti = A2.t([128, 8, 128], BF16); r_kti = Res("kti")
    A2END = A2.off

    def state_update(kt_src, vp_src, r_k, r_v, cidx, gidx, j, ubanks):
        for h in range(8):
            eng = 'pool' if h % 2 else 'dve'
            S.op(eng, lambda e, h=h: e.tensor_scalar(out=kti[:, h, :], in0=kt_src[:, h, :], scalar1=SC[:, j, cidx, h:h + 1], scalar2=None, op0=ALU.mult),
                 [r_k, r_SC[j]], [r_kti])
        for h in range(8):
            bank = ubanks[h // 3]
            off = (h % 3) * 130
            S.op('pe', lambda e, h=h, bank=bank, off=off: e.matmul(psb[bank][:, off:off + 129], lhsT=kti[:, h, :], rhs=vp_src[:, h, 0:129], start=True, stop=True),
                 [r_kti, r_v], [r_ps[bank]])
        for h in range(8):
            bank = ubanks[h // 3]
            off = (h % 3) * 130
            S.op('dve', lambda e, h=h, bank=bank, off=off: e.tensor_tensor(out=Sst[:, h, 0:129], in0=psb[bank][:, off:off + 129], in1=Sst[:, h, 0:129], op=ALU.add),
                 [r_ps[bank], r_Sst], [r_Sst])
            S.op('act', lambda e, h=h: e.activation(out=Sst[:, h, 0:129], in_=Sst[:, h, 0:129], func=AF.Copy, scale=SC[:, j, gidx, h:h + 1]),
                 [r_Sst, r_SC[j]], [r_Sst])

    S.op('pool', lambda e: e.memset(Sst[:], 0.0), [], [r_Sst])
    for b in range(2):
        S.op('pool', lambda e, b=b: e.memset(Sbf[b][:], 0.0), [], [r_Sbf[b]])
    for j in range(NT - 1, -1, -1):
        b = j % 2
        S.dma('sp', lambda e, j=j, b=b: e.dma_start(out=kmt[b][:], in_=KM[j]), [r_KM[j]], [r_kmt[b]], f'l2km{b}')
        S.dma('sp', lambda e, j=j, b=b: e.dma_start(out=vpt[b][:], in_=VP[j]), [r_VP[j]], [r_vpt[b]], f'l2vp{b}')
        S.op('act', lambda e, b=b: e.copy(out=Sbf[b][:, :, 0:129], in_=Sst[:, :, 0:129]), [r_Sst], [r_Sbf[b]])
        S.dma('sp', lambda e, j=j, b=b: e.dma_start(out=SBS[j], in_=Sbf[b][:]), [r_Sbf[b]], [r_SBS[j]], f's2a{b}')
        if j > 0:
            state_update(kmt[b], vpt[b], r_kmt[b], r_vpt[b], 1, 5, j, (0, 1, 2))

    S.barrier()
    A3 = Arena(nc, A2END)
    Wo = A3.t([128, 8, D], BF16); r_Wo = Res("Wo")
    Wr = A3.t([128, 8, 16], BF16); r_Wr = Res("Wr")
    wr32 = A3.t([128, 8, 16], F32); r_wr32 = Res("wr32")
    gnb = A3.t([128, D], F32); r_gnb = Res("gnb")
    fgb = A3.t([128, D], F32); r_fgb = Res("fgb")
    qtt = [A3.t([128, 8, 128], BF16) for _ in range(2)]; r_qtt = [Res("qtt0"), Res("qtt1")]
    ktt = [A3.t([128, 8, 128], BF16) for _ in range(2)]; r_ktt = [Res("ktt0"), Res("ktt1")]
    sbt = [A3.t([128, 8, 130], BF16) for _ in range(2)]; r_sbt = [Res("sbt0"), Res("sbt1")]
    got = [A3.t([128, D], F32) for _ in range(2)]; r_got = [Res("got0"), Res("got1")]
    xt2 = [A3.t([128, D], F32) for _ in range(2)]; r_xt2 = [Res("xt20"), Res("xt21")]
    Pf = [A3.t([128, 128], BF16) for _ in range(2)]; r_Pf = [Res("Pf0"), Res("Pf1")]
    Pb = [A3.t([128, 128], BF16) for _ in range(2)]; r_Pb = [Res("Pb0"), Res("Pb1")]
    Sfb = A3.t([128, 8, 130], BF16); r_Sfb = Res("Sfb")
    Y = A3.t([128, D], F32); r_Y = Res("Y")
    dn = A3.t([128, 8, 4], F32); r_dn = Res("dn")
    bst = A3.t([128, 8, 6], F32); r_bst = Res("bst")
    mv = A3.t([128, 8, 2], F32); r_mv = Res("mv")
    ybf = A3.t([128, D], BF16); r_ybf = Res("ybf")
    yT = A3.t([128, 8, 128], BF16); r_yT = Res("yT")
    h1 = [A3.t([128, D], F32) for _ in range(2)]; r_h1 = [Res("h10"), Res("h11")]
    u2x = [A3.t([128, XW], BF16) for _ in range(2)]; r_u2x = [Res("u2x0"), Res("u2x1")]
    u2T = A3.t([128, 8, 128], BF16); r_u2T = Res("u2T")
    sm = A3.t([128, 4], F32); r_sm = Res("sm")
    junk2 = A3.t([128, D], BF16); r_junk2 = Res("junk2")
    ss2 = A3.t([128, 1], F32); r_ss2 = Res("ss2")
    rstd2 = A3.t([128, 1], F32); r_rstd2 = Res("rstd2")
    ex = A3.t([128, 16], F32); r_ex = Res("ex")
    r_U2X = [Res(f"U2X{j}") for j in range(NT)]
    r_OACC = Res("OACC")

    for c in range(8):
        b = c % 2
        S.dma('sp', lambda e, b=b, c=c: e.dma_start(out=xt2[b][:], in_=w_out[c * 128:(c + 1) * 128, :]), [], [r_xt2[b]], f'xt2{b}')
        S.op('dve' if c % 2 else 'pool', lambda e, b=b, c=c: e.tensor_copy(out=Wo[:, c, :], in_=xt2[b][:]), [r_xt2[b]], [r_Wo])
    S.dma('sp', lambda e: e.dma_start(out=wr32[:], in_=w_router.rearrange("(c p) n -> p c n", p=128)), [], [r_wr32], 'w3')
    S.op('dve', lambda e: e.tensor_copy(out=Wr[:], in_=wr32[:]), [r_wr32], [r_Wr])
    S.dma('sp', lambda e: e.dma_start(out=gnb[:], in_=gn_g.to_broadcast([128, D])), [], [r_gnb], 'w4a')
    S.dma('sp', lambda e: e.dma_start(out=fgb[:], in_=ffn_g.to_broadcast([128, D])), [], [r_fgb], 'w4b')
    S.op('pool', lambda e: e.memset(Sst[:], 0.0), [], [r_Sst])
    S.op('pool', lambda e: e.memset(Sfb[:], 0.0), [], [r_Sfb])
    for b in range(2):
        S.op('pool', lambda e, b=b: e.memset(u2x[b][:], 0.0), [], [r_u2x[b]])
    S.op('pool', lambda e: e.memset(xt2[0][:], 0.0), [r_Wo], [r_xt2[0]])

    for j in range(NT):
        b = j % 2
        S.dma('sp', lambda e, j=j, b=b: e.dma_start(out=qtt[b][:], in_=QT[j]), [r_QT[j]], [r_qtt[b]], f'l2q{b}')
        S.dma('sp', lambda e, j=j, b=b: e.dma_start(out=ktt[b][:], in_=KT[j]), [r_KT[j]], [r_ktt[b]], f'l2k{b}')
        S.dma('sp', lambda e, j=j, b=b: e.dma_start(out=kmt[b][:], in_=KM[j]), [r_KM[j]], [r_kmt[b]], f'l2km{b}')
        S.dma('sp', lambda e, j=j, b=b: e.dma_start(out=vpt[b][:], in_=VP[j]), [r_VP[j]], [r_vpt[b]], f'l2vp{b}')
        S.dma('sp', lambda e, j=j, b=b: e.dma_start(out=sbt[b][:], in_=SBS[j]), [r_SBS[j]], [r_sbt[b]], f'l2s{b}')
        S.dma('sp', lambda e, j=j, b=b: e.dma_start(out=got[b][:], in_=GO[j]), [r_GO[j]], [r_got[b]], f'l2g{b}')
        if j == 0:
            S.dma('sp', lambda e: e.dma_start(out=xt2[0][PADF:128, :], in_=meta[:, :]), [], [r_xt2[0]], 'xt20')
        else:
            S.dma('sp', lambda e, j=j, b=b: e.dma_start(out=xt2[b][:], in_=x_rows(j)), [], [r_xt2[b]], f'xt2{b}')
        for h in range(8):
            hb = h % 2
            sbank = 3 + hb
            S.op('pe', lambda e, h=h, sbank=sbank, b=b: e.matmul(psb[sbank][:, 0:128], lhsT=ktt[b][:, h, :], rhs=qtt[b][:, h, :], start=True, stop=True),
                 [r_ktt[b], r_qtt[b]], [r_ps[sbank]])
            S.op('dve', lambda e, h=h, sbank=sbank, hb=hb, j=j: e.scalar_tensor_tensor(out=Pf[hb][:], in0=psb[sbank][:, 0:128], scalar=SC[:, j, 0, h:h + 1], in1=maskfb[:, 0, :],
                                                                                 op0=ALU.mult, op1=ALU.mult),
                 [r_ps[sbank], r_SC[j], r_mask], [r_Pf[hb]])
            S.op('dve', lambda e, h=h, sbank=sbank, hb=hb, j=j: e.scalar_tensor_tensor(out=Pb[hb][:], in0=psb[sbank][:, 0:128], scalar=SC[:, j, 1, h:h + 1], in1=maskfb[:, 1, :],
                                                                                 op0=ALU.mult, op1=ALU.mult),
                 [r_ps[sbank], r_SC[j], r_mask], [r_Pb[hb]])
            obank = 5 + hb
            S.op('pe', lambda e, h=h, obank=obank, hb=hb, b=b: e.matmul(psb[obank][:, 0:129], lhsT=Pf[hb][:], rhs=vpt[b][:, h, 0:129], start=True, stop=False),
                 [r_Pf[hb], r_vpt[b]], [r_ps[obank]])
            S.op('pe', lambda e, h=h, obank=obank, b=b: e.matmul(psb[obank][:, 0:129], lhsT=qtt[b][:, h, :], rhs=Sfb[:, h, 0:129], start=False, stop=True),
                 [r_qtt[b], r_Sfb], [r_ps[obank]])
            S.op('pe', lambda e, h=h, obank=obank, hb=hb, b=b: e.matmul(psb[obank][:, 130:259], lhsT=Pb[hb][:], rhs=vpt[b][:, h, 0:129], start=True, stop=False),
                 [r_Pb[hb], r_vpt[b]], [r_ps[obank]])
            S.op('pe', lambda e, h=h, obank=obank, b=b: e.matmul(psb[obank][:, 130:259], lhsT=qtt[b][:, h, :], rhs=sbt[b][:, h, 0:129], start=False, stop=True),
                 [r_qtt[b], r_sbt[b]], [r_ps[obank]])
            if h >= 4:
                S.op('dve', lambda e, h=h, obank=obank, j=j: e.tensor_tensor(out=dn[:, h, 0:2], in0=psb[obank][:, 128:259:130], in1=SC[:, j, 2:4, h], op=ALU.mult),
                     [r_ps[obank], r_SC[j]], [r_dn])
                S.op('dve', lambda e, h=h: e.scalar_tensor_tensor(out=dn[:, h, 2:4], in0=dn[:, h, 0:2], scalar=-1.0, in1=dn[:, h, 0:2], op0=ALU.mult, op1=ALU.max), [r_dn], [r_dn])
                S.op('dve', lambda e, h=h: e.tensor_scalar(out=dn[:, h, 0:2], in0=dn[:, h, 2:4], scalar1=1.0, scalar2=None, op0=ALU.max), [r_dn], [r_dn])
                S.op('dve', lambda e, h=h: e.reciprocal(out=dn[:, h, 0:2], in_=dn[:, h, 0:2]), [r_dn], [r_dn])
                S.op('dve', lambda e, h=h, j=j: e.tensor_tensor(out=dn[:, h, 2:4], in0=dn[:, h, 0:2], in1=SC[:, j, 2:4, h], op=ALU.mult), [r_dn, r_SC[j]], [r_dn])
            else:
                S.op('dve', lambda e, h=h, j=j: e.tensor_copy(out=dn[:, h, 2:4], in_=SC[:, j, 2:4, h]), [r_SC[j]], [r_dn])
            S.op('act', lambda e, h=h, obank=obank: e.activation(out=Y[:, h * 128:(h + 1) * 128], in_=psb[obank][:, 0:128], func=AF.Copy, scale=dn[:, h, 2:3]),
                 [r_ps[obank], r_dn], [r_Y])
            S.op('dve', lambda e, h=h, obank=obank: e.scalar_tensor_tensor(out=Y[:, h * 128:(h + 1) * 128], in0=psb[obank][:, 130:258], scalar=dn[:, h, 3:4],
                                                                        in1=Y[:, h * 128:(h + 1) * 128], op0=ALU.mult, op1=ALU.add),
                 [r_ps[obank], r_dn, r_Y], [r_Y])
        state_update(kmt[b], vpt[b], r_kmt[b], r_vpt[b], 0, 4, j, (0, 1, 2))
        S.op('act', lambda e: e.copy(out=Sfb[:, :, 0:129], in_=Sst[:, :, 0:129]), [r_Sst], [r_Sfb])
        S.op('pool', lambda e, b=b: e.tensor_tensor(out=Y[:, 512:1024], in0=Y[:, 512:1024], in1=got[b][:, 512:1024], op=ALU.mult), [r_Y, r_got[b]], [r_Y])
        for h in range(8):
            S.op('dve', lambda e, h=h: e.bn_stats(out=bst[:, h, :], in_=Y[:, h * 128:(h + 1) * 128]), [r_Y], [r_bst])
            S.op('dve', lambda e, h=h: e.bn_aggr(out=mv[:, h, :], in_=bst[:, h, :]), [r_bst], [r_mv])
        S.op('act', lambda e: e.activation(out=mv[:, :, 1], in_=mv[:, :, 1], func=AF.Sqrt, bias=EPS), [r_mv], [r_mv])
        S.op('dve', lambda e: e.reciprocal(out=mv[:, :, 1], in_=mv[:, :, 1]), [r_mv], [r_mv])
        for h in range(8):
            S.op('dve' if h % 2 else 'pool', lambda e, h=h: e.tensor_scalar(out=Y[:, h * 128:(h + 1) * 128], in0=Y[:, h * 128:(h + 1) * 128], scalar1=mv[:, h, 0:1], scalar2=mv[:, h, 1:2],
                                                                            op0=ALU.subtract, op1=ALU.mult), [r_Y, r_mv], [r_Y])
        S.op('pool', lambda e: e.tensor_tensor(out=Y[:], in0=Y[:], in1=gnb[:], op=ALU.mult), [r_Y, r_gnb], [r_Y])
        S.op('pool', lambda e, b=b: e.tensor_tensor(out=Y[:, 0:512], in0=Y[:, 0:512], in1=got[b][:, 0:512], op=ALU.mult), [r_Y, r_got[b]], [r_Y])
        S.op('act', lambda e: e.copy(out=ybf[:], in_=Y[:]), [r_Y], [r_ybf])
        for c in range(8):
            S.op('pe', lambda e, c=c: e.transpose(out=psbf(7)[:, c * 128:(c + 1) * 128], in_=ybf[:, c * 128:(c + 1) * 128], identity=ident[:]), [r_ybf, r_ident], [r_ps[7]])
        S.op('act', lambda e: e.copy(out=yT[:], in_=psbf(7).rearrange("p (c t) -> p c t", c=8)), [r_ps[7]], [r_yT])
        for half in range(2):
            bank = 3 + half
            for c in range(8):
                S.op('pe', lambda e, c=c, half=half, bank=bank: e.matmul(psb[bank][:], lhsT=yT[:, c, :], rhs=Wo[:, c, half * 512:(half + 1) * 512], start=(c == 0), stop=(c == 7)),
                     [r_yT, r_Wo], [r_ps[bank]])
            S.op('dve', lambda e, half=half, bank=bank, b=b: e.tensor_tensor(out=h1[b][:, half * 512:(half + 1) * 512], in0=psb[bank][:], in1=xt2[b][:, half * 512:(half + 1) * 512], op=ALU.add),
                 [r_ps[bank], r_xt2[b]], [r_h1[b]])
        S.dma('sp', lambda e, j=j, b=b: e.dma_start(out=OACC[j * 128:(j + 1) * 128, :], in_=h1[b][:]), [r_h1[b]], [r_OACC], f'st_h1{b}')
        S.op('act', lambda e, b=b: e.activation(out=junk2[:], in_=h1[b][:], func=AF.Square, accum_out=ss2[:]), [r_h1[b]], [r_junk2, r_ss2])
        S.op('act', lambda e: e.activation(out=rstd2[:], in_=ss2[:], func=AF.Sqrt, scale=1.0 / D, bias=EPS), [r_ss2], [r_rstd2])
        S.op('dve', lambda e: e.reciprocal(out=rstd2[:], in_=rstd2[:]), [r_rstd2], [r_rstd2])
        S.op('dve', lambda e, b=b: e.scalar_tensor_tensor(out=u2x[b][:, 0:D], in0=h1[b][:], scalar=rstd2[:], in1=fgb[:], op0=ALU.mult, op1=ALU.mult),
             [r_h1[b], r_rstd2, r_fgb], [r_u2x[b]])
        for c in range(8):
            S.op('pe', lambda e, c=c, b=b: e.transpose(out=psbf(7)[:, c * 128:(c + 1) * 128], in_=u2x[b][:, c * 128:(c + 1) * 128], identity=ident[:]), [r_u2x[b], r_ident], [r_ps[7]])
        S.op('act', lambda e: e.copy(out=u2T[:], in_=psbf(7).rearrange("p (c t) -> p c t", c=8)), [r_ps[7]], [r_u2T])
        for c in range(8):
            S.op('pe', lambda e, c=c: e.matmul(psb[6][:, 0:16], lhsT=u2T[:, c, :], rhs=Wr[:, c, :], start=(c == 0), stop=(c == 7)), [r_u2T, r_Wr], [r_ps[6]])
        S.op('dve', lambda e: e.tensor_reduce(out=sm[:, 0:1], in_=psb[6][:, 0:16], axis=AX.X, op=ALU.max, negate=True), [r_ps[6]], [r_sm])
        S.op('act', lambda e: e.activation(out=ex[:], in_=psb[6][:, 0:16], func=AF.Exp, bias=sm[:, 0:1], accum_out=sm[:, 1:2]), [r_ps[6], r_sm], [r_ex, r_sm])
        S.op('dve', lambda e: e.reciprocal(out=sm[:, 2:3], in_=sm[:, 1:2]), [r_sm], [r_sm])
        if j == 0:
            S.op('dve', lambda e: e.tensor_tensor(out=sm[:, 2:3], in0=sm[:, 2:3], in1=padm[:], op=ALU.mult), [r_sm, r_padm], [r_sm])
        S.op('dve', lambda e, j=j: e.tensor_scalar(out=AFF[:, j, :], in0=ex[:], scalar1=sm[:, 2:3], scalar2=None, op0=ALU.mult), [r_ex, r_sm], [r_AFF[j]])
        S.op('pool', lambda e, j=j, b=b: e.tensor_copy(out=u2x[b][:, D:D + 32].bitcast(F32), in_=AFF[:, j, :]), [r_AFF[j]], [r_u2x[b]])
        S.op('pool', lambda e, j=j, b=b: e.tensor_copy(out=u2x[b][:, D + 32:D + 34].bitcast(I32), in_=tok[:, j:j + 1]), [r_tok], [r_u2x[b]])
        S.dma('sp', lambda e, j=j, b=b: e.dma_start(out=U2X[j * 128:(j + 1) * 128, :], in_=u2x[b][:]), [r_u2x[b]], [r_U2X[j]], f'st_u2{b}')

    S.barrier()
    A4 = Arena(nc, PBASE)
    CMP = A4.t([128, NT, 16], F32); r_CMP = Res("CMP")
    SELb = A4.t([128, NT * 16], BF16); r_SELb = Res("SELb")
    lo = A4.t([128, 16], F32); r_lo = Res("lo")
    hi = A4.t([128, 16], F32); r_hi = Res("hi")
    mid = A4.t([128, 16], F32); r_mid = Res("mid")
    pc = A4.t([128, 16], F32); r_pc = Res("pc")
    mm = A4.t([128, 16], F32); r_mm = Res("mm")
    nm = A4.t([128, 16], F32); r_nm = Res("nm")
    t1 = A4.t([128, 16], F32); r_t1 = Res("t1")
    ones32 = A4.t([128, 128], F32); r_ones32 = Res("ones32")
    onesb = A4.t([128, 128], BF16); r_onesb = Res("onesb")
    sltb = A4.t([128, 128], BF16); r_sltb = Res("sltb")
    onesNT = A4.t([128, NT], F32); r_onesNT = Res("onesNT")
    WIT = A4.t([128, NT, 16], F32); r_WIT = Res("WIT")
    TOT = A4.t([128, 16, NT], F32); r_TOT = Res("TOT")
    INC = A4.t([128, 16, NT], F32); r_INC = Res("INC")
    POSf = A4.t([128, NT, 16], F32); r_POSf = Res("POSf")
    POSi = A4.t([128, NT, 16], I32); r_POSi = Res("POSi")
    A4END = A4.off
    NI = NT * 16
    all_AFF = r_AFF

    S.op('pool', lambda e: e.memset(ones32[:], 1.0), [], [r_ones32])
    S.op('pool', lambda e: e.memset(onesb[:], 1.0), [], [r_onesb])
    S.op('pool', lambda e: e.memset(onesNT[:], 1.0), [], [r_onesNT])
    S.op('pool', lambda e: e.memset(lo[:], 0.0), [], [r_lo])
    S.op('pool', lambda e: e.memset(hi[:], 1.0), [], [r_hi])
    S.dma('sp', lambda e: e.dma_start(out=sltb[:], in_=c_slt[:, :]), [], [r_sltb], 'c5')

    def thr_cmp(th, r_th):
        S.op('dve', lambda e: e.tensor_tensor(out=CMP[:], in0=AFF[:], in1=th[:].unsqueeze(1).to_broadcast([128, NT, 16]), op=ALU.is_ge),
             all_AFF + [r_th], [r_CMP])

    NITER = 34
    for it in range(NITER):
        S.op('dve', lambda e: e.tensor_tensor(out=mid[:], in0=lo[:], in1=hi[:], op=ALU.add), [r_lo, r_hi], [r_mid])
        S.op('dve', lambda e: e.tensor_scalar(out=mid[:], in0=mid[:], scalar1=0.5, scalar2=None, op0=ALU.mult), [r_mid], [r_mid])
        thr_cmp(mid, r_mid)
        S.op('dve', lambda e: e.tensor_reduce(out=pc[:], in_=CMP[:].rearrange("p j e -> p e j"), axis=AX.X, op=ALU.add), [r_CMP], [r_pc])
        S.op('pe', lambda e: e.matmul(psb[0][:, 0:16], lhsT=ones32[:], rhs=pc[:], start=True, stop=True), [r_ones32, r_pc], [r_ps[0]])
        S.op('dve', lambda e: e.tensor_scalar(out=mm[:], in0=psb[0][:, 0:16], scalar1=float(CAP) - 0.5, scalar2=None, op0=ALU.is_ge), [r_ps[0]], [r_mm])
        S.op('dve', lambda e: e.tensor_scalar(out=nm[:], in0=mm[:], scalar1=-1.0, scalar2=1.0, op0=ALU.mult, op1=ALU.add), [r_mm], [r_nm])
        S.op('dve', lambda e: e.tensor_tensor(out=t1[:], in0=mm[:], in1=mid[:], op=ALU.mult), [r_mm, r_mid], [r_t1])
        S.op('dve', lambda e: e.tensor_tensor(out=lo[:], in0=nm[:], in1=lo[:], op=ALU.mult), [r_nm, r_lo], [r_lo])
        S.op('dve', lambda e: e.tensor_tensor(out=lo[:], in0=lo[:], in1=t1[:], op=ALU.add), [r_lo, r_t1], [r_lo])
        S.op('dve', lambda e: e.tensor_tensor(out=t1[:], in0=nm[:], in1=mid[:], op=ALU.mult), [r_nm, r_mid], [r_t1])
        S.op('dve', lambda e: e.tensor_tensor(out=hi[:], in0=mm[:], in1=hi[:], op=ALU.mult), [r_mm, r_hi], [r_hi])
        S.op('dve', lambda e: e.tensor_tensor(out=hi[:], in0=hi[:], in1=t1[:], op=ALU.add), [r_hi, r_t1], [r_hi])
    thr_cmp(lo, r_lo)
    S.op('dve', lambda e: e.tensor_copy(out=SELb[:], in_=CMP[:].rearrange("p j e -> p (j e)")), [r_CMP], [r_SELb])
    nchunk = (NI + 511) // 512
    for ci in range(nchunk):
        n0 = ci * 512
        n1 = min(NI, n0 + 512)
        bank = ci % 3
        S.op('pe', lambda e, n0=n0, n1=n1, bank=bank: e.matmul(psb[bank][:, 0:n1 - n0], lhsT=sltb[:], rhs=SELb[:, n0:n1], start=True, stop=True), [r_sltb, r_SELb], [r_ps[bank]])
        S.op('act', lambda e, n0=n0, n1=n1, bank=bank: e.copy(out=WIT[:].rearrange("p j e -> p (j e)")[:, n0:n1], in_=psb[bank][:, 0:n1 - n0]), [r_ps[bank]], [r_WIT])
        S.op('pe', lambda e, n0=n0, n1=n1, bank=bank: e.matmul(psb[3 + bank][:, 0:n1 - n0], lhsT=onesb[:], rhs=SELb[:, n0:n1], start=True, stop=True), [r_onesb, r_SELb], [r_ps[3 + bank]])
        j0 = n0 // 16
        j1 = n1 // 16
        S.op('act', lambda e, n0=n0, n1=n1, bank=bank, j0=j0, j1=j1: e.copy(out=TOT[:, :, j0:j1], in_=psb[3 + bank][:, 0:n1 - n0].rearrange("p (j e) -> p e j", e=16)),
             [r_ps[3 + bank]], [r_TOT])
    for ee in range(16):
        S.op('dve', lambda e, ee=ee: e.tensor_tensor_scan(out=INC[:, ee, :], data0=onesNT[:], data1=TOT[:, ee, :], initial=0.0, op0=ALU.mult, op1=ALU.add),
             [r_onesNT, r_TOT], [r_INC])
    S.op('dve', lambda e: e.tensor_tensor(out=INC[:], in0=INC[:], in1=TOT[:], op=ALU.subtract), [r_INC, r_TOT], [r_INC])
    S.op('dve', lambda e: e.tensor_tensor(out=POSf[:], in0=WIT[:], in1=INC[:].rearrange("p e j -> p j e"), op=ALU.add), [r_WIT, r_INC], [r_POSf])
    BIG = float(1 << 20)
    S.op('dve', lambda e: e.tensor_scalar(out=POSf[:], in0=POSf[:], scalar1=-BIG, scalar2=None, op0=ALU.add), [r_POSf], [r_POSf])
    S.op('dve', lambda e: e.tensor_tensor(out=POSf[:], in0=POSf[:], in1=CMP[:], op=ALU.mult), [r_POSf, r_CMP], [r_POSf])
    S.op('dve', lambda e: e.tensor_scalar(out=POSf[:], in0=POSf[:], scalar1=BIG, scalar2=None, op0=ALU.add), [r_POSf], [r_POSf])
    S.op('dve', lambda e: e.tensor_copy(out=POSi[:], in_=POSf[:]), [r_POSf], [r_POSi])

    A5 = Arena(nc, A4END)
    u2l = [A5.t([128, XW], BF16) for _ in range(3)]; r_u2l = [Res(f"u2l{i}") for i in range(3)]
    r_XS = [Res(f"XS{e_}") for e_ in range(NEXP)]
    A5END = A5.off
    for e_ in range(NEXP_RUN):
        S.dma('sp', lambda e, e_=e_: e.dma_start(out=XS[e_][:, :], in_=c_xsinit[:, :]), [], [r_XS[e_]], f'xsi{e_}')
    for j in range(NT):
        b = j % 3
        S.dma('sp', lambda e, j=j, b=b: e.dma_start(out=u2l[b][:], in_=U2X[j * 128:(j + 1) * 128, :]), [r_U2X[j]], [r_u2l[b]], f'u2l{b}')
        for e_ in range(NEXP_RUN):
            S.dma('pool', lambda e, j=j, b=b, e_=e_: e.indirect_dma_start(out=XS[e_], out_offset=bass.IndirectOffsetOnAxis(ap=POSi[:, j, e_:e_ + 1], axis=0),
                                                                         in_=u2l[b][:], in_offset=None, bounds_check=breg(e, NSLR - 1), oob_is_err=False),
                  [r_u2l[b], r_POSi, r_XS[e_]], [], f'sc{b}')

    S.barrier()
    A6 = Arena(nc, PBASE0)
    stg = [A6.t([128, 2048], F32) for _ in range(3)]; r_stg = [Res(f"stg{i}") for i in range(3)]
    wgb = [A6.t([128, 8, 256], BF16) for _ in range(2)]; r_wgb = [Res("wgb0"), Res("wgb1")]
    wub = [A6.t([128, 8, 256], BF16) for _ in range(2)]; r_wub = [Res("wub0"), Res("wub1")]
    wdb = A6.t([128, NFC, D], BF16); r_wdb = Res("wdb")
    hidT = A6.t([128, NFC, NSLR], BF16); r_hidT = Res("hidT")
    xsT = A6.t([128, 8, NSLR], BF16); r_xsT = Res("xsT")
    xsl = [A6.t([128, XW], BF16) for _ in range(2)]; r_xsl = [Res("xsl0"), Res("xsl1")]
    meta_sl = A6.t([128, NSL, 64], BF16); r_meta_sl = Res("meta_sl")
    sg = A6.t([128, 512], F32); r_sg = Res("sg")
    yo = [A6.t([128, D], F32) for _ in range(2)]; r_yo = [Res("yo0"), Res("yo1")]
    nstg = 0
    CB = 384 if NSLR % 384 == 0 else 128
    NCB = NSLR // CB
    r_oacc_e = Res("OACCe")
    for e_ in range(NEXP_RUN):
        for s_ in range(NSL):
            b = s_ % 2
            S.dma('sp', lambda e, e_=e_, s_=s_, b=b: e.dma_start(out=xsl[b][:], in_=XS[e_][s_ * 128:(s_ + 1) * 128, :]), [r_XS[e_]], [r_xsl[b]], f'xsl{b}')
            S.op('pool', lambda e, s_=s_, b=b: e.tensor_copy(out=meta_sl[:, s_, :], in_=xsl[b][:, D:D + 64]), [r_xsl[b]], [r_meta_sl])
            for c in range(8):
                S.op('pe', lambda e, c=c, b=b: e.transpose(out=psbf(7)[:, c * 128:(c + 1) * 128], in_=xsl[b][:, c * 128:(c + 1) * 128], identity=ident[:]), [r_xsl[b], r_ident], [r_ps[7]])
            S.op('act', lambda e, s_=s_: e.copy(out=xsT[:, :, s_ * 128:(s_ + 1) * 128], in_=psbf(7).rearrange("p (c t) -> p c t", c=8)), [r_ps[7]], [r_xsT])
        for f2 in range(NFC // 2):
            sb_ = nstg % 3; nstg += 1
            S.dma('sp', lambda e, e_=e_, f2=f2, sb_=sb_: e.dma_start(out=stg[sb_][:].rearrange("p (a n) -> p a n", a=2),
                                                                   in_=w_down[e_, f2 * 256:(f2 + 1) * 256, :].rearrange("(a p) n -> p a n", p=128)), [], [r_stg[sb_]], f'stg{sb_}')
            S.op('pool', lambda e, f2=f2, sb_=sb_: e.tensor_copy(out=wdb[:, 2 * f2:2 * f2 + 2, :], in_=stg[sb_][:].rearrange("p (a n) -> p a n", a=2)), [r_stg[sb_]], [r_wdb])
        for fb in range(NFC // 2):
            wb_ = fb % 2
            for (wsrc, wdst, r_wd) in ((w_gate, wgb[wb_], r_wgb[wb_]), (w_up, wub[wb_], r_wub[wb_])):
                sb_ = nstg % 3; nstg += 1
                S.dma('sp', lambda e, e_=e_, fb=fb, sb_=sb_, wsrc=wsrc: e.dma_start(out=stg[sb_][:].rearrange("p (c n) -> p c n", c=8),
                                                                                  in_=wsrc[e_, :, fb * 256:(fb + 1) * 256].rearrange("(c p) n -> p c n", p=128)), [], [r_stg[sb_]], f'stg{sb_}')
                S.op('dve' if wsrc is w_gate else 'act',
                     (lambda e, sb_=sb_, wdst=wdst: e.tensor_copy(out=wdst[:], in_=stg[sb_][:].rearrange("p (c n) -> p c n", c=8))) if wsrc is w_gate else
                     (lambda e, sb_=sb_, wdst=wdst: e.copy(out=wdst[:], in_=stg[sb_][:].rearrange("p (c n) -> p c n", c=8))),
                     [r_stg[sb_]], [r_wd])
            for fi in range(2):
                fc = fb * 2 + fi
                for cbk in range(NCB):
                    pp = (fc * NCB + cbk) % 3
                    gb, ub = pp * 2, pp * 2 + 1
                    for c in range(8):
                        S.op('pe', lambda e, c=c, gb=gb, wb_=wb_, fi=fi, cbk=cbk: e.matmul(psb[gb][:, 0:CB], lhsT=wgb[wb_][:, c, fi * 128:(fi + 1) * 128],
                                                                                     rhs=xsT[:, c, cbk * CB:(cbk + 1) * CB], start=(c == 0), stop=(c == 7)),
                             [r_wgb[wb_], r_xsT], [r_ps[gb]])
                    for c in range(8):
                        S.op('pe', lambda e, c=c, ub=ub, wb_=wb_, fi=fi, cbk=cbk: e.matmul(psb[ub][:, 0:CB], lhsT=wub[wb_][:, c, fi * 128:(fi + 1) * 128],
                                                                                     rhs=xsT[:, c, cbk * CB:(cbk + 1) * CB], start=(c == 0), stop=(c == 7)),
                             [r_wub[wb_], r_xsT], [r_ps[ub]])
                    S.op('act', lambda e, gb=gb: e.activation(out=sg[:, 0:CB], in_=psb[gb][:, 0:CB], func=AF.Silu), [r_ps[gb]], [r_sg])
                    S.op('dve', lambda e, ub=ub, fc=fc, cbk=cbk: e.tensor_tensor(out=hidT[:, fc, cbk * CB:(cbk + 1) * CB], in0=psb[ub][:, 0:CB], in1=sg[:, 0:CB], op=ALU.mult),
                         [r_ps[ub], r_sg], [r_hidT])
        for s_ in range(NSL):
            yb = s_ % 2
            for half in range(2):
                bank = 6 + half
                for fc in range(NFC):
                    S.op('pe', lambda e, fc=fc, s_=s_, half=half, bank=bank: e.matmul(psb[bank][:], lhsT=hidT[:, fc, s_ * 128:(s_ + 1) * 128], rhs=wdb[:, fc, half * 512:(half + 1) * 512],
                                                                                start=(fc == 0), stop=(fc == NFC - 1)),
                         [r_hidT, r_wdb], [r_ps[bank]])
                gsc = meta_sl[:, s_, 2 * e_:2 * e_ + 2].bitcast(F32)
                S.op('act' if half else 'dve',
                     (lambda e, half=half, bank=bank, yb=yb, gsc=gsc: e.activation(out=yo[yb][:, half * 512:(half + 1) * 512], in_=psb[bank][:], func=AF.Copy, scale=gsc)) if half else
                     (lambda e, half=half, bank=bank, yb=yb, gsc=gsc: e.tensor_scalar(out=yo[yb][:, half * 512:(half + 1) * 512], in0=psb[bank][:], scalar1=gsc, scalar2=None, op0=ALU.mult)),
                     [r_ps[bank], r_meta_sl], [r_yo[yb]])
            tix = meta_sl[:, s_, 32:34].bitcast(I32)
            S.dma('pool', lambda e, yb=yb, tix=tix: e.indirect_dma_start(out=OACC, out_offset=bass.IndirectOffsetOnAxis(ap=tix, axis=0), in_=yo[yb][:], in_offset=None,
                                                                       bounds_check=breg(e, T + NSLR - 1), oob_is_err=True, compute_op=ALU.add),
                  [r_yo[yb], r_meta_sl, r_OACC, r_oacc_e], [r_oacc_e], f'oa{yb}')

    S.barrier()
    A7 = Arena(nc, PBASE0)
    ft = [A7.t([128, D], F32) for _ in range(2)]; r_ft = [Res("ft0"), Res("ft1")]
    fo = [A7.t([128, D], F32) for _ in range(2)]; r_fo = [Res("fo0"), Res("fo1")]
    fj = A7.t([128, D], BF16); r_fj = Res("fj")
    fss = A7.t([128, 2], F32); r_fss = Res("fss")
    fgn = A7.t([128, D], F32); r_fgn = Res("fgn")
    S.dma('sp', lambda e: e.dma_start(out=fgn[:], in_=fin_g.to_broadcast([128, D])), [], [r_fgn], 'w5')
    outs = []
    for j in range(1, NT):
        b = j % 2
        S.dma('sp', lambda e, j=j, b=b: e.dma_start(out=ft[b][:], in_=OACC[j * 128:(j + 1) * 128, :]), [r_oacc_e, r_OACC], [r_ft[b]], f'ft{b}')
        S.op('act', lambda e, b=b: e.activation(out=fj[:], in_=ft[b][:], func=AF.Square, accum_out=fss[:, 0:1]), [r_ft[b]], [r_fj, r_fss])
        S.op('act', lambda e: e.activation(out=fss[:, 1:2], in_=fss[:, 0:1], func=AF.Sqrt, scale=1.0 / D, bias=EPS), [r_fss], [r_fss])
        S.op('dve', lambda e: e.reciprocal(out=fss[:, 1:2], in_=fss[:, 1:2]), [r_fss], [r_fss])
        S.op('dve', lambda e, b=b: e.scalar_tensor_tensor(out=fo[b][:], in0=ft[b][:], scalar=fss[:, 1:2], in1=fgn[:], op0=ALU.mult, op1=ALU.mult),
             [r_ft[b], r_fss, r_fgn], [r_fo[b]])
        outs.append(S.dma('sp', lambda e, j=j, b=b: e.dma_start(out=out[(j - 1) * 128:j * 128, :], in_=fo[b][:]), [r_fo[b]], [Res("o")], f'out{b}'))
    S.final_wait('sp', outs[-2:])
    S.emit(st)
    st.close()
    return nc, dict(T=T, NSLR=NSLR, CAP=CAP, NSL=NSL)


def host_consts(NT, NSLR):
    T = NT * 128
    bf = ml_dtypes.bfloat16
    m = np.arange(128)[:, None]
    l = np.arange(128)[None, :]
    tri_f = (m <= l).astype(np.float32)
    tri_b = (m >= l).astype(np.float32)
    ones = np.ones((128, 128), np.float32)
    rowm = (np.arange(128) >= PADF).astype(np.float32)[:, None]
    c_tri = np.stack([tri_f, tri_b, ones, tri_f * rowm, tri_b * rowm, ones * rowm]).astype(np.float32)
    c_mask = np.stack([tri_f, tri_b]).astype(np.float32)
    c_slt = (m < l).astype(bf)
    pos = (np.arange(T, dtype=np.float32) - np.float32(PADF)).astype(np.float32)
    half = 64
    inv = (np.float32(10000.0) ** (-np.arange(half, dtype=np.float32) / np.float32(half))).astype(np.float32)
    ang = (pos[:, None] * inv[None, :]).astype(np.float32)
    cos = np.cos(ang.astype(np.float64)).astype(np.float32)
    sin = np.sin(ang.astype(np.float64)).astype(np.float32)
    c_cs = np.concatenate([np.tile(cos, (1, 4)), np.tile(sin, (1, 4))], axis=1).astype(np.float32)
    c_tok = (np.arange(128)[:, None] + 128 * np.arange(NT)[None, :]).astype(np.int32)
    c_padm = rowm.astype(np.float32)
    xs = np.zeros((NSLR, XW), dtype=bf)
    tokid = (T + np.arange(NSLR)).astype(np.int32)
    xs_i32 = xs.view(np.int32).reshape(NSLR, XW // 2)
    xs_i32[:, (D + 32) // 2] = tokid
    return dict(c_ident=np.eye(128).astype(bf), c_tri=c_tri, c_mask=c_mask, c_slt=c_slt, c_cs=c_cs,
                c_tok=c_tok, c_padm=c_padm, c_xsinit=xs)


def core_inputs(b, x, meta_tokens, mix_norm_g, w_in, b_gates, conv_w, conv_b, ret_decay_logit, ret_gn_g,
                mlstm_gn_g, w_out, ffn_norm_g, w_router, w_gate, w_up, w_down, final_norm_g, consts):
    f = np.float32
    d = dict(
        x=np.ascontiguousarray(x[b], dtype=f), meta=np.ascontiguousarray(meta_tokens, dtype=f),
        w_in=np.ascontiguousarray(w_in[0], dtype=f), mix_g=np.ascontiguousarray(mix_norm_g[0][None, :], dtype=f),
        b_gates=np.ascontiguousarray(b_gates[0][None, :], dtype=f),
        conv_w=np.ascontiguousarray(conv_w[0], dtype=f), conv_b=np.ascontiguousarray(conv_b[0][None, :], dtype=f),
        rlogit=np.ascontiguousarray(ret_decay_logit[0].reshape(1, 8), dtype=f),
        gn_g=np.ascontiguousarray(np.concatenate([ret_gn_g[0], mlstm_gn_g[0]])[None, :], dtype=f),
        w_out=np.ascontiguousarray(w_out[0], dtype=f), ffn_g=np.ascontiguousarray(ffn_norm_g[0][None, :], dtype=f),
        w_router=np.ascontiguousarray(w_router[0], dtype=f),
        w_gate=np.ascontiguousarray(w_gate[0], dtype=f), w_up=np.ascontiguousarray(w_up[0], dtype=f),
        w_down=np.ascontiguousarray(w_down[0], dtype=f), fin_g=np.ascontiguousarray(final_norm_g[None, :], dtype=f))
    d.update(consts)
    return d


_CACHE = {}


def kernel(**inputs):
    x = np.asarray(inputs['x'])
    B, SEQ, _ = x.shape
    NT = SEQ // 128 + 1
    if NT not in _CACHE:
        _CACHE[NT] = build(NT)
    nc, info = _CACHE[NT]
    consts = host_consts(NT, info['NSLR'])
    args = {k: np.asarray(v) for k, v in inputs.items()}
    in_maps = []
    for c in range(8):
        b = (c // 2) % B
        in_maps.append(core_inputs(b, consts=consts, **args))
    res = run_bass_kernel_spmd(nc, in_maps, core_ids=list(range(8)))
    outs = [np.asarray(res.results[2 * b]["out"]) for b in range(B)]
    return np.stack(outs, axis=0).astype(np.float32)
```

```python
import math
import numpy as np
import ml_dtypes
from contextlib import ExitStack
import concourse.bass as bass
import concourse.mybir as mybir
from concourse.bass_utils import run_bass_kernel_spmd

F32 = mybir.dt.float32
BF16 = mybir.dt.bfloat16
I32 = mybir.dt.int32
ALU = mybir.AluOpType
AF = mybir.ActivationFunctionType
AX = mybir.AxisListType

ENGS = ['pe', 'act', 'dve', 'pool', 'sp']
EPOCH = 12000


class Res:
    __slots__ = ('name', 'w', 'rs')

    def __init__(self, name):
        self.name = name
        self.w = None
        self.rs = []


class Ins:
    __slots__ = ('eng', 'emit', 'deps', 'pos', 'needed', 'is_dma', 'semkey', 'dk', 'dv',
                 'waits', 'cbase', 'ord')


class Sched:
    def __init__(self, nc):
        self.nc = nc
        self.streams = {e: [] for e in ENGS}
        self.all = []
        self.finals = []
        self.pending = {e: [] for e in ENGS}

    def op(self, eng, emit, reads=(), writes=(), after=()):
        return self._add(eng, emit, reads, writes, False, None, after)

    def dma(self, queue, emit, reads=(), writes=(), semkey=None, after=()):
        return self._add(queue, emit, reads, writes, True, semkey, after)

    def final_wait(self, eng, ins_list):
        self.finals.append((eng, list(ins_list)))

    def barrier(self):
        lasts = []
        for e in ENGS:
            st = self.streams[e]
            for ins in reversed(st):
                if not ins.is_dma:
                    lasts.append(ins)
                    break
        dl = {}
        for i in self.all[getattr(self, '_bar_idx', 0):]:
            if i.is_dma:
                dl[i.semkey] = i
        dmas = list(dl.values())
        self._bar_idx = len(self.all)
        for e in ENGS:
            self.pending[e] = self.pending[e] + lasts + dmas

    def _add(self, eng, emit, reads, writes, is_dma, semkey, after=()):
        ins = Ins()
        ins.eng = eng
        ins.emit = emit
        ins.is_dma = is_dma
        ins.needed = is_dma
        ins.semkey = semkey
        deps = [(0, X) for X in after]
        if self.pending[eng]:
            deps += [(0, X) for X in self.pending[eng]]
            self.pending[eng] = []
        for r in reads:
            if r.w is not None:
                deps.append((0, r.w))
            r.rs.append(ins)
        for r in writes:
            if r.w is not None and r.w is not ins:
                deps.append((1, r.w))
            for q in r.rs:
                if q is not ins:
                    deps.append((2, q))
            r.w = ins
            r.rs = []
        ins.deps = deps
        ins.pos = len(self.streams[eng])
        self.streams[eng].append(ins)
        self.all.append(ins)
        return ins

    def finalize(self):
        eclock = {e: {} for e in ENGS}
        dcnt = {}
        for ins in self.all:
            ec = eclock[ins.eng]
            waits = []
            for kind, X in ins.deps:
                if X is ins:
                    continue
                if (not X.is_dma) and (not ins.is_dma) and X.eng == ins.eng:
                    if ins.eng == 'pe' or kind != 0:
                        continue
                if ec.get(X.dk, 0) >= X.dv:
                    continue
                waits.append(X)
                X.needed = True
                nec = dict(ec)
                for k, v in X.cbase.items():
                    if nec.get(k, 0) < v:
                        nec[k] = v
                if nec.get(X.dk, 0) < X.dv:
                    nec[X.dk] = X.dv
                ec = nec
                eclock[ins.eng] = ec
            ins.waits = waits
            if ins.is_dma:
                c = dcnt.get(ins.semkey, 0) + 1
                dcnt[ins.semkey] = c
                ins.dk = ('d', ins.semkey)
                ins.dv = c
            else:
                ins.dk = ins.eng
                ins.dv = ins.pos + 1
            ins.cbase = ec
        self.dma_keys = list(dcnt.keys())
        self.n_ord = {}
        for e in ENGS:
            o = 0
            for ins in self.streams[e]:
                if ins.needed and not ins.is_dma:
                    o += 1
                    ins.ord = o
            self.n_ord[e] = o

    def emit(self, stack):
        nc = self.nc
        self.finalize()
        csem = {}
        for e in ENGS:
            n = (self.n_ord[e] + EPOCH - 1) // EPOCH
            csem[e] = [stack.enter_context(nc.semaphore(f"c_{e}_{i}")) for i in range(n)]
        dsem = {}
        for i, k in enumerate(self.dma_keys):
            dsem[k] = stack.enter_context(nc.semaphore(f"d_{i}"))
        self.nsem = sum(len(v) for v in csem.values()) + len(dsem)

        def semval(X):
            if X.is_dma:
                return dsem[X.semkey], X.dv * 16
            o = X.ord - 1
            return csem[X.eng][o // EPOCH], (o % EPOCH) + 1

        def run(ename, eng):
            for ins in self.streams[ename]:
                for X in ins.waits:
                    s, v = semval(X)
                    eng.wait_ge(s, v)
                bi = ins.emit(eng)
                if ins.is_dma:
                    bi.then_inc(dsem[ins.semkey], 16)
                elif ins.needed:
                    s, v = semval(ins)
                    bi.then_inc(s, 1)
            for fe, lst in self.finals:
                if fe == ename:
                    for X in lst:
                        s, v = semval(X)
                        eng.wait_ge(s, v)

        block = stack.enter_context(nc.Block())

        @block.tensor
        def _(eng):
            run('pe', eng)

        @block.scalar
        def _(eng):
            run('act', eng)

        @block.vector
        def _(eng):
            run('dve', eng)

        @block.gpsimd
        def _(eng):
            run('pool', eng)

        @block.sync
        def _(eng):
            run('sp', eng)


D = 1024
PCOLS = 4112
DFF = 2816
NFC = DFF // 128
NEXP = 16
XW = 1088
EPS = 1e-6
PADF = 112
QSCALE = 128 ** -0.5


SB_BASE = 17408
SB_LIMIT = 229376


class Arena:
    def __init__(self, nc, base=0):
        self.nc = nc
        self.off = base
        self.n = 0

    def t(self, shape, dt, name=None):
        sz = {F32: 4, BF16: 2, I32: 4}[dt]
        per = 1
        for s in shape[1:]:
            per *= s
        nbytes = per * sz
        self.off = (self.off + 63) // 64 * 64
        Arena.cnt = getattr(Arena, 'cnt', 0) + 1
        h = self.nc.alloc_sbuf_tensor_at(name or f"t{Arena.cnt}", list(shape), dt, offset=self.off)
        self.off += nbytes
        assert self.off <= SB_LIMIT, f"SBUF overflow {self.off}"
        return h


def build(NT, NEXP_RUN=NEXP, dbg=False):
    T = NT * 128
    NREAL = T - PADF
    CAP = 2 * NREAL // 16
    NSL = (CAP + 127) // 128 + (1 if CAP % 128 == 0 else 0)
    NSLR = NSL * 128
    SEQ = T - 128
    nc = bass.Bass("TRN2", target_bir_lowering=False)

    def din(name, shape, dt=F32):
        return nc.dram_tensor(name, list(shape), dt, kind="ExternalInput").ap()

    def dscr(name, shape, dt):
        return nc.dram_tensor(name, list(shape), dt, kind=("ExternalOutput" if dbg else "Internal")).ap()

    x = din("x", [SEQ, D])
    meta = din("meta", [16, D])
    w_in = din("w_in", [D, PCOLS])
    mixg = din("mix_g", [1, D])
    b_gates = din("b_gates", [1, 16])
    conv_w = din("conv_w", [5, 1024])
    conv_b = din("conv_b", [1, 1024])
    rlogit = din("rlogit", [1, 8])
    gn_g = din("gn_g", [1, 1024])
    w_out = din("w_out", [D, D])
    ffn_g = din("ffn_g", [1, D])
    w_router = din("w_router", [D, 16])
    w_gate = din("w_gate", [NEXP, D, DFF])
    w_up = din("w_up", [NEXP, D, DFF])
    w_down = din("w_down", [NEXP, DFF, D])
    fin_g = din("fin_g", [1, D])
    c_ident = din("c_ident", [128, 128], BF16)
    c_tri = din("c_tri", [6, 128, 128])
    c_mask = din("c_mask", [2, 128, 128])
    c_slt = din("c_slt", [128, 128], BF16)
    c_cs = din("c_cs", [T, 512])
    c_tok = din("c_tok", [128, NT], I32)
    c_padm = din("c_padm", [128, 1])
    c_xsinit = din("c_xsinit", [NSLR, XW], BF16)

    out = nc.dram_tensor("out", [SEQ, D], F32, kind="ExternalOutput").ap()

    QT = dscr("QT", [NT, 128, 8, 128], BF16)
    KT = dscr("KT", [NT, 128, 8, 128], BF16)
    KM = dscr("KM", [NT, 128, 8, 128], BF16)
    VP = dscr("VP", [NT, 128, 8, 130], BF16)
    GO = dscr("GO", [NT, 128, 1024], F32)
    SBS = dscr("SBS", [NT, 128, 8, 130], BF16)
    U2X = dscr("U2X", [T, XW], BF16)
    XS = [dscr(f"XS{i}", [NSLR, XW], BF16) for i in range(NEXP)]
    OACC = dscr("OACC", [T + NSLR, D], F32)

    S = Sched(nc)
    st = ExitStack()
    _regs = {}

    def breg(e, v):
        if v not in _regs:
            _regs[v] = e.to_reg(v)
        return _regs[v]

    psb = [st.enter_context(nc.psum_tensor(f"ps{i}", [128, 512], F32)) for i in range(8)]
    r_ps = [Res(f"ps{i}") for i in range(8)]

    def psbf(i):
        return psb[i][:].bitcast(BF16)

    P = Arena(nc, SB_BASE)
    ident = P.t([128, 128], BF16); r_ident = Res("ident")
    tri = P.t([128, 6, 128], F32); r_tri = Res("tri")
    maskfb = P.t([128, 2, 128], F32); r_mask = Res("mask")
    padm = P.t([128, 1], F32); r_padm = Res("padm")
    tok = P.t([128, NT], I32); r_tok = Res("tok")
    PBASE0 = P.off
    SC = P.t([128, NT, 6, 8], F32); r_SC = [Res(f"SC{j}") for j in range(NT)]
    AFF = P.t([128, NT, 16], F32); r_AFF = [Res(f"AFF{j}") for j in range(NT)]
    PBASE = P.off

    def ld(dst, src, res, key, q='sp'):
        return S.dma(q, lambda e: e.dma_start(out=dst, in_=src), [], [res], key)

    ld(ident[:], c_ident[:, :], r_ident, 'c0')
    ld(tri[:], c_tri.rearrange("k m l -> m k l"), r_tri, 'c1')
    ld(maskfb[:], c_mask.rearrange("k m l -> m k l"), r_mask, 'c2')
    ld(padm[:], c_padm[:, :], r_padm, 'c3')
    ld(tok[:], c_tok[:, :], r_tok, 'c4')

    A1 = Arena(nc, PBASE)
    Wb = A1.t([128, 8, PCOLS], BF16); r_Wb = Res("Wb")
    wstg = [A1.t([128, 1028], F32) for _ in range(2)]; r_wstg = [Res("wstg0"), Res("wstg1")]
    mixgT = A1.t([128, 8], F32); r_mixgT = Res("mixgT")
    xt = [A1.t([128, D], F32) for _ in range(2)]; r_xt = [Res("xt0"), Res("xt1")]
    junk = A1.t([128, D], BF16); r_junk = Res("junk")
    ss = A1.t([128, 1], F32); r_ss = Res("ss")
    rstd = A1.t([128, 1], F32); r_rstd = Res("rstd")
    xn = [A1.t([128, D], BF16) for _ in range(2)]; r_xn = [Res("xn0"), Res("xn1")]
    UW = [A1.t([128, 8, 384], BF16) for _ in range(2)]; r_UW = [Res("UW0"), Res("UW1")]
    cs = [A1.t([128, 512], F32) for _ in range(2)]; r_cs = [Res("cs0"), Res("cs1")]
    qk32 = [A1.t([128, 2, 512], F32) for _ in range(2)]; r_qk32 = [Res("qk320"), Res("qk321")]
    rt = [A1.t([128, 4, 256], F32) for _ in range(2)]; r_rt = [Res("rt0"), Res("rt1")]
    qkr = [A1.t([128, 2, 512], BF16) for _ in range(2)]; r_qkr = [Res("qkr0"), Res("qkr1")]
    qkT = [A1.t([128, 8, 128], BF16) for _ in range(2)]; r_qkT = [Res("qkT0"), Res("qkT1")]
    vp = [A1.t([128, 8, 130], BF16) for _ in range(2)]; r_vp = [Res("vp0"), Res("vp1")]
    go = [A1.t([128, 1024], F32) for _ in range(2)]; r_go = [Res("go0"), Res("go1")]
    G32 = A1.t([128, 32], F32); r_G32 = Res("G32")
    BASE = A1.t([128, 32], F32); r_BASE = Res("BASE")
    spx = A1.t([128, 16], F32); r_spx = Res("spx")
    A16 = A1.t([128, 16], F32); r_A16 = Res("A16")
    Xc = [A1.t([128, 8, 132], F32) for _ in range(2)]; r_Xc = [Res("Xc0"), Res("Xc1")]
    cw = A1.t([128, 5, 8], F32); r_cw = Res("cw")
    cb = A1.t([128, 8], F32); r_cb = Res("cb")
    cacc = [A1.t([128, 8, 128], F32) for _ in range(2)]; r_cacc = [Res("cacc0"), Res("cacc1")]
    r_cacc2 = [Res("cacc20"), Res("cacc21")]
    mqkT = [A1.t([128, 8, 128], BF16) for _ in range(2)]; r_mqkT = [Res("mqkT0"), Res("mqkT1")]
    kmm = [A1.t([128, 4, 128], BF16) for _ in range(2)]; r_kmm = [Res("kmm0"), Res("kmm1")]
    ctmp_t = A1.t([128, 4, 128], F32); r_ctmp = Res("ctmp")

    S.dma('sp', lambda e: e.dma_start(out=mixgT[:], in_=mixg.rearrange("o (c p) -> p (o c)", p=128),
                                      allow_slow_non_contiguous=True), [], [r_mixgT], 'w0')
    k = 0
    for c in range(8):
        for q4 in range(4):
            b = k % 2
            c0 = q4 * 1028
            S.dma('sp', lambda e, b=b, c=c, c0=c0: e.dma_start(out=wstg[b][:], in_=w_in[c * 128:(c + 1) * 128, c0:c0 + 1028]),
                  [], [r_wstg[b]], f'wst{b}')
            eng = 'dve' if k % 2 == 0 else 'pool'
            S.op(eng, lambda e, b=b, c=c, c0=c0: e.tensor_scalar(out=Wb[:, c, c0:c0 + 1028], in0=wstg[b][:], scalar1=mixgT[:, c:c + 1],
                                                                 scalar2=None, op0=ALU.mult),
                 [r_wstg[b], r_mixgT], [r_Wb])
            k += 1
    S.op('pool', lambda e: e.memset(BASE[:], 0.0), [], [r_BASE])
    S.op('pool', lambda e: e.memset(G32[:], 0.0), [], [r_G32])
    for (c0, src) in ((4, b_gates[:, 0:4]), (12, b_gates[:, 4:8]), (20, b_gates[:, 8:12]), (28, b_gates[:, 12:16]),
                      (16, rlogit[:, 0:4]), (24, rlogit[:, 4:8])):
        S.dma('sp', lambda e, c0=c0, src=src: e.dma_start(out=BASE[:, c0:c0 + 4], in_=src.to_broadcast([128, 4])), [], [r_BASE], 'w1')
    for t in range(5):
        S.dma('sp', lambda e, t=t: e.dma_start(out=cw[:, t, :], in_=conv_w[t:t + 1, :].rearrange("o (g p) -> p (o g)", p=128), allow_slow_non_contiguous=True), [], [r_cw], 'w2a')
    S.dma('sp', lambda e: e.dma_start(out=cb[:], in_=conv_b.rearrange("o (g p) -> p (o g)", p=128), allow_slow_non_contiguous=True), [], [r_cb], 'w2b')
    for b in range(2):
        S.op('pool', lambda e, b=b: e.memset(UW[b][:], 0.0), [], [r_UW[b]])
        S.op('pool', lambda e, b=b: e.memset(vp[b][:], 0.0), [], [r_vp[b]])
        S.op('pool', lambda e, b=b: e.memset(vp[b][:, :, 128:129], 1.0), [], [r_vp[b]])
    S.op('pool', lambda e: e.memset(xt[0][:], 0.0), [], [r_xt[0]])

    r_QT = [Res(f"QT{j}") for j in range(NT)]
    r_KT = [Res(f"KT{j}") for j in range(NT)]
    r_KM = [Res(f"KM{j}") for j in range(NT)]
    r_VP = [Res(f"VP{j}") for j in range(NT)]
    r_GO = [Res(f"GO{j}") for j in range(NT)]
    r_SBS = [Res(f"SBS{j}") for j in range(NT)]

    def x_rows(j):
        return x[(j - 1) * 128:j * 128, :]

    for j in range(NT + 1):
        b = j % 2
        if j < NT:
            if j == 0:
                S.dma('sp', lambda e: e.dma_start(out=xt[0][PADF:128, :], in_=meta[:, :]), [], [r_xt[0]], 'xt0')
            else:
                S.dma('sp', lambda e, j=j, b=b: e.dma_start(out=xt[b][:], in_=x_rows(j)), [], [r_xt[b]], f'xt{b}')
            S.dma('sp', lambda e, j=j, b=b: e.dma_start(out=cs[b][:], in_=c_cs[j * 128:(j + 1) * 128, :]), [], [r_cs[b]], f'cs{b}')
            S.op('act', lambda e, b=b: e.activation(out=junk[:], in_=xt[b][:], func=AF.Square, accum_out=ss[:]), [r_xt[b]], [r_junk, r_ss])
            S.op('act', lambda e: e.activation(out=rstd[:], in_=ss[:], func=AF.Sqrt, scale=1.0 / D, bias=EPS), [r_ss], [r_rstd])
            S.op('dve', lambda e: e.reciprocal(out=rstd[:], in_=rstd[:]), [r_rstd], [r_rstd])
            S.op('act', lambda e, b=b: e.activation(out=xn[b][:], in_=xt[b][:], func=AF.Copy, scale=rstd[:]), [r_xt[b], r_rstd], [r_xn[b]])
            for c in range(8):
                S.op('pe', lambda e, c=c, b=b: e.transpose(out=psbf(0)[:, c * 128:(c + 1) * 128], in_=xn[b][:, c * 128:(c + 1) * 128], identity=ident[:]),
                     [r_xn[b], r_ident], [r_ps[0]])
        if j >= 1:
            S.op('dve', lambda e, b=b: e.tensor_copy(out=UW[b][:, :, 0:256], in_=UW[1 - b][:, :, 128:384]), [r_UW[1 - b]], [r_UW[b]])
        if j < NT:
            S.op('dve', lambda e, b=b: e.tensor_copy(out=UW[b][:, :, 256:384], in_=psbf(0).rearrange("p (c t) -> p c t", c=8)),
                 [r_ps[0]], [r_UW[b]])
        else:
            S.op('dve', lambda e, b=b: e.memset(UW[b][:, :, 256:384], 0.0), [], [r_UW[b]])

        if j < NT:
            def uT(c, b=b):
                return UW[b][:, c, 256:384]
            def tok_proj(bank, col0, ncol, b=b):
                for c in range(8):
                    lhs = UW[b][:, c, 256:384]
                    S.op('pe', lambda e, c=c, lhs=lhs: e.matmul(psb[bank][:, 0:ncol], lhsT=lhs, rhs=Wb[:, c, col0:col0 + ncol], start=(c == 0), stop=(c == 7)),
                         [r_UW[b], r_Wb], [r_ps[bank]])
            tok_proj(1, 0, 512)
            S.op('act', lambda e, b=b: e.copy(out=qk32[b][:, 0, :], in_=psb[1][:]), [r_ps[1]], [r_qk32[b]])
            tok_proj(2, 512, 512)
            S.op('act', lambda e, b=b: e.copy(out=qk32[b][:, 1, :], in_=psb[2][:]), [r_ps[2]], [r_qk32[b]])
            def rot(b=b):
                xv = qk32[b][:].rearrange("p a (h t d) -> p (a h) t d", h=4, t=2)
                ov = qkr[b][:].rearrange("p a (h t d) -> p (a h) t d", h=4, t=2)
                x1 = xv[:, :, 0, :]; x2 = xv[:, :, 1, :]
                cosv = cs[b][:, 0:256].rearrange("p (h d) -> p h d", h=4)
                sinv = cs[b][:, 256:512].rearrange("p (h d) -> p h d", h=4)
                tv = rt[b][:].rearrange("p k (a d) -> p k a d", a=4)
                for a in range(2):
                    X1 = x1[:, a * 4:(a + 1) * 4, :]; X2 = x2[:, a * 4:(a + 1) * 4, :]
                    O1 = ov[:, a * 4:(a + 1) * 4, 0, :]; O2 = ov[:, a * 4:(a + 1) * 4, 1, :]
                    S.op('dve', lambda e, X1=X1: e.tensor_tensor(out=tv[:, 0], in0=X1, in1=cosv, op=ALU.mult), [r_qk32[b], r_cs[b]], [r_rt[b]])
                    S.op('dve', lambda e, X2=X2: e.tensor_tensor(out=tv[:, 1], in0=X2, in1=sinv, op=ALU.mult), [r_qk32[b], r_cs[b]], [r_rt[b]])
                    S.op('dve', lambda e, X2=X2: e.tensor_tensor(out=tv[:, 2], in0=X2, in1=cosv, op=ALU.mult), [r_qk32[b], r_cs[b]], [r_rt[b]])
                    S.op('dve', lambda e, X1=X1: e.tensor_tensor(out=tv[:, 3], in0=X1, in1=sinv, op=ALU.mult), [r_qk32[b], r_cs[b]], [r_rt[b]])
                    S.op('dve', lambda e, O1=O1: e.tensor_tensor(out=O1, in0=tv[:, 0], in1=tv[:, 1], op=ALU.subtract), [r_rt[b]], [r_qkr[b]])
                    S.op('dve', lambda e, O2=O2: e.tensor_tensor(out=O2, in0=tv[:, 2], in1=tv[:, 3], op=ALU.add), [r_rt[b]], [r_qkr[b]])
            rot()
            for a in range(2):
                for h in range(4):
                    i = a * 4 + h
                    S.op('pe', lambda e, a=a, h=h, i=i, b=b: e.transpose(out=psbf(5)[:, i * 128:(i + 1) * 128], in_=qkr[b][:, a, h * 128:(h + 1) * 128], identity=ident[:]),
                         [r_qkr[b], r_ident], [r_ps[5]])
            S.op('act', lambda e, b=b: e.copy(out=qkT[b][:], in_=psbf(5).rearrange("p (i t) -> p i t", i=8)), [r_ps[5]], [r_qkT[b]])
            S.dma('sp', lambda e, j=j, b=b: e.dma_start(out=QT[j, :, 0:4, :], in_=qkT[b][:, 0:4, :]), [r_qkT[b]], [r_QT[j]], f'st_qkT{b}')
            S.dma('sp', lambda e, j=j, b=b: e.dma_start(out=KT[j, :, 0:4, :], in_=qkT[b][:, 4:8, :]), [r_qkT[b]], [r_KT[j]], f'st_qkT{b}')
            S.dma('sp', lambda e, j=j, b=b: e.dma_start(out=KM[j, :, 0:4, :], in_=qkr[b][:, 1, :].rearrange("p (h d) -> p h d", h=4)), [r_qkr[b]], [r_KM[j]], f'st_qkr{b}')
            tok_proj(1, 1024, 512)
            S.op('act', lambda e, b=b: e.copy(out=vp[b][:, 0:4, 0:128], in_=psb[1][:].rearrange("p (h d) -> p h d", h=4)), [r_ps[1]], [r_vp[b]])
            tok_proj(2, 1536, 512)
            S.op('act', lambda e, b=b: e.activation(out=go[b][:, 0:512], in_=psb[2][:], func=AF.Silu), [r_ps[2]], [r_go[b]])
            tok_proj(1, 3072, 512)
            S.op('act', lambda e, b=b: e.copy(out=vp[b][:, 4:8, 0:128], in_=psb[1][:].rearrange("p (h d) -> p h d", h=4)), [r_ps[1]], [r_vp[b]])
            tok_proj(2, 3584, 512)
            S.op('act', lambda e, b=b: e.activation(out=go[b][:, 512:1024], in_=psb[2][:], func=AF.Sigmoid), [r_ps[2]], [r_go[b]])
            S.dma('sp', lambda e, j=j, b=b: e.dma_start(out=VP[j], in_=vp[b][:]), [r_vp[b]], [r_VP[j]], f'st_vp{b}')
            S.dma('sp', lambda e, j=j, b=b: e.dma_start(out=GO[j], in_=go[b][:]), [r_go[b]], [r_GO[j]], f'st_go{b}')
            tok_proj(6, 4096, 16)
            g32v = G32[:].rearrange("p (a h) -> p a h", a=4)
            basev = BASE[:].rearrange("p (a h) -> p a h", a=4)
            S.op('dve', lambda e: e.tensor_tensor(out=g32v[:, :, 4:8], in0=psb[6][:, 0:16].rearrange("p (a h) -> p a h", a=4), in1=basev[:, :, 4:8], op=ALU.add),
                 [r_ps[6], r_BASE], [r_G32])
            if j == 0:
                S.op('dve', lambda e: e.tensor_copy(out=g32v[:, :, 0:4], in_=basev[:, :, 0:4]), [r_BASE], [r_G32])
            S.op('act', lambda e: e.activation(out=spx[:], in_=G32[:, 16:32], func=AF.Exp, scale=-1.0), [r_G32], [r_spx])
            S.op('act', lambda e: e.activation(out=spx[:], in_=spx[:], func=AF.Ln, bias=1.0), [r_spx], [r_spx])
            k0 = 3 if j == 0 else 0
            for q in range(3):
                S.op('pe', lambda e, q=q, k0=k0: e.matmul(psb[6][:, 16 + q * 16:32 + q * 16], lhsT=tri[:, k0 + q, :], rhs=spx[:], start=True, stop=True),
                     [r_tri, r_spx], [r_ps[6]])
            cumv = psb[6][:, 16:64].rearrange("p (a b) -> p a b", b=24)[:, :, 0:8]
            totv = psb[6][:, 48:64].rearrange("p (a h) -> p a h", a=2)
            S.op('dve', lambda e: e.tensor_tensor(out=A16[:].rearrange("p (a h) -> p a h", a=2), in0=cumv, in1=G32[:, 0:16].rearrange("p (a h) -> p a h", a=2), op=ALU.add),
                 [r_ps[6], r_G32], [r_A16])
            S.op('act', lambda e, j=j: e.activation(out=SC[:, j, 0:2, :], in_=A16[:].rearrange("p (a h) -> p a h", a=2), func=AF.Exp), [r_A16], [r_SC[j]])
            S.op('act', lambda e, j=j: e.activation(out=SC[:, j, 2:4, :], in_=cumv, func=AF.Exp, scale=-1.0, bias=math.log(QSCALE)), [r_ps[6]], [r_SC[j]])
            S.op('act', lambda e, j=j: e.activation(out=SC[:, j, 4:6, :], in_=totv, func=AF.Exp, scale=-1.0), [r_ps[6]], [r_SC[j]])

        if j >= 1:
            jj = j - 1
            for g in range(8):
                bank = 3 if g < 3 else (4 if g < 6 else 7)
                off = (g % 3) * 132
                col0 = 2048 + g * 128
                for c in range(8):
                    S.op('pe', lambda e, c=c, bank=bank, off=off, col0=col0, b=b: e.matmul(psb[bank][:, off:off + 132], lhsT=Wb[:, c, col0:col0 + 128],
                                                                                      rhs=UW[b][:, c, 126:258], start=(c == 0), stop=(c == 7)),
                         [r_UW[b], r_Wb], [r_ps[bank]])
            for bank, g0, ng in ((3, 0, 3), (4, 3, 3), (7, 6, 2)):
                S.op('act', lambda e, bank=bank, g0=g0, ng=ng, b=b: e.copy(out=Xc[b][:, g0:g0 + ng, :], in_=psb[bank][:, 0:ng * 132].rearrange("p (g t) -> p g t", g=ng)),
                     [r_ps[bank]], [r_Xc[b]])
            ctmp = ctmp_t[:]
            for t in range(5):
                wb_t = cw[:, t, 0:4].unsqueeze(2).to_broadcast([128, 4, 128])
                if t == 0:
                    S.op('pool', lambda e, wb_t=wb_t, b=b: e.tensor_tensor(out=cacc[b][:, 0:4, :], in0=Xc[b][:, 0:4, 0:128], in1=wb_t, op=ALU.mult), [r_Xc[b], r_cw], [r_cacc[b]])
                else:
                    S.op('pool', lambda e, wb_t=wb_t, t=t, b=b: e.tensor_tensor(out=ctmp, in0=Xc[b][:, 0:4, t:t + 128], in1=wb_t, op=ALU.mult), [r_Xc[b], r_cw], [r_ctmp])
                    S.op('pool', lambda e, b=b: e.tensor_tensor(out=cacc[b][:, 0:4, :], in0=cacc[b][:, 0:4, :], in1=ctmp, op=ALU.add), [r_cacc[b], r_ctmp], [r_cacc[b]])
            S.op('pool', lambda e, b=b: e.tensor_tensor(out=cacc[b][:, 0:4, :], in0=cacc[b][:, 0:4, :], in1=cb[:, 0:4].unsqueeze(2).to_broadcast([128, 4, 128]), op=ALU.add), [r_cacc[b], r_cb], [r_cacc[b]])
            for g in range(4, 8):
                S.op('dve', lambda e, g=g, b=b: e.tensor_scalar(out=cacc[b][:, g, :], in0=Xc[b][:, g, 0:128], scalar1=cw[:, 0, g:g + 1], scalar2=cb[:, g:g + 1], op0=ALU.mult, op1=ALU.add),
                     [r_Xc[b], r_cw, r_cb], [r_cacc2[b]])
                for t in range(1, 5):
                    S.op('dve', lambda e, g=g, t=t, b=b: e.scalar_tensor_tensor(out=cacc[b][:, g, :], in0=Xc[b][:, g, t:t + 128], scalar=cw[:, t, g:g + 1], in1=cacc[b][:, g, :],
                                                                             op0=ALU.mult, op1=ALU.add),
                         [r_Xc[b], r_cw, r_cacc2[b]], [r_cacc2[b]])
            S.op('act', lambda e, b=b: e.activation(out=mqkT[b][:], in_=cacc[b][:], func=AF.Silu), [r_cacc[b], r_cacc2[b]], [r_mqkT[b]])
            S.dma('sp', lambda e, jj=jj, b=b: e.dma_start(out=QT[jj, :, 4:8, :], in_=mqkT[b][:, 0:4, :]), [r_mqkT[b]], [r_QT[jj]], f'st_mqk{b}')
            S.dma('sp', lambda e, jj=jj, b=b: e.dma_start(out=KT[jj, :, 4:8, :], in_=mqkT[b][:, 4:8, :]), [r_mqkT[b]], [r_KT[jj]], f'st_mqk{b}')
            for h in range(4):
                S.op('pe', lambda e, h=h, b=b: e.transpose(out=psbf(5)[:, h * 128:(h + 1) * 128], in_=mqkT[b][:, 4 + h, :], identity=ident[:]), [r_mqkT[b], r_ident], [r_ps[5]])
            S.op('dve', lambda e, b=b: e.tensor_copy(out=kmm[b][:], in_=psbf(5)[:, 0:512].rearrange("p (h d) -> p h d", h=4)), [r_ps[5]], [r_kmm[b]])
            S.dma('sp', lambda e, jj=jj, b=b: e.dma_start(out=KM[jj, :, 4:8, :], in_=kmm[b][:]), [r_kmm[b]], [r_KM[jj]], f'st_kmm{b}')

    S.barrier()
    A2 = Arena(nc, PBASE)
    Sst = A2.t([128, 8, 130], F32); r_Sst = Res("Sst")
    Sbf = [A2.t([128, 8, 130], BF16) for _ in range(2)]; r_Sbf = [Res("Sbf0"), Res("Sbf1")]
    kmt = [A2.t([128, 8, 128], BF16) for _ in range(2)]; r_kmt = [Res("kmt0"), Res("kmt1")]
    vpt = [A2.t([128, 8, 130], BF16) for _ in range(2)]; r_vpt = [Res("vpt0"), Res("vpt1")]
    kti = [A2.t([128, 8, 128], BF16) for _ in range(2)]; r_kti = [Res("kti0"), Res("kti1")]
    A2END = A2.off

    def state_update(kt_src, vp_src, r_k, r_v, cidx, gidx, j, ugroups, kb):
        S.op('dve', lambda e: e.tensor_tensor(out=kti[kb][:], in0=kt_src[:], in1=SC[:, j, cidx, :].unsqueeze(2).to_broadcast([128, 8, 128]), op=ALU.mult),
             [r_k, r_SC[j]], [r_kti[kb]])
        for h in range(8):
            bank, c0 = ugroups[h // 3]
            off = c0 + (h % 3) * 130
            S.op('pe', lambda e, h=h, bank=bank, off=off: e.matmul(psb[bank][:, off:off + 129], lhsT=kti[kb][:, h, :], rhs=vp_src[:, h, 0:129], start=True, stop=True),
                 [r_kti[kb], r_v], [r_ps[bank]])
        for gi, (bank, c0) in enumerate(ugroups):
            nh = 3 if gi < 2 else 2
            S.op('dve', lambda e, gi=gi, bank=bank, c0=c0, nh=nh: e.tensor_tensor(out=Sst[:, gi * 3:gi * 3 + nh, 0:129],
                                                                             in0=psb[bank][:, c0:c0 + nh * 130].rearrange("p (h n) -> p h n", h=nh)[:, :, 0:129],
                                                                             in1=Sst[:, gi * 3:gi * 3 + nh, 0:129], op=ALU.add),
                 [r_ps[bank], r_Sst], [r_Sst])
        S.op('dve', lambda e: e.tensor_tensor(out=Sst[:, :, 0:129], in0=Sst[:, :, 0:129], in1=SC[:, j, gidx, :].unsqueeze(2).to_broadcast([128, 8, 129]), op=ALU.mult),
             [r_Sst, r_SC[j]], [r_Sst])

    S.op('pool', lambda e: e.memset(Sst[:], 0.0), [], [r_Sst])
    for b in range(2):
        S.op('pool', lambda e, b=b: e.memset(Sbf[b][:], 0.0), [], [r_Sbf[b]])
    for j in range(NT - 1, -1, -1):
        b = j % 2
        S.dma('sp', lambda e, j=j, b=b: e.dma_start(out=kmt[b][:], in_=KM[j]), [r_KM[j]], [r_kmt[b]], f'l2km{b}')
        S.dma('sp', lambda e, j=j, b=b: e.dma_start(out=vpt[b][:], in_=VP[j]), [r_VP[j]], [r_vpt[b]], f'l2vp{b}')
        S.op('act', lambda e, b=b: e.copy(out=Sbf[b][:, :, 0:129], in_=Sst[:, :, 0:129]), [r_Sst], [r_Sbf[b]])
        S.dma('sp', lambda e, j=j, b=b: e.dma_start(out=SBS[j], in_=Sbf[b][:]), [r_Sbf[b]], [r_SBS[j]], f's2a{b}')
        if j > 0:
            state_update(kmt[b], vpt[b], r_kmt[b], r_vpt[b], 1, 5, j, ((0, 0), (1, 0), (2, 0)), b)

    S.barrier()
    A3 = Arena(nc, A2END)
    Wo = A3.t([128, 8, D], BF16); r_Wo = Res("Wo")
    Wr = A3.t([128, 8, 16], BF16); r_Wr = Res("Wr")
    wr32 = A3.t([128, 8, 16], F32); r_wr32 = Res("wr32")
    gnb = A3.t([128, D], F32); r_gnb = Res("gnb")
    fgb = A3.t([128, D], F32); r_fgb = Res("fgb")
    qtt = [A3.t([128, 8, 128], BF16) for _ in range(2)]; r_qtt = [Res("qtt0"), Res("qtt1")]
    ktt = [A3.t([128, 8, 128], BF16) for _ in range(2)]; r_ktt = [Res("ktt0"), Res("ktt1")]
    sbt = [A3.t([128, 8, 130], BF16) for _ in range(2)]; r_sbt = [Res("sbt0"), Res("sbt1")]
    got = [A3.t([128, D], F32) for _ in range(2)]; r_got = [Res("got0"), Res("got1")]
    xt2 = [A3.t([128, D], F32) for _ in range(2)]; r_xt2 = [Res("xt20"), Res("xt21")]
    PF = [A3.t([128, 8, 128], BF16) for _ in range(2)]; r_PF = [Res("PF0"), Res("PF1")]
    PB = [A3.t([128, 8, 128], BF16) for _ in range(2)]; r_PB = [Res("PB0"), Res("PB1")]
    Sfb = [A3.t([128, 8, 130], BF16) for _ in range(2)]; r_Sfb = [Res("Sfb0"), Res("Sfb1")]
    Y = [A3.t([128, D], F32) for _ in range(2)]; r_Y = [Res("Y0"), Res("Y1")]
    dn = A3.t([128, 3, 16], F32); r_dn = Res("dn")
    bst = A3.t([128, 8, 6], F32); r_bst = Res("bst")
    mv = A3.t([128, 8, 2], F32); r_mv = Res("mv")
    ybf = A3.t([128, D], BF16); r_ybf = Res("ybf")
    yT = A3.t([128, 8, 128], BF16); r_yT = Res("yT")
    h1 = [A3.t([128, D], F32) for _ in range(2)]; r_h1 = [Res("h10"), Res("h11")]
    u2x = [A3.t([128, XW], BF16) for _ in range(2)]; r_u2x = [Res("u2x0"), Res("u2x1")]
    u2T = A3.t([128, 8, 128], BF16); r_u2T = Res("u2T")
    sm = A3.t([128, 4], F32); r_sm = Res("sm")
    junk2 = A3.t([128, D], BF16); r_junk2 = Res("junk2")
    ss2 = A3.t([128, 1], F32); r_ss2 = Res("ss2")
    rstd2 = A3.t([128, 1], F32); r_rstd2 = Res("rstd2")
    ex = A3.t([128, 16], F32); r_ex = Res("ex")
    r_U2X = [Res(f"U2X{j}") for j in range(NT)]
    r_OACC = Res("OACC")

    for c in range(8):
        b = c % 2
        S.dma('sp', lambda e, b=b, c=c: e.dma_start(out=xt2[b][:], in_=w_out[c * 128:(c + 1) * 128, :]), [], [r_xt2[b]], f'xt2{b}')
        S.op('dve' if c % 2 else 'act', (lambda e, b=b, c=c: e.tensor_copy(out=Wo[:, c, :], in_=xt2[b][:])) if c % 2 else
             (lambda e, b=b, c=c: e.copy(out=Wo[:, c, :], in_=xt2[b][:])), [r_xt2[b]], [r_Wo])
    S.dma('sp', lambda e: e.dma_start(out=wr32[:], in_=w_router.rearrange("(c p) n -> p c n", p=128)), [], [r_wr32], 'w3')
    S.op('dve', lambda e: e.tensor_copy(out=Wr[:], in_=wr32[:]), [r_wr32], [r_Wr])
    S.dma('sp', lambda e: e.dma_start(out=gnb[:], in_=gn_g.to_broadcast([128, D])), [], [r_gnb], 'w4a')
    S.dma('sp', lambda e: e.dma_start(out=fgb[:], in_=ffn_g.to_broadcast([128, D])), [], [r_fgb], 'w4b')
    S.op('pool', lambda e: e.memset(Sst[:], 0.0), [], [r_Sst])
    for b in range(2):
        S.op('pool', lambda e, b=b: e.memset(Sfb[b][:], 0.0), [], [r_Sfb[b]])
        S.op('pool', lambda e, b=b: e.memset(u2x[b][:], 0.0), [], [r_u2x[b]])
    S.op('pool', lambda e: e.memset(xt2[0][:], 0.0), [r_Wo], [r_xt2[0]])

    for j in range(NT):
        b = j % 2
        S.dma('sp', lambda e, j=j, b=b: e.dma_start(out=qtt[b][:], in_=QT[j]), [r_QT[j]], [r_qtt[b]], f'l2q{b}')
        S.dma('sp', lambda e, j=j, b=b: e.dma_start(out=ktt[b][:], in_=KT[j]), [r_KT[j]], [r_ktt[b]], f'l2k{b}')
        S.dma('sp', lambda e, j=j, b=b: e.dma_start(out=kmt[b][:], in_=KM[j]), [r_KM[j]], [r_kmt[b]], f'l2km{b}')
        S.dma('sp', lambda e, j=j, b=b: e.dma_start(out=vpt[b][:], in_=VP[j]), [r_VP[j]], [r_vpt[b]], f'l2vp{b}')
        S.dma('sp', lambda e, j=j, b=b: e.dma_start(out=sbt[b][:], in_=SBS[j]), [r_SBS[j]], [r_sbt[b]], f'l2s{b}')
        S.dma('sp', lambda e, j=j, b=b: e.dma_start(out=got[b][:], in_=GO[j]), [r_GO[j]], [r_got[b]], f'l2g{b}')
        if j == 0:
            S.dma('sp', lambda e: e.dma_start(out=xt2[0][PADF:128, :], in_=meta[:, :]), [], [r_xt2[0]], 'xt20')
        else:
            S.dma('sp', lambda e, j=j, b=b: e.dma_start(out=xt2[b][:], in_=x_rows(j)), [], [r_xt2[b]], f'xt2{b}')
        for h in range(8):
            S.op('pe', lambda e, h=h, b=b: e.matmul(psb[h // 4][:, (h % 4) * 128:(h % 4 + 1) * 128], lhsT=ktt[b][:, h, :], rhs=qtt[b][:, h, :], start=True, stop=True),
                 [r_ktt[b], r_qtt[b]], [r_ps[h // 4]])
        for h in range(8):
            S.op('dve', lambda e, h=h, b=b, j=j: e.scalar_tensor_tensor(out=PF[b][:, h, :], in0=psb[h // 4][:, (h % 4) * 128:(h % 4 + 1) * 128], scalar=SC[:, j, 0, h:h + 1],
                                                                       in1=maskfb[:, 0, :], op0=ALU.mult, op1=ALU.mult),
                 [r_ps[h // 4], r_SC[j], r_mask], [r_PF[b]])
            S.op('dve', lambda e, h=h, b=b, j=j: e.scalar_tensor_tensor(out=PB[b][:, h, :], in0=psb[h // 4][:, (h % 4) * 128:(h % 4 + 1) * 128], scalar=SC[:, j, 1, h:h + 1],
                                                                       in1=maskfb[:, 1, :], op0=ALU.mult, op1=ALU.mult),
                 [r_ps[h // 4], r_SC[j], r_mask], [r_PB[b]])
        for h in range(8):
            S.op('pe', lambda e, h=h, b=b: e.matmul(psb[6][:, h:h + 1], lhsT=PF[b][:, h, :], rhs=vpt[b][:, h, 128:129], start=True, stop=False), [r_PF[b], r_vpt[b]], [r_ps[6]])
            S.op('pe', lambda e, h=h, b=b: e.matmul(psb[6][:, h:h + 1], lhsT=qtt[b][:, h, :], rhs=Sfb[b][:, h, 128:129], start=False, stop=True), [r_qtt[b], r_Sfb[b]], [r_ps[6]])
            S.op('pe', lambda e, h=h, b=b: e.matmul(psb[6][:, 8 + h:9 + h], lhsT=PB[b][:, h, :], rhs=vpt[b][:, h, 128:129], start=True, stop=False), [r_PB[b], r_vpt[b]], [r_ps[6]])
            S.op('pe', lambda e, h=h, b=b: e.matmul(psb[6][:, 8 + h:9 + h], lhsT=qtt[b][:, h, :], rhs=sbt[b][:, h, 128:129], start=False, stop=True), [r_qtt[b], r_sbt[b]], [r_ps[6]])
        for h in range(8):
            ob = 2 + h // 2
            c0 = (h % 2) * 256
            S.op('pe', lambda e, h=h, b=b, ob=ob, c0=c0: e.matmul(psb[ob][:, c0:c0 + 128], lhsT=PF[b][:, h, :], rhs=vpt[b][:, h, 0:128], start=True, stop=False), [r_PF[b], r_vpt[b]], [r_ps[ob]])
            S.op('pe', lambda e, h=h, b=b, ob=ob, c0=c0: e.matmul(psb[ob][:, c0:c0 + 128], lhsT=qtt[b][:, h, :], rhs=Sfb[b][:, h, 0:128], start=False, stop=True), [r_qtt[b], r_Sfb[b]], [r_ps[ob]])
            S.op('pe', lambda e, h=h, b=b, ob=ob, c0=c0: e.matmul(psb[ob][:, c0 + 128:c0 + 256], lhsT=PB[b][:, h, :], rhs=vpt[b][:, h, 0:128], start=True, stop=False), [r_PB[b], r_vpt[b]], [r_ps[ob]])
            S.op('pe', lambda e, h=h, b=b, ob=ob, c0=c0: e.matmul(psb[ob][:, c0 + 128:c0 + 256], lhsT=qtt[b][:, h, :], rhs=sbt[b][:, h, 0:128], start=False, stop=True), [r_qtt[b], r_sbt[b]], [r_ps[ob]])
        state_update(kmt[b], vpt[b], r_kmt[b], r_vpt[b], 0, 4, j, ((0, 0), (1, 0), (6, 64)), b)
        S.op('act', lambda e, b=b: e.copy(out=Sfb[1 - b][:, :, 0:129], in_=Sst[:, :, 0:129]), [r_Sst], [r_Sfb[1 - b]])
        dnv = dn[:].rearrange("p k (a h) -> p k a h", a=2)
        S.op('dve', lambda e, j=j: e.tensor_tensor(out=dnv[:, 0], in0=psb[6][:, 0:16].rearrange("p (a h) -> p a h", a=2), in1=SC[:, j, 2:4, :], op=ALU.mult), [r_ps[6], r_SC[j]], [r_dn])
        S.op('dve', lambda e: e.scalar_tensor_tensor(out=dn[:, 1, :], in0=dn[:, 0, :], scalar=-1.0, in1=dn[:, 0, :], op0=ALU.mult, op1=ALU.max), [r_dn], [r_dn])
        S.op('dve', lambda e: e.tensor_scalar(out=dn[:, 1, :], in0=dn[:, 1, :], scalar1=1.0, scalar2=None, op0=ALU.max), [r_dn], [r_dn])
        S.op('dve', lambda e: e.reciprocal(out=dn[:, 0, :], in_=dn[:, 1, :]), [r_dn], [r_dn])
        S.op('dve', lambda e, j=j: e.tensor_tensor(out=dnv[:, 2], in0=dnv[:, 0], in1=SC[:, j, 2:4, :], op=ALU.mult), [r_dn, r_SC[j]], [r_dn])
        S.op('dve', lambda e, j=j: e.tensor_copy(out=dnv[:, 2, :, 0:4], in_=SC[:, j, 2:4, 0:4]), [r_dn, r_SC[j]], [r_dn])
        for h in range(8):
            ob = 2 + h // 2
            c0 = (h % 2) * 256
            S.op('act', lambda e, h=h, ob=ob, c0=c0, b=b: e.activation(out=Y[b][:, h * 128:(h + 1) * 128], in_=psb[ob][:, c0:c0 + 128], func=AF.Copy, scale=dn[:, 2, h:h + 1]),
                 [r_ps[ob], r_dn], [r_Y[b]])
            S.op('dve', lambda e, h=h, ob=ob, c0=c0, b=b: e.scalar_tensor_tensor(out=Y[b][:, h * 128:(h + 1) * 128], in0=psb[ob][:, c0 + 128:c0 + 256], scalar=dn[:, 2, 8 + h:9 + h],
                                                                             in1=Y[b][:, h * 128:(h + 1) * 128], op0=ALU.mult, op1=ALU.add),
                 [r_ps[ob], r_dn, r_Y[b]], [r_Y[b]])
        S.op('dve', lambda e, b=b: e.tensor_tensor(out=Y[b][:, 512:1024], in0=Y[b][:, 512:1024], in1=got[b][:, 512:1024], op=ALU.mult), [r_Y[b], r_got[b]], [r_Y[b]])
        for h in range(8):
            S.op('dve', lambda e, h=h, b=b: e.bn_stats(out=bst[:, h, :], in_=Y[b][:, h * 128:(h + 1) * 128]), [r_Y[b]], [r_bst])
            S.op('dve', lambda e, h=h: e.bn_aggr(out=mv[:, h, :], in_=bst[:, h, :]), [r_bst], [r_mv])
        S.op('act', lambda e: e.activation(out=mv[:, :, 1], in_=mv[:, :, 1], func=AF.Sqrt, bias=EPS), [r_mv], [r_mv])
        S.op('dve', lambda e: e.reciprocal(out=mv[:, :, 1], in_=mv[:, :, 1]), [r_mv], [r_mv])
        for h in range(8):
            hs = slice(h * 128, (h + 1) * 128)
            S.op('dve', lambda e, h=h, hs=hs, b=b: e.scalar_tensor_tensor(out=Y[b][:, hs], in0=Y[b][:, hs], scalar=mv[:, h, 0:1], in1=gnb[:, hs], op0=ALU.subtract, op1=ALU.mult),
                 [r_Y[b], r_mv, r_gnb], [r_Y[b]])
            if h < 4:
                S.op('dve', lambda e, h=h, hs=hs, b=b: e.scalar_tensor_tensor(out=ybf[:, hs], in0=Y[b][:, hs], scalar=mv[:, h, 1:2], in1=got[b][:, hs], op0=ALU.mult, op1=ALU.mult),
                     [r_Y[b], r_mv, r_got[b]], [r_ybf])
            else:
                S.op('act', lambda e, h=h, hs=hs, b=b: e.activation(out=ybf[:, hs], in_=Y[b][:, hs], func=AF.Copy, scale=mv[:, h, 1:2]), [r_Y[b], r_mv], [r_ybf])
        for c in range(8):
            S.op('pe', lambda e, c=c: e.transpose(out=psbf(7)[:, c * 128:(c + 1) * 128], in_=ybf[:, c * 128:(c + 1) * 128], identity=ident[:]), [r_ybf, r_ident], [r_ps[7]])
        S.op('act', lambda e: e.copy(out=yT[:], in_=psbf(7).rearrange("p (c t) -> p c t", c=8)), [r_ps[7]], [r_yT])
        for half in range(2):
            bank = half
            for c in range(8):
                S.op('pe', lambda e, c=c, half=half, bank=bank: e.matmul(psb[bank][:], lhsT=yT[:, c, :], rhs=Wo[:, c, half * 512:(half + 1) * 512], start=(c == 0), stop=(c == 7)),
                     [r_yT, r_Wo], [r_ps[bank]])
            S.op('dve', lambda e, half=half, bank=bank, b=b: e.tensor_tensor(out=h1[b][:, half * 512:(half + 1) * 512], in0=psb[bank][:], in1=xt2[b][:, half * 512:(half + 1) * 512], op=ALU.add),
                 [r_ps[bank], r_xt2[b]], [r_h1[b]])
        S.dma('sp', lambda e, j=j, b=b: e.dma_start(out=OACC[j * 128:(j + 1) * 128, :], in_=h1[b][:]), [r_h1[b]], [r_OACC], f'st_h1{b}')
        S.op('act', lambda e, b=b: e.activation(out=junk2[:], in_=h1[b][:], func=AF.Square, accum_out=ss2[:]), [r_h1[b]], [r_junk2, r_ss2])
        S.op('act', lambda e: e.activation(out=rstd2[:], in_=ss2[:], func=AF.Sqrt, scale=1.0 / D, bias=EPS), [r_ss2], [r_rstd2])
        S.op('dve', lambda e: e.reciprocal(out=rstd2[:], in_=rstd2[:]), [r_rstd2], [r_rstd2])
        S.op('dve', lambda e, b=b: e.scalar_tensor_tensor(out=u2x[b][:, 0:D], in0=h1[b][:], scalar=rstd2[:], in1=fgb[:], op0=ALU.mult, op1=ALU.mult),
             [r_h1[b], r_rstd2, r_fgb], [r_u2x[b]])
        for c in range(8):
            S.op('pe', lambda e, c=c, b=b: e.transpose(out=psbf(7)[:, c * 128:(c + 1) * 128], in_=u2x[b][:, c * 128:(c + 1) * 128], identity=ident[:]), [r_u2x[b], r_ident], [r_ps[7]])
        S.op('act', lambda e: e.copy(out=u2T[:], in_=psbf(7).rearrange("p (c t) -> p c t", c=8)), [r_ps[7]], [r_u2T])
        for c in range(8):
            S.op('pe', lambda e, c=c: e.matmul(psb[6][:, 16:32], lhsT=u2T[:, c, :], rhs=Wr[:, c, :], start=(c == 0), stop=(c == 7)), [r_u2T, r_Wr], [r_ps[6]])
        S.op('dve', lambda e: e.tensor_reduce(out=sm[:, 0:1], in_=psb[6][:, 16:32], axis=AX.X, op=ALU.max, negate=True), [r_ps[6]], [r_sm])
        S.op('act', lambda e: e.activation(out=ex[:], in_=psb[6][:, 16:32], func=AF.Exp, bias=sm[:, 0:1], accum_out=sm[:, 1:2]), [r_ps[6], r_sm], [r_ex, r_sm])
        S.op('dve', lambda e: e.reciprocal(out=sm[:, 2:3], in_=sm[:, 1:2]), [r_sm], [r_sm])
        if j == 0:
            S.op('dve', lambda e: e.tensor_tensor(out=sm[:, 2:3], in0=sm[:, 2:3], in1=padm[:], op=ALU.mult), [r_sm, r_padm], [r_sm])
        S.op('dve', lambda e, j=j: e.tensor_scalar(out=AFF[:, j, :], in0=ex[:], scalar1=sm[:, 2:3], scalar2=None, op0=ALU.mult), [r_ex, r_sm], [r_AFF[j]])
        S.op('dve', lambda e, j=j, b=b: e.tensor_copy(out=u2x[b][:, D:D + 32].bitcast(F32), in_=AFF[:, j, :]), [r_AFF[j]], [r_u2x[b]])
        S.op('dve', lambda e, j=j, b=b: e.tensor_copy(out=u2x[b][:, D + 32:D + 34].bitcast(I32), in_=tok[:, j:j + 1]), [r_tok], [r_u2x[b]])
        S.dma('sp', lambda e, j=j, b=b: e.dma_start(out=U2X[j * 128:(j + 1) * 128, :], in_=u2x[b][:]), [r_u2x[b]], [r_U2X[j]], f'st_u2{b}')

    S.barrier()
    A4 = Arena(nc, PBASE)
    CMP = A4.t([128, NT, 16], F32); r_CMP = Res("CMP")
    SELb = A4.t([128, NT * 16], BF16); r_SELb = Res("SELb")
    lo = A4.t([128, 16], F32); r_lo = Res("lo")
    hi = A4.t([128, 16], F32); r_hi = Res("hi")
    mid = A4.t([128, 16], F32); r_mid = Res("mid")
    pc = A4.t([128, 16], F32); r_pc = Res("pc")
    mm = A4.t([128, 16], F32); r_mm = Res("mm")
    nm = A4.t([128, 16], F32); r_nm = Res("nm")
    t1 = A4.t([128, 16], F32); r_t1 = Res("t1")
    ones32 = A4.t([128, 128], F32); r_ones32 = Res("ones32")
    onesb = A4.t([128, 128], BF16); r_onesb = Res("onesb")
    sltb = A4.t([128, 128], BF16); r_sltb = Res("sltb")
    onesNT = A4.t([128, NT], F32); r_onesNT = Res("onesNT")
    WIT = A4.t([128, NT, 16], F32); r_WIT = Res("WIT")
    TOT = A4.t([128, 16, NT], F32); r_TOT = Res("TOT")
    INC = A4.t([128, 16, NT], F32); r_INC = Res("INC")
    POSf = A4.t([128, NT, 16], F32); r_POSf = Res("POSf")
    POSi = A4.t([128, NT, 16], I32); r_POSi = Res("POSi")
    A4END = A4.off
    NI = NT * 16
    all_AFF = r_AFF

    S.op('pool', lambda e: e.memset(ones32[:], 1.0), [], [r_ones32])
    S.op('pool', lambda e: e.memset(onesb[:], 1.0), [], [r_onesb])
    S.op('pool', lambda e: e.memset(onesNT[:], 1.0), [], [r_onesNT])
    S.op('pool', lambda e: e.memset(lo[:], 0.0), [], [r_lo])
    S.op('pool', lambda e: e.memset(hi[:], 1.0), [], [r_hi])
    S.dma('sp', lambda e: e.dma_start(out=sltb[:], in_=c_slt[:, :]), [], [r_sltb], 'c5')

    def thr_cmp(th, r_th):
        S.op('dve', lambda e: e.tensor_tensor(out=CMP[:], in0=AFF[:], in1=th[:].unsqueeze(1).to_broadcast([128, NT, 16]), op=ALU.is_ge),
             all_AFF + [r_th], [r_CMP])

    NITER = 34
    for it in range(NITER):
        S.op('dve', lambda e: e.tensor_tensor(out=mid[:], in0=lo[:], in1=hi[:], op=ALU.add), [r_lo, r_hi], [r_mid])
        S.op('dve', lambda e: e.tensor_scalar(out=mid[:], in0=mid[:], scalar1=0.5, scalar2=None, op0=ALU.mult), [r_mid], [r_mid])
        thr_cmp(mid, r_mid)
        S.op('dve', lambda e: e.tensor_reduce(out=pc[:], in_=CMP[:].rearrange("p j e -> p e j"), axis=AX.X, op=ALU.add), [r_CMP], [r_pc])
        S.op('pe', lambda e: e.matmul(psb[0][:, 0:16], lhsT=ones32[:], rhs=pc[:], start=True, stop=True), [r_ones32, r_pc], [r_ps[0]])
        S.op('dve', lambda e: e.tensor_scalar(out=mm[:], in0=psb[0][:, 0:16], scalar1=float(CAP) - 0.5, scalar2=None, op0=ALU.is_ge), [r_ps[0]], [r_mm])
        S.op('dve', lambda e: e.tensor_scalar(out=nm[:], in0=mm[:], scalar1=-1.0, scalar2=1.0, op0=ALU.mult, op1=ALU.add), [r_mm], [r_nm])
        S.op('dve', lambda e: e.tensor_tensor(out=t1[:], in0=mm[:], in1=mid[:], op=ALU.mult), [r_mm, r_mid], [r_t1])
        S.op('dve', lambda e: e.tensor_tensor(out=lo[:], in0=nm[:], in1=lo[:], op=ALU.mult), [r_nm, r_lo], [r_lo])
        S.op('dve', lambda e: e.tensor_tensor(out=lo[:], in0=lo[:], in1=t1[:], op=ALU.add), [r_lo, r_t1], [r_lo])
        S.op('dve', lambda e: e.tensor_tensor(out=t1[:], in0=nm[:], in1=mid[:], op=ALU.mult), [r_nm, r_mid], [r_t1])
        S.op('dve', lambda e: e.tensor_tensor(out=hi[:], in0=mm[:], in1=hi[:], op=ALU.mult), [r_mm, r_hi], [r_hi])
        S.op('dve', lambda e: e.tensor_tensor(out=hi[:], in0=hi[:], in1=t1[:], op=ALU.add), [r_hi, r_t1], [r_hi])
    thr_cmp(lo, r_lo)
    S.op('dve', lambda e: e.tensor_copy(out=SELb[:], in_=CMP[:].rearrange("p j e -> p (j e)")), [r_CMP], [r_SELb])
    nchunk = (NI + 511) // 512
    for ci in range(nchunk):
        n0 = ci * 512
        n1 = min(NI, n0 + 512)
        bank = ci % 3
        S.op('pe', lambda e, n0=n0, n1=n1, bank=bank: e.matmul(psb[bank][:, 0:n1 - n0], lhsT=sltb[:], rhs=SELb[:, n0:n1], start=True, stop=True), [r_sltb, r_SELb], [r_ps[bank]])
        S.op('act', lambda e, n0=n0, n1=n1, bank=bank: e.copy(out=WIT[:].rearrange("p j e -> p (j e)")[:, n0:n1], in_=psb[bank][:, 0:n1 - n0]), [r_ps[bank]], [r_WIT])
        S.op('pe', lambda e, n0=n0, n1=n1, bank=bank: e.matmul(psb[3 + bank][:, 0:n1 - n0], lhsT=onesb[:], rhs=SELb[:, n0:n1], start=True, stop=True), [r_onesb, r_SELb], [r_ps[3 + bank]])
        j0 = n0 // 16
        j1 = n1 // 16
        S.op('act', lambda e, n0=n0, n1=n1, bank=bank, j0=j0, j1=j1: e.copy(out=TOT[:, :, j0:j1], in_=psb[3 + bank][:, 0:n1 - n0].rearrange("p (j e) -> p e j", e=16)),
             [r_ps[3 + bank]], [r_TOT])
    for ee in range(16):
        S.op('dve', lambda e, ee=ee: e.tensor_tensor_scan(out=INC[:, ee, :], data0=onesNT[:], data1=TOT[:, ee, :], initial=0.0, op0=ALU.mult, op1=ALU.add),
             [r_onesNT, r_TOT], [r_INC])
    S.op('dve', lambda e: e.tensor_tensor(out=INC[:], in0=INC[:], in1=TOT[:], op=ALU.subtract), [r_INC, r_TOT], [r_INC])
    S.op('dve', lambda e: e.tensor_tensor(out=POSf[:], in0=WIT[:], in1=INC[:].rearrange("p e j -> p j e"), op=ALU.add), [r_WIT, r_INC], [r_POSf])
    BIG = float(1 << 20)
    S.op('dve', lambda e: e.tensor_scalar(out=POSf[:], in0=POSf[:], scalar1=-BIG, scalar2=None, op0=ALU.add), [r_POSf], [r_POSf])
    S.op('dve', lambda e: e.tensor_tensor(out=POSf[:], in0=POSf[:], in1=CMP[:], op=ALU.mult), [r_POSf, r_CMP], [r_POSf])
    S.op('dve', lambda e: e.tensor_scalar(out=POSf[:], in0=POSf[:], scalar1=BIG, scalar2=None, op0=ALU.add), [r_POSf], [r_POSf])
    S.op('dve', lambda e: e.tensor_copy(out=POSi[:], in_=POSf[:]), [r_POSf], [r_POSi])

    A5 = Arena(nc, A4END)
    u2l = [A5.t([128, XW], BF16) for _ in range(3)]; r_u2l = [Res(f"u2l{i}") for i in range(3)]
    r_XS = [Res(f"XS{e_}") for e_ in range(NEXP)]
    A5END = A5.off
    for e_ in range(NEXP_RUN):
        S.dma('sp', lambda e, e_=e_: e.dma_start(out=XS[e_][:, :], in_=c_xsinit[:, :]), [], [r_XS[e_]], f'xsi{e_}')
    for j in range(NT):
        b = j % 3
        S.dma('sp', lambda e, j=j, b=b: e.dma_start(out=u2l[b][:], in_=U2X[j * 128:(j + 1) * 128, :]), [r_U2X[j]], [r_u2l[b]], f'u2l{b}')
        for e_ in range(NEXP_RUN):
            S.dma('pool', lambda e, j=j, b=b, e_=e_: e.indirect_dma_start(out=XS[e_], out_offset=bass.IndirectOffsetOnAxis(ap=POSi[:, j, e_:e_ + 1], axis=0),
                                                                         in_=u2l[b][:], in_offset=None, bounds_check=breg(e, NSLR - 1), oob_is_err=False),
                  [r_u2l[b], r_POSi, r_XS[e_]], [], f'sc{b}')

    S.barrier()
    A6 = Arena(nc, PBASE0)
    NSTG = 6
    stg = [A6.t([128, 2048], F32) for _ in range(NSTG)]; r_stg = [Res(f"stg{i}") for i in range(NSTG)]
    wgb = [A6.t([128, 8, 256], BF16) for _ in range(2)]; r_wgb = [Res("wgb0"), Res("wgb1")]
    wub = [A6.t([128, 8, 256], BF16) for _ in range(2)]; r_wub = [Res("wub0"), Res("wub1")]
    wdb = A6.t([128, NFC, D], BF16); r_wdb = Res("wdb")
    hidT = A6.t([128, NFC, NSLR], BF16); r_hidT = Res("hidT")
    xsT = A6.t([128, 8, NSLR], BF16); r_xsT = Res("xsT")
    xsl = [A6.t([128, XW], BF16) for _ in range(2)]; r_xsl = [Res("xsl0"), Res("xsl1")]
    meta_sl = A6.t([128, NSL, 64], BF16); r_meta_sl = Res("meta_sl")
    sg = A6.t([128, 512], F32); r_sg = Res("sg")
    yo = [A6.t([128, D], F32) for _ in range(2)]; r_yo = [Res("yo0"), Res("yo1")]
    nstg = 0
    CB = 384 if NSLR % 384 == 0 else 128
    NCB = NSLR // CB
    r_oacc_e = Res("OACCe")
    for e_ in range(NEXP_RUN):
        for s_ in range(NSL):
            b = s_ % 2
            S.dma('sp', lambda e, e_=e_, s_=s_, b=b: e.dma_start(out=xsl[b][:], in_=XS[e_][s_ * 128:(s_ + 1) * 128, :]), [r_XS[e_]], [r_xsl[b]], f'xsl{b}')
            S.op('dve', lambda e, s_=s_, b=b: e.tensor_copy(out=meta_sl[:, s_, :], in_=xsl[b][:, D:D + 64]), [r_xsl[b]], [r_meta_sl])
            for c in range(8):
                S.op('pe', lambda e, c=c, b=b: e.transpose(out=psbf(7)[:, c * 128:(c + 1) * 128], in_=xsl[b][:, c * 128:(c + 1) * 128], identity=ident[:]), [r_xsl[b], r_ident], [r_ps[7]])
            S.op('act', lambda e, s_=s_: e.copy(out=xsT[:, :, s_ * 128:(s_ + 1) * 128], in_=psbf(7).rearrange("p (c t) -> p c t", c=8)), [r_ps[7]], [r_xsT])
        for fb in range(NFC // 2):
            wb_ = fb % 2
            for (wsrc, wdst, r_wd) in ((w_gate, wgb[wb_], r_wgb[wb_]), (w_up, wub[wb_], r_wub[wb_])):
                sb_ = nstg % NSTG; nstg += 1
                S.dma('sp', lambda e, e_=e_, fb=fb, sb_=sb_, wsrc=wsrc: e.dma_start(out=stg[sb_][:].rearrange("p (c n) -> p c n", c=8),
                                                                                  in_=wsrc[e_, :, fb * 256:(fb + 1) * 256].rearrange("(c p) n -> p c n", p=128)), [], [r_stg[sb_]], f'stg{sb_}')
                S.op('dve' if wsrc is w_gate else 'act',
                     (lambda e, sb_=sb_, wdst=wdst: e.tensor_copy(out=wdst[:], in_=stg[sb_][:].rearrange("p (c n) -> p c n", c=8))) if wsrc is w_gate else
                     (lambda e, sb_=sb_, wdst=wdst: e.copy(out=wdst[:], in_=stg[sb_][:].rearrange("p (c n) -> p c n", c=8))),
                     [r_stg[sb_]], [r_wd])
            f2 = fb
            sb_ = nstg % NSTG; nstg += 1
            S.dma('sp', lambda e, e_=e_, f2=f2, sb_=sb_: e.dma_start(out=stg[sb_][:].rearrange("p (a n) -> p a n", a=2),
                                                                   in_=w_down[e_, f2 * 256:(f2 + 1) * 256, :].rearrange("(a p) n -> p a n", p=128)), [], [r_stg[sb_]], f'stg{sb_}')
            S.op('dve' if fb % 2 else 'act',
                 (lambda e, f2=f2, sb_=sb_: e.tensor_copy(out=wdb[:, 2 * f2:2 * f2 + 2, :], in_=stg[sb_][:].rearrange("p (a n) -> p a n", a=2))) if fb % 2 else
                 (lambda e, f2=f2, sb_=sb_: e.copy(out=wdb[:, 2 * f2:2 * f2 + 2, :], in_=stg[sb_][:].rearrange("p (a n) -> p a n", a=2))),
                 [r_stg[sb_]], [r_wdb])
            for fi in range(2):
                fc = fb * 2 + fi
                for cbk in range(NCB):
                    pp = (fc * NCB + cbk) % 3
                    gb, ub = pp * 2, pp * 2 + 1
                    for c in range(8):
                        S.op('pe', lambda e, c=c, gb=gb, wb_=wb_, fi=fi, cbk=cbk: e.matmul(psb[gb][:, 0:CB], lhsT=wgb[wb_][:, c, fi * 128:(fi + 1) * 128],
                                                                                     rhs=xsT[:, c, cbk * CB:(cbk + 1) * CB], start=(c == 0), stop=(c == 7)),
                             [r_wgb[wb_], r_xsT], [r_ps[gb]])
                    for c in range(8):
                        S.op('pe', lambda e, c=c, ub=ub, wb_=wb_, fi=fi, cbk=cbk: e.matmul(psb[ub][:, 0:CB], lhsT=wub[wb_][:, c, fi * 128:(fi + 1) * 128],
                                                                                     rhs=xsT[:, c, cbk * CB:(cbk + 1) * CB], start=(c == 0), stop=(c == 7)),
                             [r_wub[wb_], r_xsT], [r_ps[ub]])
                    S.op('act', lambda e, gb=gb: e.activation(out=sg[:, 0:CB], in_=psb[gb][:, 0:CB], func=AF.Silu), [r_ps[gb]], [r_sg])
                    S.op('dve', lambda e, ub=ub, fc=fc, cbk=cbk: e.tensor_tensor(out=hidT[:, fc, cbk * CB:(cbk + 1) * CB], in0=psb[ub][:, 0:CB], in1=sg[:, 0:CB], op=ALU.mult),
                         [r_ps[ub], r_sg], [r_hidT])
        for s_ in range(NSL):
            yb = s_ % 2
            for half in range(2):
                bank = 6 + half
                for fc in range(NFC):
                    S.op('pe', lambda e, fc=fc, s_=s_, half=half, bank=bank: e.matmul(psb[bank][:], lhsT=hidT[:, fc, s_ * 128:(s_ + 1) * 128], rhs=wdb[:, fc, half * 512:(half + 1) * 512],
                                                                                start=(fc == 0), stop=(fc == NFC - 1)),
                         [r_hidT, r_wdb], [r_ps[bank]])
                gsc = meta_sl[:, s_, 2 * e_:2 * e_ + 2].bitcast(F32)
                S.op('act' if half else 'dve',
                     (lambda e, half=half, bank=bank, yb=yb, gsc=gsc: e.activation(out=yo[yb][:, half * 512:(half + 1) * 512], in_=psb[bank][:], func=AF.Copy, scale=gsc)) if half else
                     (lambda e, half=half, bank=bank, yb=yb, gsc=gsc: e.tensor_scalar(out=yo[yb][:, half * 512:(half + 1) * 512], in0=psb[bank][:], scalar1=gsc, scalar2=None, op0=ALU.mult)),
                     [r_ps[bank], r_meta_sl], [r_yo[yb]])
            tix = meta_sl[:, s_, 32:34].bitcast(I32)
            S.dma('pool', lambda e, yb=yb, tix=tix: e.indirect_dma_start(out=OACC, out_offset=bass.IndirectOffsetOnAxis(ap=tix, axis=0), in_=yo[yb][:], in_offset=None,
                                                                       bounds_check=breg(e, T + NSLR - 1), oob_is_err=True, compute_op=ALU.add),
                  [r_yo[yb], r_meta_sl, r_OACC, r_oacc_e], [r_oacc_e], f'oa{yb}')

    S.barrier()
    A7 = Arena(nc, PBASE0)
    ft = [A7.t([128, D], F32) for _ in range(2)]; r_ft = [Res("ft0"), Res("ft1")]
    fo = [A7.t([128, D], F32) for _ in range(2)]; r_fo = [Res("fo0"), Res("fo1")]
    fj = A7.t([128, D], BF16); r_fj = Res("fj")
    fss = A7.t([128, 2], F32); r_fss = Res("fss")
    fgn = A7.t([128, D], F32); r_fgn = Res("fgn")
    S.dma('sp', lambda e: e.dma_start(out=fgn[:], in_=fin_g.to_broadcast([128, D])), [], [r_fgn], 'w5')
    outs = []
    for j in range(1, NT):
        b = j % 2
        S.dma('sp', lambda e, j=j, b=b: e.dma_start(out=ft[b][:], in_=OACC[j * 128:(j + 1) * 128, :]), [r_oacc_e, r_OACC], [r_ft[b]], f'ft{b}')
        S.op('act', lambda e, b=b: e.activation(out=fj[:], in_=ft[b][:], func=AF.Square, accum_out=fss[:, 0:1]), [r_ft[b]], [r_fj, r_fss])
        S.op('act', lambda e: e.activation(out=fss[:, 1:2], in_=fss[:, 0:1], func=AF.Sqrt, scale=1.0 / D, bias=EPS), [r_fss], [r_fss])
        S.op('dve', lambda e: e.reciprocal(out=fss[:, 1:2], in_=fss[:, 1:2]), [r_fss], [r_fss])
        S.op('dve', lambda e, b=b: e.scalar_tensor_tensor(out=fo[b][:], in0=ft[b][:], scalar=fss[:, 1:2], in1=fgn[:], op0=ALU.mult, op1=ALU.mult),
             [r_ft[b], r_fss, r_fgn], [r_fo[b]])
        outs.append(S.dma('sp', lambda e, j=j, b=b: e.dma_start(out=out[(j - 1) * 128:j * 128, :], in_=fo[b][:]), [r_fo[b]], [Res("o")], f'out{b}'))
    S.final_wait('sp', outs[-2:])
    S.emit(st)
    st.close()
    return nc, dict(T=T, NSLR=NSLR, CAP=CAP, NSL=NSL)


def host_consts(NT, NSLR):
    T = NT * 128
    bf = ml_dtypes.bfloat16
    m = np.arange(128)[:, None]
    l = np.arange(128)[None, :]
    tri_f = (m <= l).astype(np.float32)
    tri_b = (m >= l).astype(np.float32)
    ones = np.ones((128, 128), np.float32)
    rowm = (np.arange(128) >= PADF).astype(np.float32)[:, None]
    c_tri = np.stack([tri_f, tri_b, ones, tri_f * rowm, tri_b * rowm, ones * rowm]).astype(np.float32)
    c_mask = np.stack([tri_f, tri_b]).astype(np.float32)
    c_slt = (m < l).astype(bf)
    pos = (np.arange(T, dtype=np.float32) - np.float32(PADF)).astype(np.float32)
    half = 64
    inv = (np.float32(10000.0) ** (-np.arange(half, dtype=np.float32) / np.float32(half))).astype(np.float32)
    ang = (pos[:, None] * inv[None, :]).astype(np.float32)
    cos = np.cos(ang.astype(np.float64)).astype(np.float32)
    sin = np.sin(ang.astype(np.float64)).astype(np.float32)
    c_cs = np.concatenate([np.tile(cos, (1, 4)), np.tile(sin, (1, 4))], axis=1).astype(np.float32)
    c_tok = (np.arange(128)[:, None] + 128 * np.arange(NT)[None, :]).astype(np.int32)
    c_padm = rowm.astype(np.float32)
    xs = np.zeros((NSLR, XW), dtype=bf)
    tokid = (T + np.arange(NSLR)).astype(np.int32)
    xs_i32 = xs.view(np.int32).reshape(NSLR, XW // 2)
    xs_i32[:, (D + 32) // 2] = tokid
    return dict(c_ident=np.eye(128).astype(bf), c_tri=c_tri, c_mask=c_mask, c_slt=c_slt, c_cs=c_cs,
                c_tok=c_tok, c_padm=c_padm, c_xsinit=xs)


def core_inputs(b, x, meta_tokens, mix_norm_g, w_in, b_gates, conv_w, conv_b, ret_decay_logit, ret_gn_g,
                mlstm_gn_g, w_out, ffn_norm_g, w_router, w_gate, w_up, w_down, final_norm_g, consts):
    f = np.float32
    d = dict(
        x=np.ascontiguousarray(x[b], dtype=f), meta=np.ascontiguousarray(meta_tokens, dtype=f),
        w_in=np.ascontiguousarray(w_in[0], dtype=f), mix_g=np.ascontiguousarray(mix_norm_g[0][None, :], dtype=f),
        b_gates=np.ascontiguousarray(b_gates[0][None, :], dtype=f),
        conv_w=np.ascontiguousarray(conv_w[0], dtype=f), conv_b=np.ascontiguousarray(conv_b[0][None, :], dtype=f),
        rlogit=np.ascontiguousarray(ret_decay_logit[0].reshape(1, 8), dtype=f),
        gn_g=np.ascontiguousarray(np.concatenate([ret_gn_g[0], mlstm_gn_g[0]])[None, :], dtype=f),
        w_out=np.ascontiguousarray(w_out[0], dtype=f), ffn_g=np.ascontiguousarray(ffn_norm_g[0][None, :], dtype=f),
        w_router=np.ascontiguousarray(w_router[0], dtype=f),
        w_gate=np.ascontiguousarray(w_gate[0], dtype=f), w_up=np.ascontiguousarray(w_up[0], dtype=f),
        w_down=np.ascontiguousarray(w_down[0], dtype=f), fin_g=np.ascontiguousarray(final_norm_g[None, :], dtype=f))
    d.update(consts)
    return d


_CACHE = {}


def kernel(**inputs):
    x = np.asarray(inputs['x'])
    B, SEQ, _ = x.shape
    NT = SEQ // 128 + 1
    if NT not in _CACHE:
        _CACHE[NT] = build(NT)
    nc, info = _CACHE[NT]
    consts = host_consts(NT, info['NSLR'])
    args = {k: np.asarray(v) for k, v in inputs.items()}
    in_maps = []
    for c in range(8):
        b = (c // 2) % B
        in_maps.append(core_inputs(b, consts=consts, **args))
    res = run_bass_kernel_spmd(nc, in_maps, core_ids=list(range(8)))
    outs = [np.asarray(res.results[2 * b]["out"]) for b in range(B)]
    return np.stack(outs, axis=0).astype(np.float32)
```

```python
import math
import numpy as np
import ml_dtypes
from contextlib import ExitStack
import concourse.bass as bass
import concourse.mybir as mybir
from concourse.bass_utils import run_bass_kernel_spmd

F32 = mybir.dt.float32
BF16 = mybir.dt.bfloat16
I32 = mybir.dt.int32
ALU = mybir.AluOpType
AF = mybir.ActivationFunctionType
AX = mybir.AxisListType

ENGS = ['pe', 'act', 'dve', 'pool', 'sp']
EPOCH = 12000


class Res:
    __slots__ = ('name', 'w', 'rs')

    def __init__(self, name):
        self.name = name
        self.w = None
        self.rs = []


class Ins:
    __slots__ = ('eng', 'emit', 'deps', 'pos', 'needed', 'is_dma', 'semkey', 'dk', 'dv',
                 'waits', 'cbase', 'ord')


class Sched:
    def __init__(self, nc):
        self.nc = nc
        self.streams = {e: [] for e in ENGS}
        self.all = []
        self.finals = []
        self.pending = {e: [] for e in ENGS}

    def op(self, eng, emit, reads=(), writes=(), after=()):
        return self._add(eng, emit, reads, writes, False, None, after)

    def dma(self, queue, emit, reads=(), writes=(), semkey=None, after=()):
        return self._add(queue, emit, reads, writes, True, semkey, after)

    def final_wait(self, eng, ins_list):
        self.finals.append((eng, list(ins_list)))

    def barrier(self):
        lasts = []
        for e in ENGS:
            st = self.streams[e]
            for ins in reversed(st):
                if not ins.is_dma:
                    lasts.append(ins)
                    break
        dl = {}
        for i in self.all[getattr(self, '_bar_idx', 0):]:
            if i.is_dma:
                dl[i.semkey] = i
        dmas = list(dl.values())
        self._bar_idx = len(self.all)
        for e in ENGS:
            self.pending[e] = self.pending[e] + lasts + dmas

    def _add(self, eng, emit, reads, writes, is_dma, semkey, after=()):
        ins = Ins()
        ins.eng = eng
        ins.emit = emit
        ins.is_dma = is_dma
        ins.needed = is_dma
        ins.semkey = semkey
        deps = [(0, X) for X in after]
        if self.pending[eng]:
            deps += [(0, X) for X in self.pending[eng]]
            self.pending[eng] = []
        for r in reads:
            if r.w is not None:
                deps.append((0, r.w))
            r.rs.append(ins)
        for r in writes:
            if r.w is not None and r.w is not ins:
                deps.append((1, r.w))
            for q in r.rs:
                if q is not ins:
                    deps.append((2, q))
            r.w = ins
            r.rs = []
        ins.deps = deps
        ins.pos = len(self.streams[eng])
        self.streams[eng].append(ins)
        self.all.append(ins)
        return ins

    def finalize(self):
        eclock = {e: {} for e in ENGS}
        dcnt = {}
        for ins in self.all:
            ec = eclock[ins.eng]
            waits = []
            for kind, X in ins.deps:
                if X is ins:
                    continue
                if (not X.is_dma) and (not ins.is_dma) and X.eng == ins.eng:
                    if ins.eng == 'pe' or (kind != 0 and ins.eng != 'pool'):
                        continue
                if ec.get(X.dk, 0) >= X.dv:
                    continue
                waits.append(X)
                X.needed = True
                nec = dict(ec)
                for k, v in X.cbase.items():
                    if nec.get(k, 0) < v:
                        nec[k] = v
                if nec.get(X.dk, 0) < X.dv:
                    nec[X.dk] = X.dv
                ec = nec
                eclock[ins.eng] = ec
            ins.waits = waits
            if ins.is_dma:
                c = dcnt.get(ins.semkey, 0) + 1
                dcnt[ins.semkey] = c
                ins.dk = ('d', ins.semkey)
                ins.dv = c
            else:
                ins.dk = ins.eng
                ins.dv = ins.pos + 1
            ins.cbase = ec
        self.dma_keys = list(dcnt.keys())
        self.n_ord = {}
        for e in ENGS:
            o = 0
            for ins in self.streams[e]:
                if ins.needed and not ins.is_dma:
                    o += 1
                    ins.ord = o
            self.n_ord[e] = o

    def emit(self, stack):
        nc = self.nc
        self.finalize()
        csem = {}
        for e in ENGS:
            n = (self.n_ord[e] + EPOCH - 1) // EPOCH
            csem[e] = [stack.enter_context(nc.semaphore(f"c_{e}_{i}")) for i in range(n)]
        dsem = {}
        for i, k in enumerate(self.dma_keys):
            dsem[k] = stack.enter_context(nc.semaphore(f"d_{i}"))
        self.nsem = sum(len(v) for v in csem.values()) + len(dsem)

        def semval(X):
            if X.is_dma:
                return dsem[X.semkey], X.dv * 16
            o = X.ord - 1
            return csem[X.eng][o // EPOCH], (o % EPOCH) + 1

        def run(ename, eng):
            for ins in self.streams[ename]:
                for X in ins.waits:
                    s, v = semval(X)
                    eng.wait_ge(s, v)
                bi = ins.emit(eng)
                if ins.is_dma:
                    bi.then_inc(dsem[ins.semkey], 16)
                elif ins.needed:
                    s, v = semval(ins)
                    bi.then_inc(s, 1)
            for fe, lst in self.finals:
                if fe == ename:
                    for X in lst:
                        s, v = semval(X)
                        eng.wait_ge(s, v)

        block = stack.enter_context(nc.Block())

        @block.tensor
        def _(eng):
            run('pe', eng)

        @block.scalar
        def _(eng):
            run('act', eng)

        @block.vector
        def _(eng):
            run('dve', eng)

        @block.gpsimd
        def _(eng):
            run('pool', eng)

        @block.sync
        def _(eng):
            run('sp', eng)


D = 1024
PCOLS = 4112
DFF = 2816
NFC = DFF // 128
NEXP = 16
XW = 1088
EPS = 1e-6
PADF = 112
QSCALE = 128 ** -0.5


SB_BASE = 17408
SB_LIMIT = 229376


class Arena:
    def __init__(self, nc, base=0):
        self.nc = nc
        self.off = base
        self.n = 0

    def t(self, shape, dt, name=None):
        sz = {F32: 4, BF16: 2, I32: 4}[dt]
        per = 1
        for s in shape[1:]:
            per *= s
        nbytes = per * sz
        self.off = (self.off + 63) // 64 * 64
        Arena.cnt = getattr(Arena, 'cnt', 0) + 1
        h = self.nc.alloc_sbuf_tensor_at(name or f"t{Arena.cnt}", list(shape), dt, offset=self.off)
        self.off += nbytes
        assert self.off <= SB_LIMIT, f"SBUF overflow {self.off}"
        return h


def build(NT, NEXP_RUN=NEXP, dbg=False):
    T = NT * 128
    NREAL = T - PADF
    CAP = 2 * NREAL // 16
    NSL = (CAP + 127) // 128 + (1 if CAP % 128 == 0 else 0)
    NSLR = NSL * 128
    SEQ = T - 128
    nc = bass.Bass("TRN2", target_bir_lowering=False)

    def din(name, shape, dt=F32):
        return nc.dram_tensor(name, list(shape), dt, kind="ExternalInput").ap()

    def dscr(name, shape, dt):
        return nc.dram_tensor(name, list(shape), dt, kind=("ExternalOutput" if dbg else "Internal")).ap()

    x = din("x", [SEQ, D])
    meta = din("meta", [16, D])
    w_in = din("w_in", [D, PCOLS])
    mixg = din("mix_g", [1, D])
    b_gates = din("b_gates", [1, 16])
    conv_w = din("conv_w", [5, 1024])
    conv_b = din("conv_b", [1, 1024])
    rlogit = din("rlogit", [1, 8])
    gn_g = din("gn_g", [1, 1024])
    w_out = din("w_out", [D, D])
    ffn_g = din("ffn_g", [1, D])
    w_router = din("w_router", [D, 16])
    w_gate = din("w_gate", [NEXP, D, DFF])
    w_up = din("w_up", [NEXP, D, DFF])
    w_down = din("w_down", [NEXP, DFF, D])
    fin_g = din("fin_g", [1, D])
    c_ident = din("c_ident", [128, 128], BF16)
    c_tri = din("c_tri", [6, 128, 128])
    c_mask = din("c_mask", [2, 128, 128])
    c_slt = din("c_slt", [128, 128], BF16)
    c_cs = din("c_cs", [T, 512])
    c_tok = din("c_tok", [128, NT], I32)
    c_padm = din("c_padm", [128, 1])
    c_xsinit = din("c_xsinit", [NSLR, XW], BF16)

    out = nc.dram_tensor("out", [SEQ, D], F32, kind="ExternalOutput").ap()

    QT = dscr("QT", [NT, 128, 8, 128], BF16)
    KT = dscr("KT", [NT, 128, 8, 128], BF16)
    KM = dscr("KM", [NT, 128, 8, 128], BF16)
    VP = dscr("VP", [NT, 128, 8, 130], BF16)
    GO = dscr("GO", [NT, 128, 1024], F32)
    SBS = dscr("SBS", [NT, 128, 8, 130], BF16)
    U2X = dscr("U2X", [T, XW], BF16)
    XS = [dscr(f"XS{i}", [NSLR, XW], BF16) for i in range(NEXP)]
    OACC = dscr("OACC", [T + NSLR, D], F32)

    S = Sched(nc)
    st = ExitStack()
    _regs = {}

    def breg(e, v):
        if v not in _regs:
            _regs[v] = e.to_reg(v)
        return _regs[v]

    psb = [st.enter_context(nc.psum_tensor(f"ps{i}", [128, 512], F32)) for i in range(8)]
    r_ps = [Res(f"ps{i}") for i in range(8)]

    def psbf(i):
        return psb[i][:].bitcast(BF16)

    P = Arena(nc, SB_BASE)
    ident = P.t([128, 128], BF16); r_ident = Res("ident")
    tri = P.t([128, 6, 128], F32); r_tri = Res("tri")
    maskfb = P.t([128, 2, 128], F32); r_mask = Res("mask")
    padm = P.t([128, 1], F32); r_padm = Res("padm")
    tok = P.t([128, NT], I32); r_tok = Res("tok")
    PBASE0 = P.off
    SC = P.t([128, NT, 6, 8], F32); r_SC = [Res(f"SC{j}") for j in range(NT)]
    AFF = P.t([128, NT, 16], F32); r_AFF = [Res(f"AFF{j}") for j in range(NT)]
    PBASE = P.off

    def ld(dst, src, res, key, q='sp'):
        return S.dma(q, lambda e: e.dma_start(out=dst, in_=src), [], [res], key)

    ld(ident[:], c_ident[:, :], r_ident, 'c0')
    ld(tri[:], c_tri.rearrange("k m l -> m k l"), r_tri, 'c1')
    ld(maskfb[:], c_mask.rearrange("k m l -> m k l"), r_mask, 'c2')
    ld(padm[:], c_padm[:, :], r_padm, 'c3')
    ld(tok[:], c_tok[:, :], r_tok, 'c4')

    A1 = Arena(nc, PBASE)
    Wb = A1.t([128, 8, PCOLS], BF16); r_Wb = Res("Wb")
    wstg = [A1.t([128, 1028], F32) for _ in range(2)]; r_wstg = [Res("wstg0"), Res("wstg1")]
    mixgT = A1.t([128, 8], F32); r_mixgT = Res("mixgT")
    xt = [A1.t([128, D], F32) for _ in range(2)]; r_xt = [Res("xt0"), Res("xt1")]
    junk = A1.t([128, D], BF16); r_junk = Res("junk")
    ss = A1.t([128, 1], F32); r_ss = Res("ss")
    rstd = A1.t([128, 1], F32); r_rstd = Res("rstd")
    xn = [A1.t([128, D], BF16) for _ in range(2)]; r_xn = [Res("xn0"), Res("xn1")]
    UW = [A1.t([128, 8, 384], BF16) for _ in range(2)]; r_UW = [Res("UW0"), Res("UW1")]
    cs = [A1.t([128, 512], F32) for _ in range(2)]; r_cs = [Res("cs0"), Res("cs1")]
    qk32 = [A1.t([128, 2, 512], F32) for _ in range(2)]; r_qk32 = [Res("qk320"), Res("qk321")]
    rt = [A1.t([128, 4, 256], F32) for _ in range(2)]; r_rt = [Res("rt0"), Res("rt1")]
    qkr = [A1.t([128, 2, 512], BF16) for _ in range(2)]; r_qkr = [Res("qkr0"), Res("qkr1")]
    qkT = [A1.t([128, 8, 128], BF16) for _ in range(2)]; r_qkT = [Res("qkT0"), Res("qkT1")]
    vp = [A1.t([128, 8, 130], BF16) for _ in range(2)]; r_vp = [Res("vp0"), Res("vp1")]
    go = [A1.t([128, 1024], F32) for _ in range(2)]; r_go = [Res("go0"), Res("go1")]
    G32 = A1.t([128, 32], F32); r_G32 = Res("G32")
    BASE = A1.t([128, 32], F32); r_BASE = Res("BASE")
    spx = A1.t([128, 16], F32); r_spx = Res("spx")
    A16 = A1.t([128, 16], F32); r_A16 = Res("A16")
    Xc = [A1.t([128, 8, 132], F32) for _ in range(2)]; r_Xc = [Res("Xc0"), Res("Xc1")]
    cw = A1.t([128, 5, 8], F32); r_cw = Res("cw")
    cb = A1.t([128, 8], F32); r_cb = Res("cb")
    cacc = [A1.t([128, 8, 128], F32) for _ in range(2)]; r_cacc = [Res("cacc0"), Res("cacc1")]
    r_cacc2 = [Res("cacc20"), Res("cacc21")]
    mqkT = [A1.t([128, 8, 128], BF16) for _ in range(2)]; r_mqkT = [Res("mqkT0"), Res("mqkT1")]
    kmm = [A1.t([128, 4, 128], BF16) for _ in range(2)]; r_kmm = [Res("kmm0"), Res("kmm1")]
    ctmp_t = A1.t([128, 4, 128], F32); r_ctmp = Res("ctmp")

    S.dma('sp', lambda e: e.dma_start(out=mixgT[:], in_=mixg.rearrange("o (c p) -> p (o c)", p=128),
                                      allow_slow_non_contiguous=True), [], [r_mixgT], 'w0')
    k = 0
    for c in range(8):
        for q4 in range(4):
            b = k % 2
            c0 = q4 * 1028
            S.dma('sp', lambda e, b=b, c=c, c0=c0: e.dma_start(out=wstg[b][:], in_=w_in[c * 128:(c + 1) * 128, c0:c0 + 1028]),
                  [], [r_wstg[b]], f'wst{b}')
            eng = 'dve' if k % 2 == 0 else 'pool'
            S.op(eng, lambda e, b=b, c=c, c0=c0: e.tensor_scalar(out=Wb[:, c, c0:c0 + 1028], in0=wstg[b][:], scalar1=mixgT[:, c:c + 1],
                                                                 scalar2=None, op0=ALU.mult),
                 [r_wstg[b], r_mixgT], [r_Wb])
            k += 1
    S.op('pool', lambda e: e.memset(BASE[:], 0.0), [], [r_BASE])
    S.op('pool', lambda e: e.memset(G32[:], 0.0), [], [r_G32])
    for (c0, src) in ((4, b_gates[:, 0:4]), (12, b_gates[:, 4:8]), (20, b_gates[:, 8:12]), (28, b_gates[:, 12:16]),
                      (16, rlogit[:, 0:4]), (24, rlogit[:, 4:8])):
        S.dma('sp', lambda e, c0=c0, src=src: e.dma_start(out=BASE[:, c0:c0 + 4], in_=src.to_broadcast([128, 4])), [], [r_BASE], 'w1')
    for t in range(5):
        S.dma('sp', lambda e, t=t: e.dma_start(out=cw[:, t, :], in_=conv_w[t:t + 1, :].rearrange("o (g p) -> p (o g)", p=128), allow_slow_non_contiguous=True), [], [r_cw], 'w2a')
    S.dma('sp', lambda e: e.dma_start(out=cb[:], in_=conv_b.rearrange("o (g p) -> p (o g)", p=128), allow_slow_non_contiguous=True), [], [r_cb], 'w2b')
    for b in range(2):
        S.op('pool', lambda e, b=b: e.memset(UW[b][:], 0.0), [], [r_UW[b]])
        S.op('pool', lambda e, b=b: e.memset(vp[b][:], 0.0), [], [r_vp[b]])
        S.op('pool', lambda e, b=b: e.memset(vp[b][:, :, 128:129], 1.0), [], [r_vp[b]])
    S.op('pool', lambda e: e.memset(xt[0][:], 0.0), [], [r_xt[0]])

    r_QT = [Res(f"QT{j}") for j in range(NT)]
    r_KT = [Res(f"KT{j}") for j in range(NT)]
    r_KM = [Res(f"KM{j}") for j in range(NT)]
    r_VP = [Res(f"VP{j}") for j in range(NT)]
    r_GO = [Res(f"GO{j}") for j in range(NT)]
    r_SBS = [Res(f"SBS{j}") for j in range(NT)]

    def x_rows(j):
        return x[(j - 1) * 128:j * 128, :]

    def p1_front(j):
        b = j % 2
        if j < NT:
            if j == 0:
                S.dma('sp', lambda e: e.dma_start(out=xt[0][PADF:128, :], in_=meta[:, :]), [], [r_xt[0]], 'xt0')
            else:
                S.dma('sp', lambda e, j=j, b=b: e.dma_start(out=xt[b][:], in_=x_rows(j)), [], [r_xt[b]], f'xt{b}')
            S.dma('sp', lambda e, j=j, b=b: e.dma_start(out=cs[b][:], in_=c_cs[j * 128:(j + 1) * 128, :]), [], [r_cs[b]], f'cs{b}')
            S.op('act', lambda e, b=b: e.activation(out=junk[:], in_=xt[b][:], func=AF.Square, accum_out=ss[:]), [r_xt[b]], [r_junk, r_ss])
            S.op('act', lambda e: e.activation(out=rstd[:], in_=ss[:], func=AF.Sqrt, scale=1.0 / D, bias=EPS), [r_ss], [r_rstd])
            S.op('dve', lambda e: e.reciprocal(out=rstd[:], in_=rstd[:]), [r_rstd], [r_rstd])
            S.op('act', lambda e, b=b: e.activation(out=xn[b][:], in_=xt[b][:], func=AF.Copy, scale=rstd[:]), [r_xt[b], r_rstd], [r_xn[b]])
            for c in range(8):
                S.op('pe', lambda e, c=c, b=b: e.transpose(out=psbf(0)[:, c * 128:(c + 1) * 128], in_=xn[b][:, c * 128:(c + 1) * 128], identity=ident[:]),
                     [r_xn[b], r_ident], [r_ps[0]])
        if j >= 1:
            S.op('dve', lambda e, b=b: e.tensor_copy(out=UW[b][:, :, 0:256], in_=UW[1 - b][:, :, 128:384]), [r_UW[1 - b]], [r_UW[b]])
        if j < NT:
            S.op('dve', lambda e, b=b: e.tensor_copy(out=UW[b][:, :, 256:384], in_=psbf(0).rearrange("p (c t) -> p c t", c=8)),
                 [r_ps[0]], [r_UW[b]])
        else:
            S.op('dve', lambda e, b=b: e.memset(UW[b][:, :, 256:384], 0.0), [], [r_UW[b]])

        if j < NT:
            def uT(c, b=b):
                return UW[b][:, c, 256:384]
            def tok_proj(bank, col0, ncol, b=b):
                for c in range(8):
                    lhs = UW[b][:, c, 256:384]
                    S.op('pe', lambda e, c=c, lhs=lhs: e.matmul(psb[bank][:, 0:ncol], lhsT=lhs, rhs=Wb[:, c, col0:col0 + ncol], start=(c == 0), stop=(c == 7)),
                         [r_UW[b], r_Wb], [r_ps[bank]])
            tok_proj(1, 0, 512)
            S.op('act', lambda e, b=b: e.copy(out=qk32[b][:, 0, :], in_=psb[1][:]), [r_ps[1]], [r_qk32[b]])
            tok_proj(2, 512, 512)
            S.op('act', lambda e, b=b: e.copy(out=qk32[b][:, 1, :], in_=psb[2][:]), [r_ps[2]], [r_qk32[b]])
            tok_proj(1, 1024, 512)
            S.op('act', lambda e, b=b: e.copy(out=vp[b][:, 0:4, 0:128], in_=psb[1][:].rearrange("p (h d) -> p h d", h=4)), [r_ps[1]], [r_vp[b]])
            tok_proj(2, 1536, 512)
            S.op('act', lambda e, b=b: e.activation(out=go[b][:, 0:512], in_=psb[2][:], func=AF.Silu), [r_ps[2]], [r_go[b]])
            tok_proj(1, 3072, 512)
            S.op('act', lambda e, b=b: e.copy(out=vp[b][:, 4:8, 0:128], in_=psb[1][:].rearrange("p (h d) -> p h d", h=4)), [r_ps[1]], [r_vp[b]])
            tok_proj(2, 3584, 512)
            S.op('act', lambda e, b=b: e.activation(out=go[b][:, 512:1024], in_=psb[2][:], func=AF.Sigmoid), [r_ps[2]], [r_go[b]])
            S.dma('sp', lambda e, j=j, b=b: e.dma_start(out=VP[j], in_=vp[b][:]), [r_vp[b]], [r_VP[j]], f'st_vp{b}')
            S.dma('sp', lambda e, j=j, b=b: e.dma_start(out=GO[j], in_=go[b][:]), [r_go[b]], [r_GO[j]], f'st_go{b}')
            tok_proj(6, 4096, 16)
            g32v = G32[:].rearrange("p (a h) -> p a h", a=4)
            basev = BASE[:].rearrange("p (a h) -> p a h", a=4)
            S.op('dve', lambda e: e.tensor_tensor(out=g32v[:, :, 4:8], in0=psb[6][:, 0:16].rearrange("p (a h) -> p a h", a=4), in1=basev[:, :, 4:8], op=ALU.add),
                 [r_ps[6], r_BASE], [r_G32])
            if j == 0:
                S.op('dve', lambda e: e.tensor_copy(out=g32v[:, :, 0:4], in_=basev[:, :, 0:4]), [r_BASE], [r_G32])
            S.op('act', lambda e: e.activation(out=spx[:], in_=G32[:, 16:32], func=AF.Exp, scale=-1.0), [r_G32], [r_spx])
            S.op('act', lambda e: e.activation(out=spx[:], in_=spx[:], func=AF.Ln, bias=1.0), [r_spx], [r_spx])
            k0 = 3 if j == 0 else 0
            for q in range(3):
                S.op('pe', lambda e, q=q, k0=k0: e.matmul(psb[6][:, 16 + q * 16:32 + q * 16], lhsT=tri[:, k0 + q, :], rhs=spx[:], start=True, stop=True),
                     [r_tri, r_spx], [r_ps[6]])
            cumv = psb[6][:, 16:64].rearrange("p (a b) -> p a b", b=24)[:, :, 0:8]
            totv = psb[6][:, 48:64].rearrange("p (a h) -> p a h", a=2)
            S.op('dve', lambda e: e.tensor_tensor(out=A16[:].rearrange("p (a h) -> p a h", a=2), in0=cumv, in1=G32[:, 0:16].rearrange("p (a h) -> p a h", a=2), op=ALU.add),
                 [r_ps[6], r_G32], [r_A16])
            S.op('act', lambda e, j=j: e.activation(out=SC[:, j, 0:2, :], in_=A16[:].rearrange("p (a h) -> p a h", a=2), func=AF.Exp), [r_A16], [r_SC[j]])
            S.op('act', lambda e, j=j: e.activation(out=SC[:, j, 2:4, :], in_=cumv, func=AF.Exp, scale=-1.0, bias=math.log(QSCALE)), [r_ps[6]], [r_SC[j]])
            S.op('act', lambda e, j=j: e.activation(out=SC[:, j, 4:6, :], in_=totv, func=AF.Exp, scale=-1.0), [r_ps[6]], [r_SC[j]])

        if j >= 1:
            jj = j - 1
            for g in range(8):
                bank = 3 if g < 3 else (4 if g < 6 else 7)
                off = (g % 3) * 132
                col0 = 2048 + g * 128
                for c in range(8):
                    S.op('pe', lambda e, c=c, bank=bank, off=off, col0=col0, b=b: e.matmul(psb[bank][:, off:off + 132], lhsT=Wb[:, c, col0:col0 + 128],
                                                                                      rhs=UW[b][:, c, 126:258], start=(c == 0), stop=(c == 7)),
                         [r_UW[b], r_Wb], [r_ps[bank]])
            for bank, g0, ng in ((3, 0, 3), (4, 3, 3), (7, 6, 2)):
                S.op('act', lambda e, bank=bank, g0=g0, ng=ng, b=b: e.copy(out=Xc[b][:, g0:g0 + ng, :], in_=psb[bank][:, 0:ng * 132].rearrange("p (g t) -> p g t", g=ng)),
                     [r_ps[bank]], [r_Xc[b]])

    def p1_back(j):
        b = j % 2
        if j < NT:
            def rot(b=b):
                xv = qk32[b][:].rearrange("p a (h t d) -> p (a h) t d", h=4, t=2)
                ov = qkr[b][:].rearrange("p a (h t d) -> p (a h) t d", h=4, t=2)
                x1 = xv[:, :, 0, :]; x2 = xv[:, :, 1, :]
                cosv = cs[b][:, 0:256].rearrange("p (h d) -> p h d", h=4)
                sinv = cs[b][:, 256:512].rearrange("p (h d) -> p h d", h=4)
                tv = rt[b][:].rearrange("p k (a d) -> p k a d", a=4)
                for a in range(2):
                    X1 = x1[:, a * 4:(a + 1) * 4, :]; X2 = x2[:, a * 4:(a + 1) * 4, :]
                    O1 = ov[:, a * 4:(a + 1) * 4, 0, :]; O2 = ov[:, a * 4:(a + 1) * 4, 1, :]
                    S.op('dve', lambda e, X1=X1: e.tensor_tensor(out=tv[:, 0], in0=X1, in1=cosv, op=ALU.mult), [r_qk32[b], r_cs[b]], [r_rt[b]])
                    S.op('dve', lambda e, X2=X2: e.tensor_tensor(out=tv[:, 1], in0=X2, in1=sinv, op=ALU.mult), [r_qk32[b], r_cs[b]], [r_rt[b]])
                    S.op('dve', lambda e, X2=X2: e.tensor_tensor(out=tv[:, 2], in0=X2, in1=cosv, op=ALU.mult), [r_qk32[b], r_cs[b]], [r_rt[b]])
                    S.op('dve', lambda e, X1=X1: e.tensor_tensor(out=tv[:, 3], in0=X1, in1=sinv, op=ALU.mult), [r_qk32[b], r_cs[b]], [r_rt[b]])
                    S.op('dve', lambda e, O1=O1: e.tensor_tensor(out=O1, in0=tv[:, 0], in1=tv[:, 1], op=ALU.subtract), [r_rt[b]], [r_qkr[b]])
                    S.op('dve', lambda e, O2=O2: e.tensor_tensor(out=O2, in0=tv[:, 2], in1=tv[:, 3], op=ALU.add), [r_rt[b]], [r_qkr[b]])
            rot()
            for a in range(2):
                for h in range(4):
                    i = a * 4 + h
                    S.op('pe', lambda e, a=a, h=h, i=i, b=b: e.transpose(out=psbf(5)[:, i * 128:(i + 1) * 128], in_=qkr[b][:, a, h * 128:(h + 1) * 128], identity=ident[:]),
                         [r_qkr[b], r_ident], [r_ps[5]])
            S.op('act', lambda e, b=b: e.copy(out=qkT[b][:], in_=psbf(5).rearrange("p (i t) -> p i t", i=8)), [r_ps[5]], [r_qkT[b]])
            S.dma('sp', lambda e, j=j, b=b: e.dma_start(out=QT[j, :, 0:4, :], in_=qkT[b][:, 0:4, :]), [r_qkT[b]], [r_QT[j]], f'st_qkT{b}')
            S.dma('sp', lambda e, j=j, b=b: e.dma_start(out=KT[j, :, 0:4, :], in_=qkT[b][:, 4:8, :]), [r_qkT[b]], [r_KT[j]], f'st_qkT{b}')
            S.dma('sp', lambda e, j=j, b=b: e.dma_start(out=KM[j, :, 0:4, :], in_=qkr[b][:, 1, :].rearrange("p (h d) -> p h d", h=4)), [r_qkr[b]], [r_KM[j]], f'st_qkr{b}')
        if j >= 1:
            jj = j - 1
            ctmp = ctmp_t[:]
            for t in range(5):
                wb_t = cw[:, t, 0:4].unsqueeze(2).to_broadcast([128, 4, 128])
                if t == 0:
                    S.op('pool', lambda e, wb_t=wb_t, b=b: e.tensor_tensor(out=cacc[b][:, 0:4, :], in0=Xc[b][:, 0:4, 0:128], in1=wb_t, op=ALU.mult), [r_Xc[b], r_cw], [r_cacc[b]])
                else:
                    S.op('pool', lambda e, wb_t=wb_t, t=t, b=b: e.tensor_tensor(out=ctmp, in0=Xc[b][:, 0:4, t:t + 128], in1=wb_t, op=ALU.mult), [r_Xc[b], r_cw], [r_ctmp])
                    S.op('pool', lambda e, b=b: e.tensor_tensor(out=cacc[b][:, 0:4, :], in0=cacc[b][:, 0:4, :], in1=ctmp, op=ALU.add), [r_cacc[b], r_ctmp], [r_cacc[b]])
            S.op('pool', lambda e, b=b: e.tensor_tensor(out=cacc[b][:, 0:4, :], in0=cacc[b][:, 0:4, :], in1=cb[:, 0:4].unsqueeze(2).to_broadcast([128, 4, 128]), op=ALU.add), [r_cacc[b], r_cb], [r_cacc[b]])
            for g in range(4, 8):
                S.op('dve', lambda e, g=g, b=b: e.tensor_scalar(out=cacc[b][:, g, :], in0=Xc[b][:, g, 0:128], scalar1=cw[:, 0, g:g + 1], scalar2=cb[:, g:g + 1], op0=ALU.mult, op1=ALU.add),
                     [r_Xc[b], r_cw, r_cb], [r_cacc2[b]])
                for t in range(1, 5):
                    S.op('dve', lambda e, g=g, t=t, b=b: e.scalar_tensor_tensor(out=cacc[b][:, g, :], in0=Xc[b][:, g, t:t + 128], scalar=cw[:, t, g:g + 1], in1=cacc[b][:, g, :],
                                                                             op0=ALU.mult, op1=ALU.add),
                         [r_Xc[b], r_cw, r_cacc2[b]], [r_cacc2[b]])
            S.op('act', lambda e, b=b: e.activation(out=mqkT[b][:], in_=cacc[b][:], func=AF.Silu), [r_cacc[b], r_cacc2[b]], [r_mqkT[b]])
            S.dma('sp', lambda e, jj=jj, b=b: e.dma_start(out=QT[jj, :, 4:8, :], in_=mqkT[b][:, 0:4, :]), [r_mqkT[b]], [r_QT[jj]], f'st_mqk{b}')
            S.dma('sp', lambda e, jj=jj, b=b: e.dma_start(out=KT[jj, :, 4:8, :], in_=mqkT[b][:, 4:8, :]), [r_mqkT[b]], [r_KT[jj]], f'st_mqk{b}')
            for h in range(4):
                S.op('pe', lambda e, h=h, b=b: e.transpose(out=psbf(5)[:, h * 128:(h + 1) * 128], in_=mqkT[b][:, 4 + h, :], identity=ident[:]), [r_mqkT[b], r_ident], [r_ps[5]])
            S.op('dve', lambda e, b=b: e.tensor_copy(out=kmm[b][:], in_=psbf(5)[:, 0:512].rearrange("p (h d) -> p h d", h=4)), [r_ps[5]], [r_kmm[b]])
            S.dma('sp', lambda e, jj=jj, b=b: e.dma_start(out=KM[jj, :, 4:8, :], in_=kmm[b][:]), [r_kmm[b]], [r_KM[jj]], f'st_kmm{b}')


    for it in range(NT + 2):
        if it <= NT:
            p1_front(it)
        if it >= 1:
            p1_back(it - 1)

    S.barrier()
    A2 = Arena(nc, PBASE)
    Sst = A2.t([128, 8, 130], F32); r_Sst = Res("Sst")
    Sbf = [A2.t([128, 8, 130], BF16) for _ in range(2)]; r_Sbf = [Res("Sbf0"), Res("Sbf1")]
    kmt = [A2.t([128, 8, 128], BF16) for _ in range(2)]; r_kmt = [Res("kmt0"), Res("kmt1")]
    vpt = [A2.t([128, 8, 130], BF16) for _ in range(2)]; r_vpt = [Res("vpt0"), Res("vpt1")]
    kti = [A2.t([128, 8, 128], BF16) for _ in range(2)]; r_kti = [Res("kti0"), Res("kti1")]
    A2END = A2.off

    def state_update(kt_src, vp_src, r_k, r_v, cidx, gidx, j, ugroups, kb):
        S.op('dve', lambda e: e.tensor_tensor(out=kti[kb][:], in0=kt_src[:], in1=SC[:, j, cidx, :].unsqueeze(2).to_broadcast([128, 8, 128]), op=ALU.mult),
             [r_k, r_SC[j]], [r_kti[kb]])
        for h in range(8):
            bank, c0 = ugroups[h // 3]
            off = c0 + (h % 3) * 130
            S.op('pe', lambda e, h=h, bank=bank, off=off: e.matmul(psb[bank][:, off:off + 129], lhsT=kti[kb][:, h, :], rhs=vp_src[:, h, 0:129], start=True, stop=True),
                 [r_kti[kb], r_v], [r_ps[bank]])
        for gi, (bank, c0) in enumerate(ugroups):
            nh = 3 if gi < 2 else 2
            S.op('dve', lambda e, gi=gi, bank=bank, c0=c0, nh=nh: e.tensor_tensor(out=Sst[:, gi * 3:gi * 3 + nh, 0:129],
                                                                             in0=psb[bank][:, c0:c0 + nh * 130].rearrange("p (h n) -> p h n", h=nh)[:, :, 0:129],
                                                                             in1=Sst[:, gi * 3:gi * 3 + nh, 0:129], op=ALU.add),
                 [r_ps[bank], r_Sst], [r_Sst])
        S.op('dve', lambda e: e.tensor_tensor(out=Sst[:, :, 0:129], in0=Sst[:, :, 0:129], in1=SC[:, j, gidx, :].unsqueeze(2).to_broadcast([128, 8, 129]), op=ALU.mult),
             [r_Sst, r_SC[j]], [r_Sst])

    S.op('pool', lambda e: e.memset(Sst[:], 0.0), [], [r_Sst])
    for b in range(2):
        S.op('pool', lambda e, b=b: e.memset(Sbf[b][:], 0.0), [], [r_Sbf[b]])
    for j in range(NT - 1, -1, -1):
        b = j % 2
        S.dma('sp', lambda e, j=j, b=b: e.dma_start(out=kmt[b][:], in_=KM[j]), [r_KM[j]], [r_kmt[b]], f'l2km{b}')
        S.dma('sp', lambda e, j=j, b=b: e.dma_start(out=vpt[b][:], in_=VP[j]), [r_VP[j]], [r_vpt[b]], f'l2vp{b}')
        S.op('act', lambda e, b=b: e.copy(out=Sbf[b][:, :, 0:129], in_=Sst[:, :, 0:129]), [r_Sst], [r_Sbf[b]])
        S.dma('sp', lambda e, j=j, b=b: e.dma_start(out=SBS[j], in_=Sbf[b][:]), [r_Sbf[b]], [r_SBS[j]], f's2a{b}')
        if j > 0:
            state_update(kmt[b], vpt[b], r_kmt[b], r_vpt[b], 1, 5, j, ((0, 0), (1, 0), (2, 0)), b)

    S.barrier()
    A3 = Arena(nc, A2END)
    Wo = A3.t([128, 8, D], BF16); r_Wo = Res("Wo")
    Wr = A3.t([128, 8, 16], BF16); r_Wr = Res("Wr")
    wr32 = A3.t([128, 8, 16], F32); r_wr32 = Res("wr32")
    gnb = A3.t([128, D], F32); r_gnb = Res("gnb")
    fgb = A3.t([128, D], F32); r_fgb = Res("fgb")
    qtt = [A3.t([128, 8, 128], BF16) for _ in range(2)]; r_qtt = [Res("qtt0"), Res("qtt1")]
    ktt = [A3.t([128, 8, 128], BF16) for _ in range(2)]; r_ktt = [Res("ktt0"), Res("ktt1")]
    sbt = [A3.t([128, 8, 130], BF16) for _ in range(2)]; r_sbt = [Res("sbt0"), Res("sbt1")]
    got = [A3.t([128, D], F32) for _ in range(2)]; r_got = [Res("got0"), Res("got1")]
    xt2 = [A3.t([128, D], F32) for _ in range(2)]; r_xt2 = [Res("xt20"), Res("xt21")]
    PF = [A3.t([128, 8, 128], BF16) for _ in range(2)]; r_PF = [Res("PF0"), Res("PF1")]
    PB = [A3.t([128, 8, 128], BF16) for _ in range(2)]; r_PB = [Res("PB0"), Res("PB1")]
    Sfb = [A3.t([128, 8, 130], BF16) for _ in range(2)]; r_Sfb = [Res("Sfb0"), Res("Sfb1")]
    Y = [A3.t([128, D], F32) for _ in range(2)]; r_Y = [Res("Y0"), Res("Y1")]
    dn = A3.t([128, 3, 16], F32); r_dn = Res("dn")
    bst = A3.t([128, 8, 6], F32); r_bst = Res("bst")
    mv = A3.t([128, 8, 2], F32); r_mv = Res("mv")
    ybf = A3.t([128, D], BF16); r_ybf = Res("ybf")
    yT = A3.t([128, 8, 128], BF16); r_yT = Res("yT")
    h1 = [A3.t([128, D], F32) for _ in range(2)]; r_h1 = [Res("h10"), Res("h11")]
    u2x = [A3.t([128, XW], BF16) for _ in range(2)]; r_u2x = [Res("u2x0"), Res("u2x1")]
    u2T = A3.t([128, 8, 128], BF16); r_u2T = Res("u2T")
    sm = A3.t([128, 4], F32); r_sm = Res("sm")
    junk2 = A3.t([128, D], BF16); r_junk2 = Res("junk2")
    ss2 = A3.t([128, 1], F32); r_ss2 = Res("ss2")
    rstd2 = A3.t([128, 1], F32); r_rstd2 = Res("rstd2")
    ex = A3.t([128, 16], F32); r_ex = Res("ex")
    r_U2X = [Res(f"U2X{j}") for j in range(NT)]
    r_OACC = Res("OACC")

    for c in range(8):
        b = c % 2
        S.dma('sp', lambda e, b=b, c=c: e.dma_start(out=xt2[b][:], in_=w_out[c * 128:(c + 1) * 128, :]), [], [r_xt2[b]], f'xt2{b}')
        S.op('dve' if c % 2 else 'act', (lambda e, b=b, c=c: e.tensor_copy(out=Wo[:, c, :], in_=xt2[b][:])) if c % 2 else
             (lambda e, b=b, c=c: e.copy(out=Wo[:, c, :], in_=xt2[b][:])), [r_xt2[b]], [r_Wo])
    S.dma('sp', lambda e: e.dma_start(out=wr32[:], in_=w_router.rearrange("(c p) n -> p c n", p=128)), [], [r_wr32], 'w3')
    S.op('dve', lambda e: e.tensor_copy(out=Wr[:], in_=wr32[:]), [r_wr32], [r_Wr])
    S.dma('sp', lambda e: e.dma_start(out=gnb[:], in_=gn_g.to_broadcast([128, D])), [], [r_gnb], 'w4a')
    S.dma('sp', lambda e: e.dma_start(out=fgb[:], in_=ffn_g.to_broadcast([128, D])), [], [r_fgb], 'w4b')
    S.op('pool', lambda e: e.memset(Sst[:], 0.0), [], [r_Sst])
    for b in range(2):
        S.op('pool', lambda e, b=b: e.memset(Sfb[b][:], 0.0), [], [r_Sfb[b]])
        S.op('pool', lambda e, b=b: e.memset(u2x[b][:], 0.0), [], [r_u2x[b]])
    S.op('pool', lambda e: e.memset(xt2[0][:], 0.0), [r_Wo], [r_xt2[0]])

    r_ps6d = Res('ps6d'); r_ps6l = Res('ps6l')

    def p2_front(j):
        b = j % 2
        S.dma('sp', lambda e, j=j, b=b: e.dma_start(out=qtt[b][:], in_=QT[j]), [r_QT[j]], [r_qtt[b]], f'l2q{b}')
        S.dma('sp', lambda e, j=j, b=b: e.dma_start(out=ktt[b][:], in_=KT[j]), [r_KT[j]], [r_ktt[b]], f'l2k{b}')
        S.dma('sp', lambda e, j=j, b=b: e.dma_start(out=kmt[b][:], in_=KM[j]), [r_KM[j]], [r_kmt[b]], f'l2km{b}')
        S.dma('sp', lambda e, j=j, b=b: e.dma_start(out=vpt[b][:], in_=VP[j]), [r_VP[j]], [r_vpt[b]], f'l2vp{b}')
        S.dma('sp', lambda e, j=j, b=b: e.dma_start(out=sbt[b][:], in_=SBS[j]), [r_SBS[j]], [r_sbt[b]], f'l2s{b}')
        S.dma('sp', lambda e, j=j, b=b: e.dma_start(out=got[b][:], in_=GO[j]), [r_GO[j]], [r_got[b]], f'l2g{b}')
        if j == 0:
            S.dma('sp', lambda e: e.dma_start(out=xt2[0][PADF:128, :], in_=meta[:, :]), [], [r_xt2[0]], 'xt20')
        else:
            S.dma('sp', lambda e, j=j, b=b: e.dma_start(out=xt2[b][:], in_=x_rows(j)), [], [r_xt2[b]], f'xt2{b}')
        for h in range(8):
            S.op('pe', lambda e, h=h, b=b: e.matmul(psb[h // 4][:, (h % 4) * 128:(h % 4 + 1) * 128], lhsT=ktt[b][:, h, :], rhs=qtt[b][:, h, :], start=True, stop=True),
                 [r_ktt[b], r_qtt[b]], [r_ps[h // 4]])
        for h in range(8):
            S.op('dve', lambda e, h=h, b=b, j=j: e.scalar_tensor_tensor(out=PF[b][:, h, :], in0=psb[h // 4][:, (h % 4) * 128:(h % 4 + 1) * 128], scalar=SC[:, j, 0, h:h + 1],
                                                                       in1=maskfb[:, 0, :], op0=ALU.mult, op1=ALU.mult),
                 [r_ps[h // 4], r_SC[j], r_mask], [r_PF[b]])
            S.op('dve', lambda e, h=h, b=b, j=j: e.scalar_tensor_tensor(out=PB[b][:, h, :], in0=psb[h // 4][:, (h % 4) * 128:(h % 4 + 1) * 128], scalar=SC[:, j, 1, h:h + 1],
                                                                       in1=maskfb[:, 1, :], op0=ALU.mult, op1=ALU.mult),
                 [r_ps[h // 4], r_SC[j], r_mask], [r_PB[b]])
        for h in range(8):
            S.op('pe', lambda e, h=h, b=b: e.matmul(psb[6][:, h:h + 1], lhsT=PF[b][:, h, :], rhs=vpt[b][:, h, 128:129], start=True, stop=False), [r_PF[b], r_vpt[b]], [r_ps6d])
            S.op('pe', lambda e, h=h, b=b: e.matmul(psb[6][:, h:h + 1], lhsT=qtt[b][:, h, :], rhs=Sfb[b][:, h, 128:129], start=False, stop=True), [r_qtt[b], r_Sfb[b]], [r_ps6d])
            S.op('pe', lambda e, h=h, b=b: e.matmul(psb[6][:, 8 + h:9 + h], lhsT=PB[b][:, h, :], rhs=vpt[b][:, h, 128:129], start=True, stop=False), [r_PB[b], r_vpt[b]], [r_ps6d])
            S.op('pe', lambda e, h=h, b=b: e.matmul(psb[6][:, 8 + h:9 + h], lhsT=qtt[b][:, h, :], rhs=sbt[b][:, h, 128:129], start=False, stop=True), [r_qtt[b], r_sbt[b]], [r_ps6d])
        for h in range(8):
            ob = 2 + h // 2
            c0 = (h % 2) * 256
            S.op('pe', lambda e, h=h, b=b, ob=ob, c0=c0: e.matmul(psb[ob][:, c0:c0 + 128], lhsT=PF[b][:, h, :], rhs=vpt[b][:, h, 0:128], start=True, stop=False), [r_PF[b], r_vpt[b]], [r_ps[ob]])
            S.op('pe', lambda e, h=h, b=b, ob=ob, c0=c0: e.matmul(psb[ob][:, c0:c0 + 128], lhsT=qtt[b][:, h, :], rhs=Sfb[b][:, h, 0:128], start=False, stop=True), [r_qtt[b], r_Sfb[b]], [r_ps[ob]])
            S.op('pe', lambda e, h=h, b=b, ob=ob, c0=c0: e.matmul(psb[ob][:, c0 + 128:c0 + 256], lhsT=PB[b][:, h, :], rhs=vpt[b][:, h, 0:128], start=True, stop=False), [r_PB[b], r_vpt[b]], [r_ps[ob]])
            S.op('pe', lambda e, h=h, b=b, ob=ob, c0=c0: e.matmul(psb[ob][:, c0 + 128:c0 + 256], lhsT=qtt[b][:, h, :], rhs=sbt[b][:, h, 0:128], start=False, stop=True), [r_qtt[b], r_sbt[b]], [r_ps[ob]])
        state_update(kmt[b], vpt[b], r_kmt[b], r_vpt[b], 0, 4, j, ((0, 0), (1, 0), (6, 64)), b)
        S.op('act', lambda e, b=b: e.copy(out=Sfb[1 - b][:, :, 0:129], in_=Sst[:, :, 0:129]), [r_Sst], [r_Sfb[1 - b]])

    def p2_back_a(j):
        b = j % 2
        dnv = dn[:].rearrange("p k (a h) -> p k a h", a=2)
        S.op('dve', lambda e, j=j: e.tensor_tensor(out=dnv[:, 0], in0=psb[6][:, 0:16].rearrange("p (a h) -> p a h", a=2), in1=SC[:, j, 2:4, :], op=ALU.mult), [r_ps6d, r_SC[j]], [r_dn])
        S.op('dve', lambda e: e.scalar_tensor_tensor(out=dn[:, 1, :], in0=dn[:, 0, :], scalar=-1.0, in1=dn[:, 0, :], op0=ALU.mult, op1=ALU.max), [r_dn], [r_dn])
        S.op('dve', lambda e: e.tensor_scalar(out=dn[:, 1, :], in0=dn[:, 1, :], scalar1=1.0, scalar2=None, op0=ALU.max), [r_dn], [r_dn])
        S.op('dve', lambda e: e.reciprocal(out=dn[:, 0, :], in_=dn[:, 1, :]), [r_dn], [r_dn])
        S.op('dve', lambda e, j=j: e.tensor_tensor(out=dnv[:, 2], in0=dnv[:, 0], in1=SC[:, j, 2:4, :], op=ALU.mult), [r_dn, r_SC[j]], [r_dn])
        S.op('dve', lambda e, j=j: e.tensor_copy(out=dnv[:, 2, :, 0:4], in_=SC[:, j, 2:4, 0:4]), [r_dn, r_SC[j]], [r_dn])
        for h in range(8):
            ob = 2 + h // 2
            c0 = (h % 2) * 256
            S.op('act', lambda e, h=h, ob=ob, c0=c0, b=b: e.activation(out=Y[b][:, h * 128:(h + 1) * 128], in_=psb[ob][:, c0:c0 + 128], func=AF.Copy, scale=dn[:, 2, h:h + 1]),
                 [r_ps[ob], r_dn], [r_Y[b]])
            S.op('dve', lambda e, h=h, ob=ob, c0=c0, b=b: e.scalar_tensor_tensor(out=Y[b][:, h * 128:(h + 1) * 128], in0=psb[ob][:, c0 + 128:c0 + 256], scalar=dn[:, 2, 8 + h:9 + h],
                                                                             in1=Y[b][:, h * 128:(h + 1) * 128], op0=ALU.mult, op1=ALU.add),
                 [r_ps[ob], r_dn, r_Y[b]], [r_Y[b]])

    def p2_back_b(j):
        b = j % 2
        S.op('dve', lambda e, b=b: e.tensor_tensor(out=Y[b][:, 512:1024], in0=Y[b][:, 512:1024], in1=got[b][:, 512:1024], op=ALU.mult), [r_Y[b], r_got[b]], [r_Y[b]])
        for h in range(8):
            S.op('dve', lambda e, h=h, b=b: e.bn_stats(out=bst[:, h, :], in_=Y[b][:, h * 128:(h + 1) * 128]), [r_Y[b]], [r_bst])
            S.op('dve', lambda e, h=h: e.bn_aggr(out=mv[:, h, :], in_=bst[:, h, :]), [r_bst], [r_mv])
        S.op('act', lambda e: e.activation(out=mv[:, :, 1], in_=mv[:, :, 1], func=AF.Sqrt, bias=EPS), [r_mv], [r_mv])
        S.op('dve', lambda e: e.reciprocal(out=mv[:, :, 1], in_=mv[:, :, 1]), [r_mv], [r_mv])
        for h in range(8):
            hs = slice(h * 128, (h + 1) * 128)
            S.op('dve', lambda e, h=h, hs=hs, b=b: e.scalar_tensor_tensor(out=Y[b][:, hs], in0=Y[b][:, hs], scalar=mv[:, h, 0:1], in1=gnb[:, hs], op0=ALU.subtract, op1=ALU.mult),
                 [r_Y[b], r_mv, r_gnb], [r_Y[b]])
            if h < 4:
                S.op('dve', lambda e, h=h, hs=hs, b=b: e.scalar_tensor_tensor(out=ybf[:, hs], in0=Y[b][:, hs], scalar=mv[:, h, 1:2], in1=got[b][:, hs], op0=ALU.mult, op1=ALU.mult),
                     [r_Y[b], r_mv, r_got[b]], [r_ybf])
            else:
                S.op('act', lambda e, h=h, hs=hs, b=b: e.activation(out=ybf[:, hs], in_=Y[b][:, hs], func=AF.Copy, scale=mv[:, h, 1:2]), [r_Y[b], r_mv], [r_ybf])
        for c in range(8):
            S.op('pe', lambda e, c=c: e.transpose(out=psbf(7)[:, c * 128:(c + 1) * 128], in_=ybf[:, c * 128:(c + 1) * 128], identity=ident[:]), [r_ybf, r_ident], [r_ps[7]])
        S.op('act', lambda e: e.copy(out=yT[:], in_=psbf(7).rearrange("p (c t) -> p c t", c=8)), [r_ps[7]], [r_yT])
        for half in range(2):
            bank = half
            for c in range(8):
                S.op('pe', lambda e, c=c, half=half, bank=bank: e.matmul(psb[bank][:], lhsT=yT[:, c, :], rhs=Wo[:, c, half * 512:(half + 1) * 512], start=(c == 0), stop=(c == 7)),
                     [r_yT, r_Wo], [r_ps[bank]])
            S.op('dve', lambda e, half=half, bank=bank, b=b: e.tensor_tensor(out=h1[b][:, half * 512:(half + 1) * 512], in0=psb[bank][:], in1=xt2[b][:, half * 512:(half + 1) * 512], op=ALU.add),
                 [r_ps[bank], r_xt2[b]], [r_h1[b]])
        S.dma('sp', lambda e, j=j, b=b: e.dma_start(out=OACC[j * 128:(j + 1) * 128, :], in_=h1[b][:]), [r_h1[b]], [r_OACC], f'st_h1{b}')
        S.op('act', lambda e, b=b: e.activation(out=junk2[:], in_=h1[b][:], func=AF.Square, accum_out=ss2[:]), [r_h1[b]], [r_junk2, r_ss2])
        S.op('act', lambda e: e.activation(out=rstd2[:], in_=ss2[:], func=AF.Sqrt, scale=1.0 / D, bias=EPS), [r_ss2], [r_rstd2])
        S.op('dve', lambda e: e.reciprocal(out=rstd2[:], in_=rstd2[:]), [r_rstd2], [r_rstd2])
        S.op('dve', lambda e, b=b: e.scalar_tensor_tensor(out=u2x[b][:, 0:D], in0=h1[b][:], scalar=rstd2[:], in1=fgb[:], op0=ALU.mult, op1=ALU.mult),
             [r_h1[b], r_rstd2, r_fgb], [r_u2x[b]])
        for c in range(8):
            S.op('pe', lambda e, c=c, b=b: e.transpose(out=psbf(7)[:, c * 128:(c + 1) * 128], in_=u2x[b][:, c * 128:(c + 1) * 128], identity=ident[:]), [r_u2x[b], r_ident], [r_ps[7]])
        S.op('act', lambda e: e.copy(out=u2T[:], in_=psbf(7).rearrange("p (c t) -> p c t", c=8)), [r_ps[7]], [r_u2T])
        for c in range(8):
            S.op('pe', lambda e, c=c: e.matmul(psb[6][:, 16:32], lhsT=u2T[:, c, :], rhs=Wr[:, c, :], start=(c == 0), stop=(c == 7)), [r_u2T, r_Wr], [r_ps6l])
        S.op('dve', lambda e: e.tensor_reduce(out=sm[:, 0:1], in_=psb[6][:, 16:32], axis=AX.X, op=ALU.max, negate=True), [r_ps6l], [r_sm])
        S.op('act', lambda e: e.activation(out=ex[:], in_=psb[6][:, 16:32], func=AF.Exp, bias=sm[:, 0:1], accum_out=sm[:, 1:2]), [r_ps6l, r_sm], [r_ex, r_sm])
        S.op('dve', lambda e: e.reciprocal(out=sm[:, 2:3], in_=sm[:, 1:2]), [r_sm], [r_sm])
        if j == 0:
            S.op('dve', lambda e: e.tensor_tensor(out=sm[:, 2:3], in0=sm[:, 2:3], in1=padm[:], op=ALU.mult), [r_sm, r_padm], [r_sm])
        S.op('dve', lambda e, j=j: e.tensor_scalar(out=AFF[:, j, :], in0=ex[:], scalar1=sm[:, 2:3], scalar2=None, op0=ALU.mult), [r_ex, r_sm], [r_AFF[j]])
        S.op('dve', lambda e, j=j, b=b: e.tensor_copy(out=u2x[b][:, D:D + 32].bitcast(F32), in_=AFF[:, j, :]), [r_AFF[j]], [r_u2x[b]])
        S.op('dve', lambda e, j=j, b=b: e.tensor_copy(out=u2x[b][:, D + 32:D + 34].bitcast(I32), in_=tok[:, j:j + 1]), [r_tok], [r_u2x[b]])
        S.dma('sp', lambda e, j=j, b=b: e.dma_start(out=U2X[j * 128:(j + 1) * 128, :], in_=u2x[b][:]), [r_u2x[b]], [r_U2X[j]], f'st_u2{b}')


    for it in range(NT + 1):
        if it >= 1:
            p2_back_a(it - 1)
        if it < NT:
            p2_front(it)
        if it >= 1:
            p2_back_b(it - 1)

    S.barrier()
    A4 = Arena(nc, PBASE)
    CMP = A4.t([128, NT, 16], F32); r_CMP = Res("CMP")
    SELb = A4.t([128, NT * 16], BF16); r_SELb = Res("SELb")
    lo = A4.t([128, 16], F32); r_lo = Res("lo")
    hi = A4.t([128, 16], F32); r_hi = Res("hi")
    mid = A4.t([128, 16], F32); r_mid = Res("mid")
    pc = A4.t([128, 16], F32); r_pc = Res("pc")
    mm = A4.t([128, 16], F32); r_mm = Res("mm")
    nm = A4.t([128, 16], F32); r_nm = Res("nm")
    t1 = A4.t([128, 16], F32); r_t1 = Res("t1")
    ones32 = A4.t([128, 128], F32); r_ones32 = Res("ones32")
    onesb = A4.t([128, 128], BF16); r_onesb = Res("onesb")
    sltb = A4.t([128, 128], BF16); r_sltb = Res("sltb")
    onesNT = A4.t([128, NT], F32); r_onesNT = Res("onesNT")
    WIT = A4.t([128, NT, 16], F32); r_WIT = Res("WIT")
    TOT = A4.t([128, 16, NT], F32); r_TOT = Res("TOT")
    INC = A4.t([128, 16, NT], F32); r_INC = Res("INC")
    POSf = A4.t([128, NT, 16], F32); r_POSf = Res("POSf")
    POSi = A4.t([128, NT, 16], I32); r_POSi = Res("POSi")
    A4END = A4.off
    NI = NT * 16
    all_AFF = r_AFF

    S.op('pool', lambda e: e.memset(ones32[:], 1.0), [], [r_ones32])
    S.op('pool', lambda e: e.memset(onesb[:], 1.0), [], [r_onesb])
    S.op('pool', lambda e: e.memset(onesNT[:], 1.0), [], [r_onesNT])
    S.op('pool', lambda e: e.memset(lo[:], 0.0), [], [r_lo])
    S.op('pool', lambda e: e.memset(hi[:], 1.0), [], [r_hi])
    S.dma('sp', lambda e: e.dma_start(out=sltb[:], in_=c_slt[:, :]), [], [r_sltb], 'c5')

    def thr_cmp(th, r_th):
        S.op('dve', lambda e: e.tensor_tensor(out=CMP[:], in0=AFF[:], in1=th[:].unsqueeze(1).to_broadcast([128, NT, 16]), op=ALU.is_ge),
             all_AFF + [r_th], [r_CMP])

    NITER = 34
    for it in range(NITER):
        S.op('dve', lambda e: e.tensor_tensor(out=mid[:], in0=lo[:], in1=hi[:], op=ALU.add), [r_lo, r_hi], [r_mid])
        S.op('dve', lambda e: e.tensor_scalar(out=mid[:], in0=mid[:], scalar1=0.5, scalar2=None, op0=ALU.mult), [r_mid], [r_mid])
        thr_cmp(mid, r_mid)
        S.op('dve', lambda e: e.tensor_reduce(out=pc[:], in_=CMP[:].rearrange("p j e -> p e j"), axis=AX.X, op=ALU.add), [r_CMP], [r_pc])
        S.op('pe', lambda e: e.matmul(psb[0][:, 0:16], lhsT=ones32[:], rhs=pc[:], start=True, stop=True), [r_ones32, r_pc], [r_ps[0]])
        S.op('dve', lambda e: e.tensor_scalar(out=mm[:], in0=psb[0][:, 0:16], scalar1=float(CAP) - 0.5, scalar2=None, op0=ALU.is_ge), [r_ps[0]], [r_mm])
        S.op('dve', lambda e: e.tensor_scalar(out=nm[:], in0=mm[:], scalar1=-1.0, scalar2=1.0, op0=ALU.mult, op1=ALU.add), [r_mm], [r_nm])
        S.op('dve', lambda e: e.tensor_tensor(out=t1[:], in0=mm[:], in1=mid[:], op=ALU.mult), [r_mm, r_mid], [r_t1])
        S.op('dve', lambda e: e.tensor_tensor(out=lo[:], in0=nm[:], in1=lo[:], op=ALU.mult), [r_nm, r_lo], [r_lo])
        S.op('dve', lambda e: e.tensor_tensor(out=lo[:], in0=lo[:], in1=t1[:], op=ALU.add), [r_lo, r_t1], [r_lo])
        S.op('dve', lambda e: e.tensor_tensor(out=t1[:], in0=nm[:], in1=mid[:], op=ALU.mult), [r_nm, r_mid], [r_t1])
        S.op('dve', lambda e: e.tensor_tensor(out=hi[:], in0=mm[:], in1=hi[:], op=ALU.mult), [r_mm, r_hi], [r_hi])
        S.op('dve', lambda e: e.tensor_tensor(out=hi[:], in0=hi[:], in1=t1[:], op=ALU.add), [r_hi, r_t1], [r_hi])
    thr_cmp(lo, r_lo)
    S.op('dve', lambda e: e.tensor_copy(out=SELb[:], in_=CMP[:].rearrange("p j e -> p (j e)")), [r_CMP], [r_SELb])
    nchunk = (NI + 511) // 512
    for ci in range(nchunk):
        n0 = ci * 512
        n1 = min(NI, n0 + 512)
        bank = ci % 3
        S.op('pe', lambda e, n0=n0, n1=n1, bank=bank: e.matmul(psb[bank][:, 0:n1 - n0], lhsT=sltb[:], rhs=SELb[:, n0:n1], start=True, stop=True), [r_sltb, r_SELb], [r_ps[bank]])
        S.op('act', lambda e, n0=n0, n1=n1, bank=bank: e.copy(out=WIT[:].rearrange("p j e -> p (j e)")[:, n0:n1], in_=psb[bank][:, 0:n1 - n0]), [r_ps[bank]], [r_WIT])
        S.op('pe', lambda e, n0=n0, n1=n1, bank=bank: e.matmul(psb[3 + bank][:, 0:n1 - n0], lhsT=onesb[:], rhs=SELb[:, n0:n1], start=True, stop=True), [r_onesb, r_SELb], [r_ps[3 + bank]])
        j0 = n0 // 16
        j1 = n1 // 16
        S.op('act', lambda e, n0=n0, n1=n1, bank=bank, j0=j0, j1=j1: e.copy(out=TOT[:, :, j0:j1], in_=psb[3 + bank][:, 0:n1 - n0].rearrange("p (j e) -> p e j", e=16)),
             [r_ps[3 + bank]], [r_TOT])
    for ee in range(16):
        S.op('dve', lambda e, ee=ee: e.tensor_tensor_scan(out=INC[:, ee, :], data0=onesNT[:], data1=TOT[:, ee, :], initial=0.0, op0=ALU.mult, op1=ALU.add),
             [r_onesNT, r_TOT], [r_INC])
    S.op('dve', lambda e: e.tensor_tensor(out=INC[:], in0=INC[:], in1=TOT[:], op=ALU.subtract), [r_INC, r_TOT], [r_INC])
    S.op('dve', lambda e: e.tensor_tensor(out=POSf[:], in0=WIT[:], in1=INC[:].rearrange("p e j -> p j e"), op=ALU.add), [r_WIT, r_INC], [r_POSf])
    BIG = float(1 << 20)
    S.op('dve', lambda e: e.tensor_scalar(out=POSf[:], in0=POSf[:], scalar1=-BIG, scalar2=None, op0=ALU.add), [r_POSf], [r_POSf])
    S.op('dve', lambda e: e.tensor_tensor(out=POSf[:], in0=POSf[:], in1=CMP[:], op=ALU.mult), [r_POSf, r_CMP], [r_POSf])
    S.op('dve', lambda e: e.tensor_scalar(out=POSf[:], in0=POSf[:], scalar1=BIG, scalar2=None, op0=ALU.add), [r_POSf], [r_POSf])
    S.op('dve', lambda e: e.tensor_copy(out=POSi[:], in_=POSf[:]), [r_POSf], [r_POSi])

    A5 = Arena(nc, A4END)
    u2l = [A5.t([128, XW], BF16) for _ in range(3)]; r_u2l = [Res(f"u2l{i}") for i in range(3)]
    r_XS = [Res(f"XS{e_}") for e_ in range(NEXP)]
    A5END = A5.off
    xsi_last = []
    for e_ in range(NEXP_RUN):
        xsi_last = [S.dma('sp', lambda e, e_=e_: e.dma_start(out=XS[e_][:, :], in_=c_xsinit[:, :]), [], [r_XS[e_]], 'xsi')]
    for j in range(NT):
        b = j % 3
        S.dma('sp', lambda e, j=j, b=b: e.dma_start(out=u2l[b][:], in_=U2X[j * 128:(j + 1) * 128, :]), [r_U2X[j]], [r_u2l[b]], f'u2l{b}')
        for e_ in range(NEXP_RUN):
            S.dma('pool', lambda e, j=j, b=b, e_=e_: e.indirect_dma_start(out=XS[e_], out_offset=bass.IndirectOffsetOnAxis(ap=POSi[:, j, e_:e_ + 1], axis=0),
                                                                         in_=u2l[b][:], in_offset=None, bounds_check=breg(e, NSLR - 1), oob_is_err=False),
                  [r_u2l[b], r_POSi, r_XS[e_]], [], f'sc{b}', after=xsi_last)

    S.barrier()
    A6 = Arena(nc, PBASE0)
    NSTG = 6
    stg = [A6.t([128, 2048], F32) for _ in range(NSTG)]; r_stg = [Res(f"stg{i}") for i in range(NSTG)]
    wgb = [A6.t([128, 8, 256], BF16) for _ in range(2)]; r_wgb = [Res("wgb0"), Res("wgb1")]
    wub = [A6.t([128, 8, 256], BF16) for _ in range(2)]; r_wub = [Res("wub0"), Res("wub1")]
    wdb = A6.t([128, NFC, D], BF16); r_wdb = Res("wdb")
    hidT = A6.t([128, NFC, NSLR], BF16); r_hidT = Res("hidT")
    xsT = A6.t([128, 8, NSLR], BF16); r_xsT = Res("xsT")
    xsl = [A6.t([128, XW], BF16) for _ in range(2)]; r_xsl = [Res("xsl0"), Res("xsl1")]
    meta_sl = A6.t([128, NSL, 64], BF16); r_meta_sl = Res("meta_sl")
    sg = A6.t([128, 512], F32); r_sg = Res("sg")
    yo = [A6.t([128, D], F32) for _ in range(2)]; r_yo = [Res("yo0"), Res("yo1")]
    nstg = 0
    CB = 384 if NSLR % 384 == 0 else 128
    NCB = NSLR // CB
    r_oacc_e = Res("OACCe")
    for e_ in range(NEXP_RUN):
        for s_ in range(NSL):
            b = s_ % 2
            S.dma('sp', lambda e, e_=e_, s_=s_, b=b: e.dma_start(out=xsl[b][:], in_=XS[e_][s_ * 128:(s_ + 1) * 128, :]), [r_XS[e_]], [r_xsl[b]], f'xsl{b}')
            S.op('dve', lambda e, s_=s_, b=b: e.tensor_copy(out=meta_sl[:, s_, :], in_=xsl[b][:, D:D + 64]), [r_xsl[b]], [r_meta_sl])
            for c in range(8):
                S.op('pe', lambda e, c=c, b=b: e.transpose(out=psbf(7)[:, c * 128:(c + 1) * 128], in_=xsl[b][:, c * 128:(c + 1) * 128], identity=ident[:]), [r_xsl[b], r_ident], [r_ps[7]])
            S.op('act', lambda e, s_=s_: e.copy(out=xsT[:, :, s_ * 128:(s_ + 1) * 128], in_=psbf(7).rearrange("p (c t) -> p c t", c=8)), [r_ps[7]], [r_xsT])
        for fb in range(NFC // 2):
            wb_ = fb % 2
            for (wsrc, wdst, r_wd) in ((w_gate, wgb[wb_], r_wgb[wb_]), (w_up, wub[wb_], r_wub[wb_])):
                sb_ = nstg % NSTG; nstg += 1
                S.dma('sp', lambda e, e_=e_, fb=fb, sb_=sb_, wsrc=wsrc: e.dma_start(out=stg[sb_][:].rearrange("p (c n) -> p c n", c=8),
                                                                                  in_=wsrc[e_, :, fb * 256:(fb + 1) * 256].rearrange("(c p) n -> p c n", p=128)), [], [r_stg[sb_]], f'stg{sb_}')
                S.op('dve' if wsrc is w_gate else 'act',
                     (lambda e, sb_=sb_, wdst=wdst: e.tensor_copy(out=wdst[:], in_=stg[sb_][:].rearrange("p (c n) -> p c n", c=8))) if wsrc is w_gate else
                     (lambda e, sb_=sb_, wdst=wdst: e.copy(out=wdst[:], in_=stg[sb_][:].rearrange("p (c n) -> p c n", c=8))),
                     [r_stg[sb_]], [r_wd])
            f2 = fb
            sb_ = nstg % NSTG; nstg += 1
            S.dma('sp', lambda e, e_=e_, f2=f2, sb_=sb_: e.dma_start(out=stg[sb_][:].rearrange("p (a n) -> p a n", a=2),
                                                                   in_=w_down[e_, f2 * 256:(f2 + 1) * 256, :].rearrange("(a p) n -> p a n", p=128)), [], [r_stg[sb_]], f'stg{sb_}')
            S.op('dve' if fb % 2 else 'act',
                 (lambda e, f2=f2, sb_=sb_: e.tensor_copy(out=wdb[:, 2 * f2:2 * f2 + 2, :], in_=stg[sb_][:].rearrange("p (a n) -> p a n", a=2))) if fb % 2 else
                 (lambda e, f2=f2, sb_=sb_: e.copy(out=wdb[:, 2 * f2:2 * f2 + 2, :], in_=stg[sb_][:].rearrange("p (a n) -> p a n", a=2))),
                 [r_stg[sb_]], [r_wdb])
            for fi in range(2):
                fc = fb * 2 + fi
                for cbk in range(NCB):
                    pp = (fc * NCB + cbk) % 3
                    gb, ub = pp * 2, pp * 2 + 1
                    for c in range(8):
                        S.op('pe', lambda e, c=c, gb=gb, wb_=wb_, fi=fi, cbk=cbk: e.matmul(psb[gb][:, 0:CB], lhsT=wgb[wb_][:, c, fi * 128:(fi + 1) * 128],
                                                                                     rhs=xsT[:, c, cbk * CB:(cbk + 1) * CB], start=(c == 0), stop=(c == 7)),
                             [r_wgb[wb_], r_xsT], [r_ps[gb]])
                    for c in range(8):
                        S.op('pe', lambda e, c=c, ub=ub, wb_=wb_, fi=fi, cbk=cbk: e.matmul(psb[ub][:, 0:CB], lhsT=wub[wb_][:, c, fi * 128:(fi + 1) * 128],
                                                                                     rhs=xsT[:, c, cbk * CB:(cbk + 1) * CB], start=(c == 0), stop=(c == 7)),
                             [r_wub[wb_], r_xsT], [r_ps[ub]])
                    S.op('act', lambda e, gb=gb: e.activation(out=sg[:, 0:CB], in_=psb[gb][:, 0:CB], func=AF.Silu), [r_ps[gb]], [r_sg])
                    S.op('dve', lambda e, ub=ub, fc=fc, cbk=cbk: e.tensor_tensor(out=hidT[:, fc, cbk * CB:(cbk + 1) * CB], in0=psb[ub][:, 0:CB], in1=sg[:, 0:CB], op=ALU.mult),
                         [r_ps[ub], r_sg], [r_hidT])
        for s_ in range(NSL):
            yb = s_ % 2
            for half in range(2):
                bank = 6 + half
                for fc in range(NFC):
                    S.op('pe', lambda e, fc=fc, s_=s_, half=half, bank=bank: e.matmul(psb[bank][:], lhsT=hidT[:, fc, s_ * 128:(s_ + 1) * 128], rhs=wdb[:, fc, half * 512:(half + 1) * 512],
                                                                                start=(fc == 0), stop=(fc == NFC - 1)),
                         [r_hidT, r_wdb], [r_ps[bank]])
                gsc = meta_sl[:, s_, 2 * e_:2 * e_ + 2].bitcast(F32)
                S.op('act' if half else 'dve',
                     (lambda e, half=half, bank=bank, yb=yb, gsc=gsc: e.activation(out=yo[yb][:, half * 512:(half + 1) * 512], in_=psb[bank][:], func=AF.Copy, scale=gsc)) if half else
                     (lambda e, half=half, bank=bank, yb=yb, gsc=gsc: e.tensor_scalar(out=yo[yb][:, half * 512:(half + 1) * 512], in0=psb[bank][:], scalar1=gsc, scalar2=None, op0=ALU.mult)),
                     [r_ps[bank], r_meta_sl], [r_yo[yb]])
            tix = meta_sl[:, s_, 32:34].bitcast(I32)
            S.dma('pool', lambda e, yb=yb, tix=tix: e.indirect_dma_start(out=OACC, out_offset=bass.IndirectOffsetOnAxis(ap=tix, axis=0), in_=yo[yb][:], in_offset=None,
                                                                       bounds_check=breg(e, T + NSLR - 1), oob_is_err=True, compute_op=ALU.add),
                  [r_yo[yb], r_meta_sl, r_OACC, r_oacc_e], [r_oacc_e], f'oa{yb}')

    S.barrier()
    A7 = Arena(nc, PBASE0)
    GF = 4
    ft = [[A7.t([128, D], F32) for _ in range(GF)] for _ in range(2)]; r_ft = [[Res(f"ft{i}_{k}") for k in range(GF)] for i in range(2)]
    fo = [[A7.t([128, D], F32) for _ in range(GF)] for _ in range(2)]; r_fo = [[Res(f"fo{i}_{k}") for k in range(GF)] for i in range(2)]
    fj = A7.t([128, D], BF16); r_fj = Res("fj")
    fss = [A7.t([128, 2, GF], F32) for _ in range(2)]; r_fss = [Res("fss0"), Res("fss1")]
    fgn = A7.t([128, D], F32); r_fgn = Res("fgn")
    S.dma('sp', lambda e: e.dma_start(out=fgn[:], in_=fin_g.to_broadcast([128, D])), [], [r_fgn], 'w5')
    outs = []
    tiles = list(range(1, NT))
    for gi in range(0, len(tiles), GF):
        grp = tiles[gi:gi + GF]
        gb = (gi // GF) % 2
        for k, j in enumerate(grp):
            S.dma('sp', lambda e, j=j, gb=gb, k=k: e.dma_start(out=ft[gb][k][:], in_=OACC[j * 128:(j + 1) * 128, :]), [r_oacc_e, r_OACC], [r_ft[gb][k]], f'ft{gb}_{k}')
        for k, j in enumerate(grp):
            S.op('act', lambda e, gb=gb, k=k: e.activation(out=fj[:], in_=ft[gb][k][:], func=AF.Square, accum_out=fss[gb][:, 0, k:k + 1]), [r_ft[gb][k]], [r_fj, r_fss[gb]])
        S.op('act', lambda e, gb=gb: e.activation(out=fss[gb][:, 1, :], in_=fss[gb][:, 0, :], func=AF.Sqrt, scale=1.0 / D, bias=EPS), [r_fss[gb]], [r_fss[gb]])
        S.op('dve', lambda e, gb=gb: e.reciprocal(out=fss[gb][:, 1, :], in_=fss[gb][:, 1, :]), [r_fss[gb]], [r_fss[gb]])
        for k, j in enumerate(grp):
            S.op('dve', lambda e, gb=gb, k=k: e.scalar_tensor_tensor(out=fo[gb][k][:], in0=ft[gb][k][:], scalar=fss[gb][:, 1, k:k + 1], in1=fgn[:], op0=ALU.mult, op1=ALU.mult),
                 [r_ft[gb][k], r_fss[gb], r_fgn], [r_fo[gb][k]])
            outs.append(S.dma('sp', lambda e, j=j, gb=gb, k=k: e.dma_start(out=out[(j - 1) * 128:j * 128, :], in_=fo[gb][k][:]), [r_fo[gb][k]], [Res("o")], f'out{gb}_{k}'))
    S.final_wait('sp', outs[-2 * GF:])
    S.emit(st)
    st.close()
    return nc, dict(T=T, NSLR=NSLR, CAP=CAP, NSL=NSL)


def host_consts(NT, NSLR):
    T = NT * 128
    bf = ml_dtypes.bfloat16
    m = np.arange(128)[:, None]
    l = np.arange(128)[None, :]
    tri_f = (m <= l).astype(np.float32)
    tri_b = (m >= l).astype(np.float32)
    ones = np.ones((128, 128), np.float32)
    rowm = (np.arange(128) >= PADF).astype(np.float32)[:, None]
    c_tri = np.stack([tri_f, tri_b, ones, tri_f * rowm, tri_b * rowm, ones * rowm]).astype(np.float32)
    c_mask = np.stack([tri_f, tri_b]).astype(np.float32)
    c_slt = (m < l).astype(bf)
    pos = (np.arange(T, dtype=np.float32) - np.float32(PADF)).astype(np.float32)
    half = 64
    inv = (np.float32(10000.0) ** (-np.arange(half, dtype=np.float32) / np.float32(half))).astype(np.float32)
    ang = (pos[:, None] * inv[None, :]).astype(np.float32)
    cos = np.cos(ang.astype(np.float64)).astype(np.float32)
    sin = np.sin(ang.astype(np.float64)).astype(np.float32)
    c_cs = np.concatenate([np.tile(cos, (1, 4)), np.tile(sin, (1, 4))], axis=1).astype(np.float32)
    c_tok = (np.arange(128)[:, None] + 128 * np.arange(NT)[None, :]).astype(np.int32)
    c_padm = rowm.astype(np.float32)
    xs = np.zeros((NSLR, XW), dtype=bf)
    tokid = (T + np.arange(NSLR)).astype(np.int32)
    xs_i32 = xs.view(np.int32).reshape(NSLR, XW // 2)
    xs_i32[:, (D + 32) // 2] = tokid
    return dict(c_ident=np.eye(128).astype(bf), c_tri=c_tri, c_mask=c_mask, c_slt=c_slt, c_cs=c_cs,
                c_tok=c_tok, c_padm=c_padm, c_xsinit=xs)


def core_inputs(b, x, meta_tokens, mix_norm_g, w_in, b_gates, conv_w, conv_b, ret_decay_logit, ret_gn_g,
                mlstm_gn_g, w_out, ffn_norm_g, w_router, w_gate, w_up, w_down, final_norm_g, consts):
    f = np.float32
    d = dict(
        x=np.ascontiguousarray(x[b], dtype=f), meta=np.ascontiguousarray(meta_tokens, dtype=f),
        w_in=np.ascontiguousarray(w_in[0], dtype=f), mix_g=np.ascontiguousarray(mix_norm_g[0][None, :], dtype=f),
        b_gates=np.ascontiguousarray(b_gates[0][None, :], dtype=f),
        conv_w=np.ascontiguousarray(conv_w[0], dtype=f), conv_b=np.ascontiguousarray(conv_b[0][None, :], dtype=f),
        rlogit=np.ascontiguousarray(ret_decay_logit[0].reshape(1, 8), dtype=f),
        gn_g=np.ascontiguousarray(np.concatenate([ret_gn_g[0], mlstm_gn_g[0]])[None, :], dtype=f),
        w_out=np.ascontiguousarray(w_out[0], dtype=f), ffn_g=np.ascontiguousarray(ffn_norm_g[0][None, :], dtype=f),
        w_router=np.ascontiguousarray(w_router[0], dtype=f),
        w_gate=np.ascontiguousarray(w_gate[0], dtype=f), w_up=np.ascontiguousarray(w_up[0], dtype=f),
        w_down=np.ascontiguousarray(w_down[0], dtype=f), fin_g=np.ascontiguousarray(final_norm_g[None, :], dtype=f))
    d.update(consts)
    return d


_CACHE = {}


def kernel(**inputs):
    x = np.asarray(inputs['x'])
    B, SEQ, _ = x.shape
    NT = SEQ // 128 + 1
    if NT not in _CACHE:
        _CACHE[NT] = build(NT)
    nc, info = _CACHE[NT]
    consts = host_consts(NT, info['NSLR'])
    args = {k: np.asarray(v) for k, v in inputs.items()}
    in_maps = []
    for c in range(8):
        b = (c // 2) % B
        in_maps.append(core_inputs(b, consts=consts, **args))
    res = run_bass_kernel_spmd(nc, in_maps, core_ids=list(range(8)))
    outs = [np.asarray(res.results[2 * b]["out"]) for b in range(B)]
    return np.stack(outs, axis=0).astype(np.float32)
```
